# Optimizing a Trainium2 kernel written in Bass

```python
import jax, jax.numpy as jnp
from jax import lax
import numpy as np

D_MODEL = 1024
BATCH = 8
SEQ = 8192
DEPTH = 1

D_MIX = D_MODEL
D_LRU = D_MIX // 2
LRU_HEADS = 8
LRU_HEAD_DIM = D_LRU // LRU_HEADS
CONV_WIDTH = 4
LRU_C = 8.0
D_GLA = D_MIX - D_LRU
GLA_HEADS = 4
GLA_DV = D_GLA // GLA_HEADS
GLA_DK = GLA_DV // 2
GLA_GATE_RANK = 16
GLA_GATE_NORM = 16.0
GLA_CHUNK = 64
N_GROUPS = 4
EXPERTS_PER_GROUP = 8
N_EXPERTS = N_GROUPS * EXPERTS_PER_GROUP
TOP_K = 2
D_EXPERT = D_MODEL // 2
MOE_BLOCK = 256
D_IN_PROJ = 2 * D_LRU + 2 * GLA_HEADS * GLA_DK + 2 * D_GLA + GLA_GATE_RANK
EPS = 1e-6

kernel_name = "hybrid_rglru_gla_hmoe_adaln"


def rms_norm(x, gain):
    x32 = x.astype(jnp.float32)
    y = x32 * lax.rsqrt(jnp.mean(x32 * x32, axis=-1, keepdims=True) + EPS)
    return (y * gain.astype(jnp.float32)).astype(x.dtype)


def causal_depthwise_conv(x, w, b):
    seq = x.shape[1]
    xp = jnp.pad(x, ((0, 0), (CONV_WIDTH - 1, 0), (0, 0)))
    return b + sum(w[k] * xp[:, k:k + seq] for k in range(CONV_WIDTH))


def rg_lru(x, w_r, b_r, w_i, b_i, lam):
    bsz, seq, width = x.shape
    xh = x.reshape(bsz, seq, LRU_HEADS, LRU_HEAD_DIM)
    r = jax.nn.sigmoid(jnp.einsum("bshi,hij->bshj", xh, w_r).reshape(bsz, seq, width) + b_r)
    gate_i = jax.nn.sigmoid(jnp.einsum("bshi,hij->bshj", xh, w_i).reshape(bsz, seq, width) + b_i)
    log_a = -LRU_C * r.astype(jnp.float32) * jax.nn.softplus(-lam.astype(jnp.float32))
    a = jnp.exp(log_a)
    u = jnp.sqrt(-jnp.expm1(2.0 * log_a)) * (gate_i * x).astype(jnp.float32)

    def step(h, au):
        a_t, u_t = au
        h = a_t * h + u_t
        return h, h

    _, hs = lax.scan(step, jnp.zeros((bsz, width), jnp.float32),
                     (a.swapaxes(0, 1), u.swapaxes(0, 1)))
    return hs.swapaxes(0, 1).astype(x.dtype)


def gla_chunked(q, k, v, log_alpha):
    bsz, seq, nh, dk = q.shape
    dv = v.shape[-1]
    n = seq // GLA_CHUNK
    f32 = jnp.float32
    q = q.astype(f32).reshape(bsz, n, GLA_CHUNK, nh, dk) * dk ** -0.5
    k = k.astype(f32).reshape(bsz, n, GLA_CHUNK, nh, dk)
    v32 = v.astype(f32).reshape(bsz, n, GLA_CHUNK, nh, dv)
    b = jnp.cumsum(log_alpha.astype(f32).reshape(bsz, n, GLA_CHUNK, nh, dk), axis=2)
    b_last = b[:, :, -1]
    q_dec = q * jnp.exp(b)
    scores = jnp.einsum("bnchd,bnshd->bnhcs", q_dec, k * jnp.exp(-b))
    causal = jnp.tril(jnp.ones((GLA_CHUNK, GLA_CHUNK), dtype=bool))
    scores = jnp.where(causal, scores, 0.0)
    o_intra = jnp.einsum("bnhcs,bnshe->bnche", scores, v32)
    chunk_kv = jnp.einsum("bnchd,bnche->bnhde", k * jnp.exp(b_last[:, :, None] - b), v32)

    def step(state, inp):
        decay, kv = inp
        return decay[..., None] * state + kv, state

    _, states = lax.scan(step, jnp.zeros((bsz, nh, dk, dv), f32),
                         (jnp.exp(b_last).swapaxes(0, 1), chunk_kv.swapaxes(0, 1)))
    o_inter = jnp.einsum("bnchd,bnhde->bnche", q_dec, states.swapaxes(0, 1))
    return (o_intra + o_inter).reshape(bsz, seq, nh, dv).astype(v.dtype)


def hierarchical_moe(h, w_coarse, b_coarse, w_fine, b_fine, w_gate, w_up, w_down):
    bsz, seq, d = h.shape
    f32 = jnp.float32
    n_tok = bsz * seq
    xf = h.reshape(n_tok, d)
    coarse_logits = (xf @ w_coarse).astype(f32) + b_coarse.astype(f32)
    coarse_p = jax.nn.softmax(coarse_logits, axis=-1)
    grp = jnp.argmax(coarse_logits, axis=-1).astype(jnp.int32)
    p_grp = jnp.take_along_axis(coarse_p, grp[:, None], axis=-1)
    fine_logits = ((xf @ w_fine).astype(f32) + b_fine.astype(f32)).reshape(
        n_tok, N_GROUPS, EXPERTS_PER_GROUP)
    fine_logits = jnp.take_along_axis(fine_logits, grp[:, None, None], axis=1)[:, 0]
    top_p, top_i = lax.top_k(jax.nn.softmax(fine_logits, axis=-1), TOP_K)
    weights = p_grp * top_p / jnp.sum(top_p, axis=-1, keepdims=True)
    experts = grp[:, None] * EXPERTS_PER_GROUP + top_i.astype(jnp.int32)

    n_assign = n_tok * TOP_K
    flat_e = experts.reshape(n_assign)
    flat_t = jnp.repeat(jnp.arange(n_tok, dtype=jnp.int32), TOP_K)
    flat_w = weights.reshape(n_assign)
    order = jnp.argsort(flat_e)
    se, st, sw = flat_e[order], flat_t[order], flat_w[order]
    counts = jnp.bincount(flat_e, length=N_EXPERTS)
    starts = jnp.cumsum(counts) - counts
    padded = (counts + MOE_BLOCK - 1) // MOE_BLOCK * MOE_BLOCK
    pends = jnp.cumsum(padded)
    dest = (pends - padded)[se] + jnp.arange(n_assign, dtype=jnp.int32) - starts[se]
    cap = (n_assign + MOE_BLOCK - 1) // MOE_BLOCK * MOE_BLOCK + N_EXPERTS * MOE_BLOCK
    n_blocks = cap // MOE_BLOCK
    buf_t = jnp.zeros((cap,), jnp.int32).at[dest].set(st)
    buf_w = jnp.zeros((cap,), f32).at[dest].set(sw)
    block_e = jnp.minimum(
        jnp.searchsorted(pends, jnp.arange(n_blocks, dtype=jnp.int32) * MOE_BLOCK, side="right"),
        N_EXPERTS - 1)

    def run_block(args):
        tok, e = args
        xb = xf[tok]
        hid = jax.nn.silu(xb @ w_gate[e]) * (xb @ w_up[e])
        return hid @ w_down[e]

    outs = lax.map(run_block, (buf_t.reshape(n_blocks, MOE_BLOCK), block_e))
    y = jnp.zeros((n_tok, d), f32).at[buf_t].add(
        outs.reshape(cap, d).astype(f32) * buf_w[:, None])
    return y.reshape(bsz, seq, d).astype(h.dtype)


def setup_inputs(seed: int = 0) -> dict:
    key = jax.random.key(seed)
    ks = jax.random.split(key, 32)
    f32 = jnp.float32
    L = DEPTH
    qk = GLA_HEADS * GLA_DK

    def nrm(k, shape, scale):
        return jax.random.normal(k, shape, f32) * scale

    a_c = jax.random.uniform(ks[14], (L, D_LRU), f32, 0.9, 0.999)
    a_base = a_c ** (1.0 / LRU_C)
    lru_lambda = jnp.log(a_base) - jnp.log1p(-a_base)
    return {
        "x": nrm(ks[0], (BATCH, SEQ, D_MODEL), 1.0),
        "c": nrm(ks[1], (BATCH, D_MODEL), 1.0),
        "w_ada": nrm(ks[2], (L, D_MODEL, 6 * D_MODEL), D_MODEL ** -0.5),
        "b_ada": nrm(ks[3], (L, 6 * D_MODEL), 0.02),
        "g_mix": 1.0 + nrm(ks[4], (L, D_MODEL), 0.1),
        "g_ffn": 1.0 + nrm(ks[5], (L, D_MODEL), 0.1),
        "g_final": 1.0 + nrm(ks[6], (D_MODEL,), 0.1),
        "w_in": nrm(ks[7], (L, D_MODEL, D_IN_PROJ), D_MODEL ** -0.5),
        "conv_w": nrm(ks[8], (L, CONV_WIDTH, D_LRU), CONV_WIDTH ** -0.5),
        "conv_b": nrm(ks[9], (L, D_LRU), 0.02),
        "lru_wr": nrm(ks[10], (L, LRU_HEADS, LRU_HEAD_DIM, LRU_HEAD_DIM), LRU_HEAD_DIM ** -0.5),
        "lru_br": nrm(ks[11], (L, D_LRU), 0.02),
        "lru_wi": nrm(ks[12], (L, LRU_HEADS, LRU_HEAD_DIM, LRU_HEAD_DIM), LRU_HEAD_DIM ** -0.5),
        "lru_bi": nrm(ks[13], (L, D_LRU), 0.02),
        "lru_lambda": lru_lambda,
        "gla_wa2": nrm(ks[15], (L, GLA_GATE_RANK, qk), GLA_GATE_RANK ** -0.5),
        "gla_ba": nrm(ks[16], (L, qk), 0.02),
        "gla_gnorm": 1.0 + nrm(ks[17], (L, GLA_DV), 0.1),
        "w_out": nrm(ks[18], (L, D_MIX, D_MODEL), D_MIX ** -0.5),
        "w_coarse": nrm(ks[19], (L, D_MODEL, N_GROUPS), D_MODEL ** -0.5),
        "b_coarse": nrm(ks[20], (L, N_GROUPS), 0.01),
        "w_fine": nrm(ks[21], (L, D_MODEL, N_EXPERTS), D_MODEL ** -0.5),
        "b_fine": nrm(ks[22], (L, N_EXPERTS), 0.01),
        "w_gate": nrm(ks[23], (L, N_EXPERTS, D_MODEL, D_EXPERT), D_MODEL ** -0.5),
        "w_up": nrm(ks[24], (L, N_EXPERTS, D_MODEL, D_EXPERT), D_MODEL ** -0.5),
        "w_down": nrm(ks[25], (L, N_EXPERTS, D_EXPERT, D_MODEL), D_EXPERT ** -0.5),
    }


def reference(x, c, w_ada, b_ada, g_mix, g_ffn, g_final, w_in, conv_w, conv_b, lru_wr, lru_br,
              lru_wi, lru_bi, lru_lambda, gla_wa2, gla_ba, gla_gnorm, w_out, w_coarse, b_coarse,
              w_fine, b_fine, w_gate, w_up, w_down):
    bsz, seq, _ = x.shape
    qk = GLA_HEADS * GLA_DK
    splits = [D_LRU, 2 * D_LRU, 2 * D_LRU + qk, 2 * D_LRU + 2 * qk,
              2 * D_LRU + 2 * qk + D_GLA, 2 * D_LRU + 2 * qk + 2 * D_GLA]
    for l in range(DEPTH):
        mod = jax.nn.silu(c) @ w_ada[l] + b_ada[l]
        sh1, sc1, gt1, sh2, sc2, gt2 = [m[:, None, :] for m in jnp.split(mod, 6, axis=-1)]

        h = rms_norm(x, g_mix[l]) * (1 + sc1) + sh1
        proj = h @ w_in[l]
        lru_x, lru_y, q, k, v, g_out, gate_lr = jnp.split(proj, splits, axis=-1)

        lru_h = rg_lru(causal_depthwise_conv(lru_x, conv_w[l], conv_b[l]),
                       lru_wr[l], lru_br[l], lru_wi[l], lru_bi[l], lru_lambda[l])
        lru_out = lru_h * jax.nn.gelu(lru_y)

        log_alpha = jax.nn.log_sigmoid(
            (gate_lr @ gla_wa2[l] + gla_ba[l]).astype(jnp.float32)) / GLA_GATE_NORM
        o = gla_chunked(q.reshape(bsz, seq, GLA_HEADS, GLA_DK),
                        k.reshape(bsz, seq, GLA_HEADS, GLA_DK),
                        v.reshape(bsz, seq, GLA_HEADS, GLA_DV),
                        log_alpha.reshape(bsz, seq, GLA_HEADS, GLA_DK))
        gla_out = rms_norm(o, gla_gnorm[l]).reshape(bsz, seq, D_GLA) * jax.nn.silu(g_out)

        mix = jnp.concatenate([lru_out, gla_out], axis=-1) @ w_out[l]
        x = x + gt1 * mix

        h2 = rms_norm(x, g_ffn[l]) * (1 + sc2) + sh2
        x = x + gt2 * hierarchical_moe(h2, w_coarse[l], b_coarse[l], w_fine[l], b_fine[l],
                                       w_gate[l], w_up[l], w_down[l])
    return rms_norm(x, g_final)
```

```python
import numpy as np
import concourse.bass as bass
import concourse.mybir as mybir
from concourse.bass_utils import run_bass_kernel_spmd

F32 = mybir.dt.float32
BF16 = mybir.dt.bfloat16
I32 = mybir.dt.int32
U32 = mybir.dt.uint32
AF = mybir.ActivationFunctionType
ALU = mybir.AluOpType
AX = mybir.AxisListType

D = 1024
NPROJ = 2576
NE = 32
CAP = 8192 + 512
EPS = 1e-6
T = 256
BLK = 512


class Sched:
    EPOCH = 6000

    def __init__(self, nc, same_engine_sync=True):
        self.nc = nc
        self.ops = []
        self.lw = {}
        self.rd = {}
        self.same = same_engine_sync

    def op(self, eng, fns, reads=(), writes=(), dma=False):
        if callable(fns):
            fns = [fns]
        reads, writes = list(reads), list(writes)
        for k in reads:
            if isinstance(k, str) and k.startswith("ps") and k[2:].isdigit() and k not in writes:
                writes.append(k)
        idx = len(self.ops)
        deps = set()
        for k in reads:
            if k in self.lw:
                deps.add(self.lw[k])
        for k in writes:
            if k in self.lw:
                deps.add(self.lw[k])
            r = self.rd.get(k)
            if r:
                deps.update(r[0].values())
                deps.update(r[1])
        self.ops.append(dict(eng=eng, fns=fns, deps=deps, dma=dma, tag=(list(reads), list(writes))))
        for k in writes:
            self.lw[k] = idx
            self.rd[k] = ({}, [])
        for k in reads:
            r = self.rd.setdefault(k, ({}, []))
            if dma:
                r[1].append(idx)
            else:
                r[0][eng] = idx
        return idx

    def barrier(self):
        last = {}
        dmas = []
        for i, o in enumerate(self.ops):
            if o["dma"]:
                dmas.append(i)
            else:
                last[o["eng"]] = i
        deps = set(last.values()) | set(dmas)
        for eng in ("pe", "act", "dve", "pool", "sp"):
            idx = len(self.ops)
            self.ops.append(dict(eng=eng, fns=[lambda e: e.nop()], deps=set(deps), dma=False))
            last[eng] = idx
        self._pending_dma_done = True
        self.lw = {}
        self.rd = {}
        self._bar = dict(last)
        for eng, i in last.items():
            self.lw[("__bar__", eng)] = i

    def emit(self):
        import os
        lim = int(os.environ.get("OPLIMIT", "0"))
        print("total ops", len(self.ops))
        if lim:
            self.ops = self.ops[:lim]
            o = self.ops[-1]
            print("last op", o["eng"], o.get("tag"))
        nc = self.nc
        engs = ("pe", "act", "dve", "pool", "sp")
        count = {e: 0 for e in engs}
        prog = {e: [] for e in engs}
        dma_pool = {}
        dma_rr = {e: 0 for e in engs}
        dma_val = {}
        NPOOL = {"sp": 10, "pool": 8, "act": 4, "dve": 2, "pe": 2}
        tokens = []
        pre_wait = []
        for o in self.ops:
            e = o["eng"]
            if o["dma"]:
                pl = dma_pool.setdefault(e, [])
                if len(pl) < NPOOL[e]:
                    s = nc.alloc_semaphore(name=f"dq_{e}_{len(pl)}")
                    pl.append(s)
                    dma_val[id(s)] = 0
                s = pl[dma_rr[e] % len(pl)] if len(pl) == NPOOL[e] else pl[-1]
                dma_rr[e] += 1
                prev = dma_val[id(s)]
                dma_val[id(s)] = prev + 16
                tokens.append((s, prev + 16))
                pre_wait.append((s, prev) if prev > 0 else None)
            else:
                c = count[e]
                ep = c // self.EPOCH
                while len(prog[e]) <= ep:
                    prog[e].append(nc.alloc_semaphore(name=f"pg_{e}_{len(prog[e])}"))
                tokens.append((prog[e][ep], c % self.EPOCH + 1))
                pre_wait.append(None)
                count[e] = c + 1
        waited = {e: {} for e in engs}
        streams = {e: [] for e in engs}
        for i, o in enumerate(self.ops):
            e = o["eng"]
            ws = []
            cand = []
            if pre_wait[i] is not None:
                cand.append(pre_wait[i])
            for d in sorted(o["deps"]):
                od = self.ops[d]
                if (not od["dma"]) and od["eng"] == e and (e == "pe" or not self.same):
                    continue
                cand.append(tokens[d])
            for (s, v) in cand:
                w = waited[e]
                if w.get(id(s), 0) >= v:
                    continue
                w[id(s)] = v
                ws.append((s, v))
            streams[e].append((ws, o["fns"], tokens[i], o["dma"]))
        final_waits = []
        for e, pl in dma_pool.items():
            for s in pl:
                if dma_val[id(s)] > 0:
                    final_waits.append((s, dma_val[id(s)]))
        for e in engs:
            if count[e] > 0:
                c = count[e] - 1
                final_waits.append((prog[e][c // self.EPOCH], c % self.EPOCH + 1))

        def run(engine, name):
            for ws, fns, tok, is_dma in streams[name]:
                for (s, v) in ws:
                    engine.wait_ge(s, v)
                ins = None
                for f in fns:
                    ins = f(engine)
                ins.then_inc(tok[0], 16 if is_dma else 1)

        with nc.Block() as block:
            @block.tensor
            def _(eng):
                run(eng, "pe")

            @block.scalar
            def _(eng):
                run(eng, "act")

            @block.vector
            def _(eng):
                run(eng, "dve")

            @block.gpsimd
            def _(eng):
                run(eng, "pool")

            @block.sync
            def _(eng):
                run(eng, "sp")
                for (s, v) in final_waits:
                    eng.wait_ge(s, v)


def act(out, in_, func, bias=None, scale=None, accum_out=None):
    def f(e):
        kw = {}
        if bias is not None:
            kw["bias"] = bias
        if scale is not None:
            kw["scale"] = scale
        if accum_out is not None:
            kw["accum_out"] = accum_out
        return e.activation(out=out, in_=in_, func=func, **kw)
    return f


def ts(out, in0, s1, op0, s2=None, op1=None, accum_out=None):
    def f(e):
        kw = {}
        if op1 is not None:
            kw["op1"] = op1
        if accum_out is not None:
            kw["accum_out"] = accum_out
        return e.tensor_scalar(out=out, in0=in0, scalar1=s1, scalar2=s2, op0=op0, **kw)
    return f


def tt(out, in0, in1, op):
    return lambda e: e.tensor_tensor(out=out, in0=in0, in1=in1, op=op)


def stt(out, in0, scalar, in1, op0, op1):
    return lambda e: e.scalar_tensor_tensor(out=out, in0=in0, scalar=scalar, in1=in1, op0=op0, op1=op1)


def mm(out, lhsT, rhs, start=True, stop=True):
    return lambda e: e.matmul(out, lhsT, rhs, start=start, stop=stop)


def tr(out, in_, ident):
    return lambda e: e.transpose(out, in_, ident)


def dma(out, in_, **kw):
    return lambda e: e.dma_start(out=out, in_=in_, **kw)


def cp(out, in_):
    return lambda e: e.tensor_copy(out=out, in_=in_)


def acp(out, in_):
    return lambda e: e.activation(out=out, in_=in_, func=AF.Copy)


def scan(out, d0, d1, init, op0, op1):
    return lambda e: e.tensor_tensor_scan(out=out, data0=d0, data1=d1, initial=init, op0=op0, op1=op1)


def red(out, in_, op):
    return lambda e: e.tensor_reduce(out=out, in_=in_, axis=AX.X, op=op)


def mset(ap, v):
    return lambda e: e.memset(ap, v)


TB_BADA = 0
TB_GMIX = 48
TB_GFFN = 56
TB_CW = 64
TB_CB = 80
TB_BR = 84
TB_BI = 88
TB_LAM = 92
TB_BA = 96
TB_GN = 98
TB_BADAP = 99
TB_GFFNP = 115
TB_N = 123

CS_ID = 0
CS_U = 128
CS_CM = 256
CS_RM = 384
CS_IO = 896
CS_B5 = 928
CS_HM = 929
CS_TH = 931
NTH = 17
CS_N = 948


class Arena:
    def __init__(self, ap, nwords):
        self.ap = ap
        self.n = nwords
        self.off = 0

    def f32(self, n):
        n = (n + 1) // 2 * 2
        assert self.off + n <= self.n, ("arena overflow", self.off, n, self.n)
        v = self.ap[:, self.off:self.off + n]
        self.off += n
        return v

    def bf(self, nel):
        w = (nel + 1) // 2
        w = (w + 1) // 2 * 2
        v = self.f32(w).bitcast(BF16)
        return v[:, 0:nel]

    def mark(self):
        return self.off

    def reset(self, m):
        self.off = m


def build(S, stop_after=None, dbg=False):
    NT = S // T
    NST = S // 128
    NB = (2 * S) // BLK + NE
    NROWS = NE * CAP + BLK
    NULLSTART = NE * CAP
    nc = bass.Bass("TRN2", target_bir_lowering=False)

    def din(name, shape, dt=F32):
        return nc.dram_tensor(name, shape, dt, kind="ExternalInput").ap()

    x_d = din("x", [S, D])
    cT_d = din("cT", [128, 8])
    wada_d = din("w_ada", [D, 6 * D])
    tab_d = din("tab", [128, TB_N])
    cst_d = din("cst", [128, CS_N])
    tok_d = din("tokid", [128, NST], I32)
    gf_d = din("gf_bc", [128, D])
    brb_d = din("br_bc", [128, 36])
    win_d = din("w_in", [D, NPROJ])
    wout_d = din("w_out", [D, D])
    bdr_d = din("bd_r", [128, 512])
    bdi_d = din("bd_i", [128, 512])
    wa2_d = din("wa2", [16, 256])
    wr_d = din("w_r", [D, 36])
    wg_d = din("w_gate", [NE, D, 512])
    wu_d = din("w_up", [NE, D, 512])
    wd_d = din("w_down", [NE, 512, D])
    out_d = nc.dram_tensor("out", [S, D], F32, kind="ExternalOutput").ap()

    xmid_d = nc.dram_tensor("xmid_scr", [S, D], F32, kind="Internal").ap()
    xn2_d = nc.dram_tensor("xn2_scr", [S, D], BF16, kind="Internal").ap()
    sidx_d = nc.dram_tensor("sidx_scr", [NROWS, 2], I32, kind="Internal").ap()
    ybuf_d = nc.dram_tensor("ybuf_scr", [NB * BLK, D], F32, kind="Internal").ap()
    dbg_d = None
    if dbg:
        dbg_d = nc.dram_tensor("dbg", [S, D], F32, kind="ExternalOutput").ap()
        dbg2_d = nc.dram_tensor("dbg2", [128, 4096], F32, kind="ExternalOutput").ap()

    sc = Sched(nc)
    op = sc.op

    def sb(name, shape, dt=F32):
        return nc.alloc_sbuf_tensor(name + "_sb", shape, dt)[:]

    tab = sb("tab", [128, TB_N])
    cst = sb("cst", [128, CS_N])
    tokid = sb("tokid", [128, NST], I32)
    gf_bc = sb("gf_bc", [128, D])
    gt1_bc = sb("gt1_bc", [128, D])
    gt2_bc = sb("gt2_bc", [128, D])
    brb = sb("brb", [128, 36])
    modT = sb("modT", [128, 48])
    scale1 = sb("scale1", [128, 8])
    scale2 = sb("scale2", [128, 8])
    scale2p = sb("scale2p", [128, 8])
    modP = sb("modP", [128, 16])
    widx = sb("widx", [128, 128], I32)
    sidx4 = sb("sidx4", [128, 4 * 128], I32)
    ltab = sb("ltab", [128, 16])
    ident_bf = sb("ident_bf", [128, 128], BF16)
    ones_bf = sb("ones_bf", [128, 128], BF16)
    ones_f = sb("ones_f", [128, 128])
    ohs = sb("ohs", [128, NST * 2 * 32], BF16)
    pos_all = sb("pos_all", [128, NST * 2])
    eid_all = sb("eid_all", [128, NST * 2])
    wts_all = sb("wts_all", [128, NST * 2])
    Ocum = sb("Ocum", [128, 32])
    pay_i = sb("pay", [128, NST * 4], I32)
    pay_f = pay_i.bitcast(F32)
    slot_u = sb("slot_u", [128, NST * 2], I32)
    slot_c = sb("slot_c", [128, NST * 2], I32)
    tbl_i = sb("tbl_i", [1, 256], I32)
    idx_sb = [sb(f"idx_sb{q}", [128, 8], I32) for q in range(2)]
    ident_f = cst[:, CS_ID:CS_ID + 128]
    Umat = cst[:, CS_U:CS_U + 128]
    cmask = cst[:, CS_CM:CS_CM + 128]
    hmask = cst[:, CS_HM:CS_HM + 2]
    rmask = cst[:, CS_RM:CS_RM + 512]
    iota32 = cst[:, CS_IO:CS_IO + 32]
    blk512 = cst[:, CS_B5:CS_B5 + 1]

    rem = nc.sbuf_bytes_remaining
    rem = rem() if callable(rem) else rem
    ARW = (int(rem) // 4) - 64
    ARW = ARW // 2 * 2
    arena_ap = sb("arena", [128, ARW])
    ar = Arena(arena_ap, ARW)

    ps = [nc.alloc_psum_tensor(f"ps{i}", [128, 512], F32)[:] for i in range(8)]

    m0 = ar.mark()
    op("sp", dma(tab, tab_d), writes=["tab"], dma=True)
    op("sp", dma(cst, cst_d), writes=["cst"], dma=True)
    cT = ar.f32(8)
    op("sp", dma(cT, cT_d), writes=["cT"], dma=True)
    op("sp", dma(tokid, tok_d), writes=["tokid"], dma=True)
    op("sp", dma(gf_bc, gf_d), writes=["gf_bc"], dma=True)
    op("sp", dma(brb, brb_d), writes=["brb"], dma=True)
    sgc = ar.f32(8)
    scT = ar.f32(8)
    op("act", act(sgc, cT, AF.Sigmoid), reads=["cT"], writes=["sgc"])
    op("dve", tt(scT, cT, sgc, ALU.mult), reads=["cT", "sgc"], writes=["scT"])
    op("dve", mset(ones_f, 1.0), writes=["ones_f"])
    op("dve", mset(ones_bf, 1.0), writes=["ones_bf"])
    op("dve", cp(ident_bf, ident_f), reads=["cst"], writes=["ident_bf"])

    zt = ar.f32(2048)
    op("pool", mset(zt, 0.0), writes=["zt"])
    zt_i = zt.bitcast(I32)
    rows_per = 128 * 1024
    r0 = 0
    while r0 < NROWS:
        n = min(rows_per, NROWS - r0)
        np_ = n // 1024
        if np_ > 0:
            op("sp", dma(sidx_d[r0:r0 + np_ * 1024, :].rearrange("(p r) c -> p (r c)", p=np_),
                         zt_i[0:np_, :]), reads=["zt"], writes=["sidx_scr"], dma=True)
            r0 += np_ * 1024
        else:
            op("sp", dma(sidx_d[r0:r0 + n, :].rearrange("(p r) c -> p (r c)", p=1),
                         zt_i[0:1, 0:2 * n]), reads=["zt"], writes=["sidx_scr"], dma=True)
            r0 += n

    wst = [ar.f32(8 * 1024).rearrange("p (k f) -> p k f", k=8) for _ in range(2)]
    for blk in range(6):
        b = blk % 2
        op("sp", dma(wst[b], wada_d[:, blk * 1024:(blk + 1) * 1024].rearrange("(k p) f -> p k f", p=128)),
           writes=[("wst", b)], dma=True)
        fns = []
        for fj in range(8):
            for kc in range(8):
                fns.append(mm(ps[0][:, blk * 8 + fj: blk * 8 + fj + 1],
                              wst[b][:, kc, fj * 128:(fj + 1) * 128], scT[:, kc:kc + 1],
                              start=(kc == 0), stop=(kc == 7)))
        if blk in (3, 4):
            for kk in range(8):
                for kc in range(8):
                    fns.append(mm(ps[0][:, 48 + (blk - 3) * 8 + kk: 48 + (blk - 3) * 8 + kk + 1],
                                  wst[b][:, kc, :].rearrange("p (m kk) -> p kk m", kk=8)[:, kk, :],
                                  scT[:, kc:kc + 1], start=(kc == 0), stop=(kc == 7)))
        op("pe", fns, reads=[("wst", b), "scT"], writes=["ps0"])
    op("dve", tt(modT, ps[0][:, 0:48], tab[:, TB_BADA:TB_BADA + 48], ALU.add),
       reads=["ps0", "tab"], writes=["modT"])
    op("dve", tt(modP, ps[0][:, 48:64], tab[:, TB_BADAP:TB_BADAP + 16], ALU.add),
       reads=["ps0", "tab"], writes=["modP"])
    op("dve", stt(scale2p, modP[:, 8:16], 1.0, tab[:, TB_GFFNP:TB_GFFNP + 8], ALU.add, ALU.mult),
       reads=["modP", "tab"], writes=["scale2p"])
    op("dve", stt(scale1, modT[:, 8:16], 1.0, tab[:, TB_GMIX:TB_GMIX + 8], ALU.add, ALU.mult),
       reads=["modT", "tab"], writes=["scale1"])
    op("dve", stt(scale2, modT[:, 32:40], 1.0, tab[:, TB_GFFN:TB_GFFN + 8], ALU.add, ALU.mult),
       reads=["modT", "tab"], writes=["scale2"])
    bias1 = modT[:, 0:8]
    bias2 = modT[:, 24:32]
    bias2p = modP[:, 0:8]
    Gt = ar.f32(8 * 128).rearrange("p (k f) -> p k f", k=8)
    for (col0, dst, nm) in ((16, gt1_bc, "gt1_bc"), (40, gt2_bc, "gt2_bc")):
        for kc in range(8):
            op("dve", ts(Gt[:, kc, :], ones_f, modT[:, col0 + kc:col0 + kc + 1], ALU.mult),
               reads=["modT", "ones_f"], writes=[("Gt", kc)])
        for half in range(2):
            fns = [mm(ps[1 + half][:, k4 * 128:(k4 + 1) * 128], Gt[:, half * 4 + k4, :], ident_f)
                   for k4 in range(4)]
            op("pe", fns, reads=[("Gt", half * 4 + k4) for k4 in range(4)] + ["cst"],
               writes=[f"ps{1 + half}"])
            op("act", acp(dst[:, half * 512:(half + 1) * 512], ps[1 + half]),
               reads=[f"ps{1 + half}"], writes=[nm])
    t4 = ar.f32(4)
    op("act", act(t4, tab[:, TB_LAM:TB_LAM + 4], AF.Exp, scale=-1.0), reads=["tab"], writes=["t4"])
    op("act", act(t4, t4, AF.Ln, bias=1.0), reads=["t4"], writes=["t4"])
    op("dve", ts(ltab[:, 0:4], t4, -8.0, ALU.mult), reads=["t4"], writes=["ltab"])
    op("dve", ts(ltab[:, 4:8], t4, -16.0, ALU.mult), reads=["t4"], writes=["ltab"])
    op("dve", ts(ltab[:, 8:10], tab[:, TB_BA:TB_BA + 2], -1.0, ALU.mult), reads=["tab"], writes=["ltab"])
    cl = ltab[:, 0:4]
    c2l = ltab[:, 4:8]
    nba = ltab[:, 8:10]
    m_setup_tmp = ar.mark()

    ar.reset(m0)
    w_in_bf = ar.bf(8 * NPROJ).rearrange("p (k f) -> p k f", k=8)
    w_out_s = ar.bf(8 * D).rearrange("p (k f) -> p k f", k=8)
    bdr_bf = ar.bf(512).rearrange("p (c m) -> p c m", c=4)
    bdi_bf = ar.bf(512).rearrange("p (c m) -> p c m", c=4)
    wa2_bf = ar.bf(256)
    wr_bf = ar.bf(8 * 36).rearrange("p (k n) -> p k n", k=8)
    m_w = ar.mark()
    sc.barrier()
    stg = [ar.f32(NPROJ) for _ in range(2)]
    cast_engs = ["dve", "pool", "act"]
    ci = 0
    for kc in range(8):
        b = kc % 2
        op("sp", dma(stg[b], win_d[kc * 128:(kc + 1) * 128, :]), writes=[("stg", b)], dma=True)
        e = cast_engs[ci % 3]
        ci += 1
        op(e, (acp if e == "act" else cp)(w_in_bf[:, kc, :], stg[b]), reads=[("stg", b)], writes=["w_in_bf"])
    for kc in range(8):
        b = kc % 2
        op("sp", dma(stg[b][:, 0:D], wout_d[kc * 128:(kc + 1) * 128, :]), writes=[("stg", b)], dma=True)
        op("dve", tt(w_out_s[:, kc, :], stg[b][:, 0:D], gt1_bc, ALU.mult),
           reads=[("stg", b), "gt1_bc"], writes=["w_out_s"])
    for (src, dst, nm) in ((bdr_d, bdr_bf, "bdr"), (bdi_d, bdi_bf, "bdi")):
        op("sp", dma(stg[0][:, 0:512], src), writes=[("stg", 0)], dma=True)
        op("dve", cp(dst.rearrange("p c m -> p (c m)"), stg[0][:, 0:512]), reads=[("stg", 0)], writes=[nm])
    op("sp", dma(stg[1][0:16, 0:256], wa2_d), writes=[("stg", 1)], dma=True)
    op("dve", cp(wa2_bf[0:16, :], stg[1][0:16, 0:256]), reads=[("stg", 1)], writes=["wa2_bf"])
    op("sp", dma(stg[0][:, 0:288].rearrange("p (k n) -> p k n", k=8),
                 wr_d.rearrange("(k p) n -> p k n", p=128)), writes=[("stg", 0)], dma=True)
    op("dve", cp(wr_bf.rearrange("p k n -> p (k n)"), stg[0][:, 0:288]), reads=[("stg", 0)], writes=["wr_bf"])
    sc.barrier()
    ar.reset(m_w)

    if stop_after == "setup":
        op("sp", dma(dbg2_d[:, 0:48], modT), reads=["modT"], writes=["dbg2"], dma=True)
        op("sp", dma(dbg2_d[:, 1024:2048], gt1_bc), reads=["gt1_bc"], writes=["dbg2"], dma=True)
        op("sp", dma(dbg2_d[:, 64:80], ltab), reads=["ltab"], writes=["dbg2"], dma=True)
        sc.emit()
        return nc
    NJ = T // 128
    NCH = T // 64
    xs = [ar.f32(NJ * D).rearrange("p (j d) -> p j d", j=NJ) for _ in range(2)]
    xn = ar.bf(NJ * D).rearrange("p (j d) -> p j d", j=NJ)
    hT = ar.bf(8 * T).rearrange("p (k t) -> p k t", k=8)
    xc = ar.f32(4 * (T + 4)).rearrange("p (c t) -> p c t", c=4)
    yb = ar.f32(4 * T).rearrange("p (c t) -> p c t", c=4)
    qf = ar.f32(2 * T).rearrange("p (c t) -> p c t", c=2)
    kf = ar.f32(2 * T).rearrange("p (c t) -> p c t", c=2)
    gfb = ar.f32(4 * T).rearrange("p (c t) -> p c t", c=4)
    glT = ar.bf(T)
    vb = ar.bf(NJ * 512).rearrange("p (j e) -> p j e", j=NJ)
    catT = ar.bf(8 * T).rearrange("p (k t) -> p k t", k=8)
    L = []
    for _ in range(2):
        L.append(dict(cv=ar.f32(T), cvb=ar.bf(T), r=ar.f32(T), i=ar.f32(T), a=ar.f32(T),
                      s=ar.f32(T), h=ar.f32(T), t1=ar.f32(T), t2=ar.f32(T)))
    hprev = ar.f32(4)
    lf = ar.f32(2 * T).rearrange("p (c t) -> p c t", c=2)
    cs = ar.f32(2 * T).rearrange("p (c t) -> p c t", c=2)
    eb = ar.f32(2 * T).rearrange("p (c t) -> p c t", c=2)
    enb = ar.f32(2 * T).rearrange("p (c t) -> p c t", c=2)
    dec = ar.f32(2 * NCH).rearrange("p (c n) -> p c n", c=2)
    qd = ar.bf(2 * T).rearrange("p (c t) -> p c t", c=2)
    kd = ar.bf(2 * T).rearrange("p (c t) -> p c t", c=2)
    ke = ar.bf(2 * T).rearrange("p (c t) -> p c t", c=2)
    keT = ar.bf(NJ * 256).rearrange("p (j f) -> p j f", j=NJ)
    scT_sb = ar.bf(512)
    Sf = ar.f32(256).rearrange("p (c e) -> p c e", c=2)
    Sb = [ar.bf(256).rearrange("p (c e) -> p c e", c=2) for _ in range(2)]
    osq = ar.bf(T)
    sd = ar.f32(T)
    rs = ar.f32(T)
    sg = ar.f32(T)
    gs = ar.f32(T)
    to = ar.f32(T)
    stat = ar.f32(16)
    lg = ar.f32(NJ * 36).rearrange("p (j n) -> p j n", j=NJ)
    rt = ar.f32(NJ * 64).rearrange("p (j n) -> p j n", j=NJ)
    mf = ar.f32(NJ * 32).rearrange("p (j n) -> p j n", j=NJ)
    m8 = ar.f32(NJ * 8).rearrange("p (j n) -> p j n", j=NJ)
    oh = ar.f32(NJ * 64).rearrange("p (j n) -> p j n", j=NJ)
    Ot = ar.f32(NJ * 32).rearrange("p (j n) -> p j n", j=NJ)
    tmp32 = ar.f32(NJ * 64).rearrange("p (j n) -> p j n", j=NJ)
    print("arena used (words):", ar.off, "of", ARW)

    for c in range(4):
        op("pool", mset(xc[:, c, 0:4], 0.0), writes=[("xc", c)])
    op("pool", mset(hprev, 0.0), writes=["hprev"])
    op("pool", mset(Sf.rearrange("p c e -> p (c e)"), 0.0), writes=["Sf"])
    op("pool", mset(Sb[0].rearrange("p c e -> p (c e)"), 0.0), writes=[("Sb", 0)])
    op("pool", mset(Sb[1].rearrange("p c e -> p (c e)"), 0.0), writes=[("Sb", 1)])
    op("pool", mset(Ocum, 0.0), writes=["Ocum"])

    mmslot = [0]

    def next_mm():
        bk = 2 + mmslot[0] % 3
        mmslot[0] += 1
        return ps[bk], f"ps{bk}"

    psTb = [ps[0].bitcast(BF16), ps[1].bitcast(BF16)]
    psS = ps[5]
    psKV = ps[5]
    psO = [ps[6], ps[7]]
    qz = [[ar.bf(T) for _ in range(2)] for _ in range(2)]
    osq2 = ar.bf(2 * T)
    sd2 = ar.f32(2 * T)
    rs2 = ar.f32(2 * T)
    sg2 = ar.f32(2 * T)
    gs2 = ar.f32(2 * T)
    to2 = ar.f32(2 * T)
    print("arena used (words):", ar.off, "of", ARW)

    def norm_stats(xsrc_key, xbuf, so):
        for j in range(NJ):
            op("act", act(xn[:, j, :], xbuf[:, j, :], AF.Square, accum_out=stat[:, so + j:so + j + 1]),
               reads=[xsrc_key], writes=[("xn", j), "stat"])
        op("act", act(stat[:, so + 2:so + 4], stat[:, so:so + 2], AF.Sqrt, bias=EPS, scale=1.0 / D),
           reads=["stat"], writes=["stat"])
        op("dve", lambda e: e.reciprocal(out=stat[:, so + 4:so + 6], in_=stat[:, so + 2:so + 4]),
           reads=["stat"], writes=["stat"])
        for j in range(NJ):
            op("dve", ts(xn[:, j, :], xbuf[:, j, :], stat[:, so + 4 + j:so + 5 + j], ALU.mult),
               reads=[xsrc_key, "stat"], writes=[("xn", j)])

    def transposes_to_hT(scale_t, bias_t):
        for hb in range(2):
            fns = []
            for k4 in range(4):
                kc = hb * 4 + k4
                for j in range(NJ):
                    fns.append(tr(psTb[hb][:, k4 * T + j * 128: k4 * T + (j + 1) * 128],
                                  xn[:, j, kc * 128:(kc + 1) * 128], ident_bf))
            op("pe", fns, reads=[("xn", j) for j in range(NJ)] + ["ident_bf"], writes=[f"ps{hb}"])
            for k4 in range(4):
                kc = hb * 4 + k4
                e = "act"
                src = psTb[hb][:, k4 * T:(k4 + 1) * T]
                if e == "act":
                    op("act", act(hT[:, kc, :], src, AF.Identity, bias=bias_t[:, kc:kc + 1],
                                  scale=scale_t[:, kc:kc + 1]),
                       reads=[f"ps{hb}", "scale1", "scale2", "modT"], writes=[("hT", kc)])
                else:
                    op("dve", ts(hT[:, kc, :], src, scale_t[:, kc:kc + 1], ALU.mult,
                                 bias_t[:, kc:kc + 1], ALU.add),
                       reads=[f"ps{hb}", "scale1", "scale2", "modT"], writes=[("hT", kc)])

    hT_keys = [("hT", kc) for kc in range(8)]
    ev_i = [0]

    def evac(dst, src, skey, dkey):
        e = "act" if (ev_i[0] // 2) % 2 == 0 else "dve"
        ev_i[0] += 1
        op(e, (acp if e == "act" else cp)(dst, src), reads=[skey], writes=[dkey])

    op("sp", dma(xs[0], x_d[0:T, :].rearrange("(j p) d -> p j d", p=128)), writes=[("xs", 0)], dma=True)
    for it in range(NT):
        xb = it % 2
        X = xs[xb]
        xk = ("xs", xb)
        if it + 1 < NT:
            op("sp", dma(xs[1 - xb], x_d[(it + 1) * T:(it + 2) * T, :].rearrange("(j p) d -> p j d", p=128)),
               writes=[("xs", 1 - xb)], dma=True)
        norm_stats(xk, X, 0)
        transposes_to_hT(scale1, bias1)

        fm_list = []
        for c in range(4):
            fm_list.append((c * 128, xc[:, c, 4:4 + T], ("xc", c)))
        for c in range(4):
            fm_list.append((512 + c * 128, yb[:, c, :], ("yb", c)))
        for i in range(2):
            fm_list.append((1024 + i * 128, qf[:, i, :], ("qf", i)))
        for i in range(2):
            fm_list.append((1280 + i * 128, kf[:, i, :], ("kf", i)))
        for h in range(4):
            fm_list.append((2048 + h * 128, gfb[:, h, :], ("gf", h)))
        for pi_ in range(0, 16, 2):
            pso, key = next_mm()
            fns = []
            for u in range(2):
                col0 = fm_list[pi_ + u][0]
                fns += [mm(pso[:, u * T:(u + 1) * T], w_in_bf[:, kc, col0:col0 + 128], hT[:, kc, :],
                           start=(kc == 0), stop=(kc == 7)) for kc in range(8)]
            op("pe", fns, reads=hT_keys + ["w_in_bf"], writes=[key])
            for u in range(2):
                _, dst, dkey = fm_list[pi_ + u]
                evac(dst, pso[:, u * T:(u + 1) * T], key, dkey)
        pso, key = next_mm()
        fns = [mm(pso[0:16, 0:T], w_in_bf[:, kc, 2560:2576], hT[:, kc, :], start=(kc == 0), stop=(kc == 7))
               for kc in range(8)]
        op("pe", fns, reads=hT_keys + ["w_in_bf"], writes=[key])
        evac(glT[0:16, :], pso[0:16, 0:T], key, "glT")
        ev_i[0] += 1
        for j in range(NJ):
            pso, key = next_mm()
            fns = [mm(pso, hT[:, kc, j * 128:(j + 1) * 128], w_in_bf[:, kc, 1536:2048],
                      start=(kc == 0), stop=(kc == 7)) for kc in range(8)]
            op("pe", fns, reads=hT_keys + ["w_in_bf"], writes=[key])
            evac(vb[:, j, :], pso, key, ("vb", j))
            ev_i[0] += 1

        for c in range(4):
            B = L[c % 2]
            lk = ("L", c % 2)
            cw = tab[:, TB_CW + c * 4:TB_CW + c * 4 + 4]
            op("dve", ts(B["cv"], xc[:, c, 4:4 + T], cw[:, 3:4], ALU.mult, tab[:, TB_CB + c:TB_CB + c + 1], ALU.add),
               reads=[("xc", c), "tab"], writes=[lk + ("cv",)])
            for k in range(3):
                op("dve", stt(B["cv"], xc[:, c, 1 + k:1 + k + T], cw[:, k:k + 1], B["cv"], ALU.mult, ALU.add),
                   reads=[("xc", c), "tab", lk + ("cv",)], writes=[lk + ("cv",)])
            op("pool", cp(B["cvb"], B["cv"]), reads=[lk + ("cv",)], writes=[lk + ("cvb",)])
            op("pool", cp(xc[:, c, 0:4], xc[:, c, T:T + 4]), reads=[("xc", c)], writes=[("xc", c)])
            pg, kg = next_mm()
            op("pe", [mm(pg[:, 0:T], bdr_bf[:, c, :], B["cvb"]), mm(pg[:, T:2 * T], bdi_bf[:, c, :], B["cvb"])],
               reads=[lk + ("cvb",), "bdr", "bdi"], writes=[kg])
            op("act", act(B["r"], pg[:, 0:T], AF.Sigmoid, bias=tab[:, TB_BR + c:TB_BR + c + 1]),
               reads=[kg, "tab"], writes=[lk + ("r",)])
            op("act", act(B["i"], pg[:, T:2 * T], AF.Sigmoid, bias=tab[:, TB_BI + c:TB_BI + c + 1]),
               reads=[kg, "tab"], writes=[lk + ("i",)])
            op("act", act(B["a"], B["r"], AF.Exp, scale=cl[:, c:c + 1]),
               reads=[lk + ("r",), "ltab"], writes=[lk + ("a",)])
            op("act", act(B["s"], B["r"], AF.Exp, scale=c2l[:, c:c + 1]),
               reads=[lk + ("r",), "ltab"], writes=[lk + ("s",)])
            op("act", act(B["s"], B["s"], AF.Sqrt, bias=1.0, scale=-1.0),
               reads=[lk + ("s",)], writes=[lk + ("s",)])
            op("pool", tt(B["i"], B["i"], B["cv"], ALU.mult), reads=[lk + ("i",), lk + ("cv",)], writes=[lk + ("i",)])
            op("pool", tt(B["i"], B["i"], B["s"], ALU.mult), reads=[lk + ("i",), lk + ("s",)], writes=[lk + ("i",)])
            op("dve", scan(B["h"], B["a"], B["i"], hprev[:, c:c + 1], ALU.mult, ALU.add),
               reads=[lk + ("a",), lk + ("i",), "hprev"], writes=[lk + ("h",)])
            op("pool", cp(hprev[:, c:c + 1], B["h"][:, T - 1:T]), reads=[lk + ("h",)], writes=["hprev"])
            Y = yb[:, c, :]
            op("pool", tt(B["t1"], Y, Y, ALU.mult), reads=[("yb", c)], writes=[lk + ("t1",)])
            op("pool", ts(B["t1"], B["t1"], 0.044715, ALU.mult, 1.0, ALU.add),
               reads=[lk + ("t1",)], writes=[lk + ("t1",)])
            op("pool", tt(B["t1"], B["t1"], Y, ALU.mult), reads=[lk + ("t1",), ("yb", c)], writes=[lk + ("t1",)])
            op("act", act(B["t2"], B["t1"], AF.Sigmoid, scale=1.5957691216057308),
               reads=[lk + ("t1",)], writes=[lk + ("t2",)])
            op("pool", tt(B["t2"], B["t2"], Y, ALU.mult), reads=[lk + ("t2",), ("yb", c)], writes=[lk + ("t2",)])
            op("dve", tt(catT[:, c, :], B["h"], B["t2"], ALU.mult),
               reads=[lk + ("h",), lk + ("t2",)], writes=[("catT", c)])

        pz, kz = next_mm()
        op("pe", [mm(pz[:, i * T:(i + 1) * T], wa2_bf[0:16, i * 128:(i + 1) * 128], glT[0:16, :]) for i in range(2)],
           reads=["glT", "wa2_bf"], writes=[kz])
        for i in range(2):
            op("act", act(lf[:, i, :], pz[:, i * T:(i + 1) * T], AF.Exp, bias=nba[:, i:i + 1], scale=-1.0),
               reads=[kz, "ltab"], writes=[("lf", i)])
            op("act", act(lf[:, i, :], lf[:, i, :], AF.Ln, bias=1.0), reads=[("lf", i)], writes=[("lf", i)])
            op("dve", scan(cs[:, i, :], rmask[:, 0:T], lf[:, i, :], 0.0, ALU.mult, ALU.add),
               reads=[("lf", i), "cst"], writes=[("cs", i)])
            op("act", act(eb[:, i, :], cs[:, i, :], AF.Exp, scale=-1.0 / 16), reads=[("cs", i)], writes=[("eb", i)])
            op("act", act(enb[:, i, :], cs[:, i, :], AF.Exp, scale=1.0 / 16), reads=[("cs", i)], writes=[("enb", i)])
            op("act", act(dec[:, i, 0:NJ], cs[:, i, :].rearrange("p (n t) -> p n t", t=128)[:, :, 127],
                          AF.Exp, scale=-1.0 / 16), reads=[("cs", i)], writes=[("dec", i)])
            op("dve", stt(qd[:, i, :], qf[:, i, :], 0.125, eb[:, i, :], ALU.mult, ALU.mult),
               reads=[("qf", i), ("eb", i)], writes=[("qd", i)])
            for hh in range(2):
                op("pool", ts(qz[i][hh], qd[:, i, :], hmask[:, hh:hh + 1], ALU.mult),
                   reads=[("qd", i), "cst"], writes=[("qz", i, hh)])
            op("dve", tt(kd[:, i, :], kf[:, i, :], enb[:, i, :], ALU.mult),
               reads=[("kf", i), ("enb", i)], writes=[("kd", i)])
            op("pool", tt(ke[:, i, :].rearrange("p (n t) -> p n t", t=128),
                          kd[:, i, :].rearrange("p (n t) -> p n t", t=128),
                          dec[:, i, 0:NJ].unsqueeze(2).to_broadcast([128, NJ, 128]), ALU.mult),
               reads=[("kd", i), ("dec", i)], writes=[("ke", i)])
        fns = []
        for j in range(NJ):
            for i in range(2):
                fns.append(tr(psTb[0][:, (j * 2 + i) * 128:(j * 2 + i + 1) * 128], ke[:, i, j * 128:(j + 1) * 128], ident_bf))
        op("pe", fns, reads=[("ke", 0), ("ke", 1), "ident_bf"], writes=["ps0"])
        op("act", acp(keT.rearrange("p j f -> p (j f)"), psTb[0][:, 0:NJ * 256]), reads=["ps0"], writes=["keT"])
        for j in range(NJ):
            par = (it * NJ + j) % 2
            tsl = slice(j * 128, (j + 1) * 128)
            fns = []
            for h in range(4):
                i, hh = h // 2, h % 2
                fns.append(mm(psS[:, h * 128:(h + 1) * 128], kd[:, i, tsl], qz[i][hh][:, tsl]))
            op("pe", fns, reads=[("kd", 0), ("kd", 1)] + [("qz", i, hh) for i in range(2) for hh in range(2)],
               writes=["ps5"])
            op("dve", tt(scT_sb.rearrange("p (a c) -> p a c", c=128),
                         psS.rearrange("p (a c) -> p a c", c=128),
                         cmask.unsqueeze(1).to_broadcast([128, 4, 128]), ALU.mult),
               reads=["ps5", "cst"], writes=["scT"])
            for i in range(2):
                fns = []
                for hh in range(2):
                    h = 2 * i + hh
                    o_ap = psO[i][:, hh * T + j * 128: hh * T + (j + 1) * 128]
                    fns.append(mm(o_ap, vb[:, j, h * 128:(h + 1) * 128], scT_sb[:, h * 128:(h + 1) * 128],
                                  start=True, stop=False))
                    fns.append(mm(o_ap, Sb[par][:, i, :], qz[i][hh][:, tsl], start=False, stop=True))
                op("pe", fns, reads=[("vb", j), "scT", ("Sb", par), ("qz", i, 0), ("qz", i, 1)], writes=[f"ps{6 + i}"])
            fns = []
            for h in range(4):
                i = h // 2
                fns.append(mm(psKV[:, h * 128:(h + 1) * 128], keT[:, j, i * 128:(i + 1) * 128],
                              vb[:, j, h * 128:(h + 1) * 128]))
            op("pe", fns, reads=["keT", ("vb", j)], writes=["ps5"])
            for h in range(4):
                i, hh = h // 2, h % 2
                r0, r1 = hh * 64, (hh + 1) * 64
                op("dve", stt(Sf[r0:r1, i, :], Sf[r0:r1, i, :], dec[r0:r1, i, j:j + 1],
                              psKV[r0:r1, h * 128:(h + 1) * 128], ALU.mult, ALU.add),
                   reads=["Sf", ("dec", i), "ps5"], writes=["Sf"])
            op("act", acp(Sb[1 - par].rearrange("p c e -> p (c e)"), Sf.rearrange("p c e -> p (c e)")),
               reads=["Sf"], writes=[("Sb", 1 - par)])
        for i in range(2):
            O = psO[i]
            ok = f"ps{6 + i}"
            op("act", act(osq2, O, AF.Square), reads=[ok], writes=["osq"])
            pss, kss = next_mm()
            op("pe", mm(pss, ones_bf, osq2), reads=["osq", "ones_bf"], writes=[kss])
            op("act", act(sd2, pss, AF.Sqrt, bias=EPS, scale=1.0 / 128), reads=[kss], writes=["sd"])
            op("dve", lambda e: e.reciprocal(out=rs2, in_=sd2), reads=["sd"], writes=["rs"])
            G = gfb[:, 2 * i:2 * i + 2, :].rearrange("p c t -> p (c t)")
            gk = [("gf", 2 * i), ("gf", 2 * i + 1)]
            op("act", act(sg2, G, AF.Sigmoid), reads=gk, writes=["sg"])
            op("dve", stt(gs2, G, tab[:, TB_GN:TB_GN + 1], sg2, ALU.mult, ALU.mult),
               reads=gk + ["sg", "tab"], writes=["gs"])
            op("dve", tt(to2, O, rs2, ALU.mult), reads=[ok, "rs"], writes=["to"])
            op("dve", tt(catT[:, 4 + 2 * i:6 + 2 * i, :].rearrange("p c t -> p (c t)"), to2, gs2, ALU.mult),
               reads=["to", "gs"], writes=[("catT", 4 + 2 * i), ("catT", 5 + 2 * i)])

        cat_keys = [("catT", k) for k in range(8)]
        for j in range(NJ):
            for half in range(2):
                pso, key = next_mm()
                fns = [mm(pso, catT[:, kc, j * 128:(j + 1) * 128], w_out_s[:, kc, half * 512:(half + 1) * 512],
                          start=(kc == 0), stop=(kc == 7)) for kc in range(8)]
                op("pe", fns, reads=cat_keys + ["w_out_s"], writes=[key])
                op("dve", tt(X[:, j, half * 512:(half + 1) * 512], X[:, j, half * 512:(half + 1) * 512], pso, ALU.add),
                   reads=[key, xk], writes=[xk])
        op("sp", dma(xmid_d[it * T:(it + 1) * T, :].rearrange("(j p) d -> p j d", p=128), X),
           reads=[xk], writes=["xmid_scr"], dma=True)
        if dbg and stop_after == "mixer":
            op("sp", dma(dbg_d[it * T:(it + 1) * T, :].rearrange("(j p) d -> p j d", p=128), X),
               reads=[xk], writes=["dbg"], dma=True)
            continue

        norm_stats(xk, X, 6)
        op("sp", dma(xn2_d[it * T:(it + 1) * T, :].rearrange("(j p) d -> p j d", p=128), xn),
           reads=[("xn", j) for j in range(NJ)], writes=["xn2_scr"], dma=True)
        transposes_to_hT(scale2, bias2)
        pso, key = next_mm()
        fns = []
        for j in range(NJ):
            for kc in range(8):
                fns.append(mm(pso[:, j * 64:j * 64 + 36], hT[:, kc, j * 128:(j + 1) * 128], wr_bf[:, kc, :],
                              start=(kc == 0), stop=(kc == 7)))
        op("pe", fns, reads=hT_keys + ["wr_bf"], writes=[key])
        for j in range(NJ):
            op("dve", tt(lg[:, j, :], pso[:, j * 64:j * 64 + 36], brb, ALU.add), reads=[key, "brb"], writes=[("rt", j)])
        for j in range(NJ):
            st = it * NJ + j
            rk = ("rt", j)
            op("dve", red(rt[:, j, 0:1], lg[:, j, 0:4], ALU.max), reads=[rk], writes=[rk])
            op("dve", ts(rt[:, j, 4:8], lg[:, j, 0:4], rt[:, j, 0:1], ALU.is_equal), reads=[rk], writes=[rk])
            op("dve", ts(rt[:, j, 8:12], lg[:, j, 0:4], rt[:, j, 0:1], ALU.subtract), reads=[rk], writes=[rk])
            op("act", act(rt[:, j, 8:12], rt[:, j, 8:12], AF.Exp, accum_out=rt[:, j, 1:2]), reads=[rk], writes=[rk])
            op("dve", lambda e, j=j: e.reciprocal(out=rt[:, j, 2:3], in_=rt[:, j, 1:2]), reads=[rk], writes=[rk])
            op("dve", ts(rt[:, j, 12:16], rt[:, j, 4:8], 1e30, ALU.mult, -1e30, ALU.add), reads=[rk], writes=[rk])
            op("dve", tt(mf[:, j, :].rearrange("p (g e) -> p g e", g=4),
                         lg[:, j, 4:36].rearrange("p (g e) -> p g e", g=4),
                         rt[:, j, 12:16].unsqueeze(2).to_broadcast([128, 4, 8]), ALU.add), reads=[rk], writes=[rk])
            op("dve", lambda e, j=j: e.max(out=m8[:, j, :], in_=mf[:, j, :]), reads=[rk], writes=[rk])
            op("dve", ts(oh[:, j, 0:32], mf[:, j, :], m8[:, j, 0:1], ALU.is_equal), reads=[rk], writes=[rk])
            op("dve", ts(oh[:, j, 32:64], mf[:, j, :], m8[:, j, 1:2], ALU.is_equal), reads=[rk], writes=[rk])
            op("dve", cp(ohs[:, st * 64:(st + 1) * 64], oh[:, j, :]), reads=[rk], writes=["ohs"])
            op("dve", tt(rt[:, j, 16:17], m8[:, j, 1:2], m8[:, j, 0:1], ALU.subtract), reads=[rk], writes=[rk])
            op("act", act(rt[:, j, 17:18], rt[:, j, 16:17], AF.Exp), reads=[rk], writes=[rk])
            op("dve", ts(rt[:, j, 18:19], rt[:, j, 17:18], 1.0, ALU.add), reads=[rk], writes=[rk])
            op("dve", lambda e, j=j: e.reciprocal(out=rt[:, j, 19:20], in_=rt[:, j, 18:19]), reads=[rk], writes=[rk])
            op("dve", tt(wts_all[:, st * 2:st * 2 + 1], rt[:, j, 19:20], rt[:, j, 2:3], ALU.mult),
               reads=[rk], writes=["wts_all"])
            op("dve", tt(wts_all[:, st * 2 + 1:st * 2 + 2], wts_all[:, st * 2:st * 2 + 1], rt[:, j, 17:18], ALU.mult),
               reads=[rk, "wts_all"], writes=["wts_all"])
            op("dve", tt(Ot[:, j, :], oh[:, j, 0:32], oh[:, j, 32:64], ALU.add), reads=[rk], writes=[("Ot", j)])
            pp, kp = next_mm()
            op("pe", [mm(pp[:, 0:32], Umat, Ot[:, j, :], start=True, stop=False),
                      mm(pp[:, 0:32], ones_f, Ocum, start=False, stop=True)],
               reads=[("Ot", j), "Ocum", "cst", "ones_f"], writes=[kp])
            op("dve", tt(Ocum, Ocum, Ot[:, j, :], ALU.add), reads=[("Ot", j), "Ocum"], writes=["Ocum"])
            for k in range(2):
                o_k = oh[:, j, k * 32:(k + 1) * 32]
                op("dve", tt(tmp32[:, j, 0:32], o_k, pp[:, 0:32], ALU.mult), reads=[rk, kp], writes=[("tmp32", j)])
                op("dve", red(pos_all[:, st * 2 + k:st * 2 + k + 1], tmp32[:, j, 0:32], ALU.add),
                   reads=[("tmp32", j)], writes=["pos_all"])
                op("dve", tt(tmp32[:, j, 32:64], o_k, iota32, ALU.mult), reads=[rk, "cst"], writes=[("tmp32", j)])
                op("dve", red(eid_all[:, st * 2 + k:st * 2 + k + 1], tmp32[:, j, 32:64], ALU.add),
                   reads=[("tmp32", j)], writes=["eid_all"])
                op("dve", stt(rt[:, j, 20 + k:21 + k], eid_all[:, st * 2 + k:st * 2 + k + 1], float(CAP),
                              pos_all[:, st * 2 + k:st * 2 + k + 1], ALU.mult, ALU.add),
                   reads=["eid_all", "pos_all", rk], writes=[rk])
                op("dve", cp(slot_u[:, st * 2 + k:st * 2 + k + 1], rt[:, j, 20 + k:21 + k]), reads=[rk], writes=["slot_u"])
                op("pool", cp(pay_i[:, (st * 2 + k) * 2:(st * 2 + k) * 2 + 1], tokid[:, st:st + 1]),
                   reads=["tokid"], writes=[("pay", st, k)])
                op("pool", cp(pay_f[:, (st * 2 + k) * 2 + 1:(st * 2 + k) * 2 + 2], wts_all[:, st * 2 + k:st * 2 + k + 1]),
                   reads=["wts_all", ("pay", st, k)], writes=[("pay", st, k)])
                sl = slot_u[:, st * 2 + k:st * 2 + k + 1]
                py = pay_i[:, (st * 2 + k) * 2:(st * 2 + k) * 2 + 2]
                op("pool", lambda e, sl=sl, py=py: e.indirect_dma_start(
                    out=sidx_d[:, :], out_offset=bass.IndirectOffsetOnAxis(ap=sl, axis=0),
                    in_=py, in_offset=None), reads=["slot_u", ("pay", st, k)], writes=["sidx_scr"], dma=True)

    if stop_after in ("mixer", "route"):
        if dbg and stop_after == "route":
            op("sp", dma(dbg2_d[:, 0:NST * 2], pos_all), reads=["pos_all"], writes=["dbg2"], dma=True)
            op("sp", dma(dbg2_d[:, 1024:1024 + NST * 2], eid_all), reads=["eid_all"], writes=["dbg2"], dma=True)
            op("sp", dma(dbg2_d[:, 2048:2048 + NST * 2], wts_all), reads=["wts_all"], writes=["dbg2"], dma=True)
        sc.emit()
        return nc

    sc.barrier()
    ar.reset(m0)
    thr = cst[:, CS_TH:CS_TH + NTH]
    cnt = ar.f32(32)
    big_full = ar.f32(max(NST * 2 * 32, 32 * NTH))
    big = big_full[:, 0:NST * 2 * 32]
    nblk = ar.f32(32)
    padded = ar.f32(32)
    pends = ar.f32(32)
    ebase = ar.f32(32)
    bt = ar.f32(64)
    slc_f = ar.f32(NST * 2)
    op("pe", mm(ps[2][:, 0:32], ones_f, Ocum), reads=["Ocum", "ones_f"], writes=["ps2"])
    op("dve", cp(cnt, ps[2][:, 0:32]), reads=["ps2"], writes=["cnt"])
    op("dve", tt(big_full[:, 0:32 * NTH].rearrange("p (e m) -> p e m", m=NTH),
                 cnt.unsqueeze(2).to_broadcast([128, 32, NTH]),
                 thr.unsqueeze(1).to_broadcast([128, 32, NTH]), ALU.is_gt),
       reads=["cnt", "cst"], writes=["big"])
    op("dve", red(nblk, big_full[:, 0:32 * NTH].rearrange("p (e m) -> p e m", m=NTH), ALU.add),
       reads=["big"], writes=["nblk"])
    op("dve", ts(padded, nblk, float(BLK), ALU.mult), reads=["nblk"], writes=["padded"])
    op("dve", scan(pends, ones_f[:, 0:32], padded, 0.0, ALU.mult, ALU.add), reads=["padded", "ones_f"], writes=["pends"])
    op("dve", tt(ebase, pends, padded, ALU.subtract), reads=["pends", "padded"], writes=["ebase"])
    op("dve", ts(bt[:, 0:32], pends, blk512, ALU.is_le), reads=["pends", "cst"], writes=["bt"])
    op("dve", red(bt[:, 32:33], bt[:, 0:32], ALU.add), reads=["bt"], writes=["bt"])
    op("dve", ts(bt[:, 32:33], bt[:, 32:33], float(NE - 1), ALU.min), reads=["bt"], writes=["bt"])
    op("dve", ts(bt[:, 0:32], iota32, bt[:, 32:33], ALU.is_equal), reads=["bt", "cst"], writes=["bt"])
    op("dve", tt(bt[:, 0:32], bt[:, 0:32], ebase, ALU.mult), reads=["bt", "ebase"], writes=["bt"])
    op("dve", red(bt[:, 33:34], bt[:, 0:32], ALU.add), reads=["bt"], writes=["bt"])
    op("dve", ts(bt[:, 34:35], blk512, pends[:, 31:32], ALU.is_lt), reads=["pends", "cst"], writes=["bt"])
    op("dve", stt(bt[:, 35:36], bt[:, 32:33], float(CAP), blk512, ALU.mult, ALU.add), reads=["bt", "cst"], writes=["bt"])
    op("dve", tt(bt[:, 35:36], bt[:, 35:36], bt[:, 33:34], ALU.subtract), reads=["bt"], writes=["bt"])
    op("dve", ts(bt[:, 35:36], bt[:, 35:36], float(-NULLSTART), ALU.add), reads=["bt"], writes=["bt"])
    op("dve", tt(bt[:, 35:36], bt[:, 35:36], bt[:, 34:35], ALU.mult), reads=["bt"], writes=["bt"])
    op("dve", ts(bt[:, 35:36], bt[:, 35:36], float(NULLSTART), ALU.add), reads=["bt"], writes=["bt"])
    Gb = ar.f32(256).rearrange("p (a m) -> p a m", a=2)
    bcf = ar.f32(256).rearrange("p (a m) -> p a m", a=2)
    pcol = ar.f32(2)
    op("dve", ts(Gb[:, 0, :], ones_f, bt[:, 32:33], ALU.mult), reads=["bt", "ones_f"], writes=["Gb"])
    op("dve", ts(Gb[:, 1, :], ones_f, bt[:, 35:36], ALU.mult), reads=["bt", "ones_f"], writes=["Gb"])
    op("pe", [mm(ps[3][:, 0:128], Gb[:, 0, :], ident_f), mm(ps[3][:, 128:256], Gb[:, 1, :], ident_f)],
       reads=["Gb", "cst"], writes=["ps3"])
    op("dve", cp(bcf.rearrange("p a m -> p (a m)"), ps[3][:, 0:256]), reads=["ps3"], writes=["bcf"])
    op("dve", ts(pcol[:, 0:1], blk512, 1.0 / BLK, ALU.mult), reads=["cst"], writes=["pcol"])
    op("dve", ts(bcf[:, 0, :], bcf[:, 0, :], 128.0, ALU.mult, pcol[:, 0:1], ALU.add), reads=["bcf", "pcol"], writes=["bcf"])
    op("dve", cp(widx, bcf[:, 0, :]), reads=["bcf"], writes=["widx"])
    op("dve", ts(bcf[:, 1, :], bcf[:, 1, :], pcol[:, 0:1], ALU.add), reads=["bcf", "pcol"], writes=["bcf"])
    for j in range(4):
        op("dve", ts(Gb[:, 0, :], bcf[:, 1, :], float(j * 128), ALU.add), reads=["bcf", "Gb"], writes=["Gb"])
        op("dve", cp(sidx4[:, j * 128:(j + 1) * 128], Gb[:, 0, :]), reads=["Gb"], writes=["sidx4"])
    op("dve", tt(big.rearrange("p (a e) -> p a e", e=32), ohs.rearrange("p (a e) -> p a e", e=32),
                 ebase.unsqueeze(1).to_broadcast([128, NST * 2, 32]), ALU.mult),
       reads=["ohs", "ebase", "big"], writes=["big"])
    op("dve", red(slc_f, big.rearrange("p (a e) -> p a e", e=32), ALU.add), reads=["big"], writes=["slc_f"])
    op("dve", tt(slc_f, slc_f, pos_all, ALU.add), reads=["slc_f", "pos_all"], writes=["slc_f"])
    op("dve", cp(slot_c, slc_f), reads=["slc_f"], writes=["slot_c"])
    if dbg and stop_after == "tables":
        op("sp", dma(dbg2_d[:, 0:128], widx.bitcast(F32)), reads=["widx"], writes=["dbg2"], dma=True)
        op("sp", dma(dbg2_d[:, 512:1024], sidx4.bitcast(F32)), reads=["sidx4"], writes=["dbg2"], dma=True)
        op("sp", dma(dbg2_d[:, 1024:1024 + NST * 2], slot_c.bitcast(F32)), reads=["slot_c"], writes=["dbg2"], dma=True)
        op("sp", dma(dbg2_d[:, 256:288], pends), reads=["pends"], writes=["dbg2"], dma=True)
        sc.emit()
        return nc

    m3 = ar.mark()
    NSTG = 3
    stg3 = [ar.f32(4096) for _ in range(NSTG)]
    wg_bf = [ar.bf(8 * 512).rearrange("p (k f) -> p k f", k=8) for _ in range(2)]
    wu_bf = [ar.bf(8 * 512).rearrange("p (k f) -> p k f", k=8) for _ in range(2)]
    wd_bf = [ar.bf(4 * 1024).rearrange("p (k f) -> p k f", k=4) for _ in range(2)]
    Xg = [ar.bf(4 * D).rearrange("p (j d) -> p j d", j=4) for _ in range(2)]
    h2T = ar.bf(8 * BLK).rearrange("p (k t) -> p k t", k=8)
    hidT = ar.bf(4 * BLK).rearrange("p (k t) -> p k t", k=4)
    sgb = [ar.f32(BLK) for _ in range(2)]
    ysb = [ar.f32(D) for _ in range(2)]
    print("arena used phase3 (words):", ar.off, "of", ARW)
    stg_i = [0]
    cast_i = [0]
    wgv = wg_d.rearrange("e (p kk) f -> (e p) (kk f)", kk=8)
    wuv = wu_d.rearrange("e (p kk) f -> (e p) (kk f)", kk=8)
    wdv = wd_d.rearrange("e (p kk) f -> (e p) (kk f)", kk=4)

    def issue_loads(b):
        q = b % 2
        for j in range(4):
            op("pool", lambda e, j=j, q=q, b=b: e.indirect_dma_start(
                out=idx_sb[q][:, 2 * j:2 * j + 2], out_offset=None, in_=sidx_d[:, :],
                in_offset=bass.IndirectOffsetOnAxis(ap=sidx4[:, j * 128 + b:j * 128 + b + 1], axis=0)),
               reads=["sidx4", "sidx_scr"], writes=[("idx", q)], dma=True)
        for j in range(4):
            op("pool", lambda e, j=j, q=q: e.indirect_dma_start(
                out=Xg[q][:, j, :], out_offset=None, in_=xn2_d[:, :],
                in_offset=bass.IndirectOffsetOnAxis(ap=idx_sb[q][:, 2 * j:2 * j + 1], axis=0)),
               reads=[("idx", q), "xn2_scr"], writes=[("Xg", q, j)], dma=True)
        for (wv, dst, nm) in ((wgv, wg_bf[q], "wg"), (wuv, wu_bf[q], "wu"), (wdv, wd_bf[q], "wd")):
            sgi = stg_i[0] % NSTG
            stg_i[0] += 1
            op("pool", lambda e, wv=wv, sgi=sgi, b=b: e.indirect_dma_start(
                out=stg3[sgi], out_offset=None, in_=wv,
                in_offset=bass.IndirectOffsetOnAxis(ap=widx[:, b:b + 1], axis=0)),
               reads=["widx"], writes=[("stg3", sgi)], dma=True)
            dflat = dst.rearrange("p k f -> p (k f)")
            for hf in range(2):
                ce = ("act", "pool", "dve")[cast_i[0] % 3]
                cast_i[0] += 1
                sl = slice(hf * 2048, (hf + 1) * 2048)
                if nm != "wd":
                    op(ce, (acp if ce == "act" else cp)(dflat[:, sl], stg3[sgi][:, sl]),
                       reads=[("stg3", sgi)], writes=[(nm, q, hf)])
                else:
                    ce = "dve" if ce == "act" else ce
                    op(ce, tt(dflat[:, sl].rearrange("p (k f) -> p k f", k=2),
                              stg3[sgi][:, sl].rearrange("p (k f) -> p k f", k=2),
                              gt2_bc.unsqueeze(1).to_broadcast([128, 2, D]), ALU.mult),
                       reads=[("stg3", sgi), "gt2_bc"], writes=[(nm, q, hf)])

    issue_loads(0)
    for b in range(NB):
        q = b % 2
        if b + 1 < NB:
            issue_loads(b + 1)
        for r4 in range(4):
            hb = r4 % 2
            fns = []
            for u in range(2):
                kc = r4 * 2 + u
                for j in range(4):
                    fns.append(tr(psTb[hb][:, u * BLK + j * 128:u * BLK + (j + 1) * 128],
                                  Xg[q][:, j, :].rearrange("p (m kk) -> p kk m", kk=8)[:, kc, :], ident_bf))
            op("pe", fns, reads=[("Xg", q, j) for j in range(4)] + ["ident_bf"], writes=[f"ps{hb}"])
            for u in range(2):
                kc = r4 * 2 + u
                op("act", act(h2T[:, kc, :], psTb[hb][:, u * BLK:(u + 1) * BLK], AF.Identity,
                              bias=bias2p[:, kc:kc + 1], scale=scale2p[:, kc:kc + 1]),
                   reads=[f"ps{hb}", "scale2p", "modP"], writes=[("h2T", kc)])
        h2_keys = [("h2T", kc) for kc in range(8)]
        for hc in range(4):
            pg, kg = next_mm()
            op("pe", [mm(pg, wg_bf[q][:, kc, :].rearrange("p (m c) -> p c m", c=4)[:, hc, :], h2T[:, kc, :], start=(kc == 0), stop=(kc == 7))
                      for kc in range(8)], reads=h2_keys + [("wg", q, 0), ("wg", q, 1)], writes=[kg])
            pu, ku = next_mm()
            op("pe", [mm(pu, wu_bf[q][:, kc, :].rearrange("p (m c) -> p c m", c=4)[:, hc, :], h2T[:, kc, :], start=(kc == 0), stop=(kc == 7))
                      for kc in range(8)], reads=h2_keys + [("wu", q, 0), ("wu", q, 1)], writes=[ku])
            sgk = ("sgb", hc % 2)
            op("act", act(sgb[hc % 2], pg, AF.Sigmoid), reads=[kg], writes=[sgk])
            op("dve", tt(sgb[hc % 2], sgb[hc % 2], pg, ALU.mult), reads=[kg, sgk], writes=[sgk])
            op("dve", tt(hidT[:, hc, :], sgb[hc % 2], pu, ALU.mult), reads=[ku, sgk], writes=[("hidT", hc)])
        hid_keys = [("hidT", hc) for hc in range(4)]
        for j in range(4):
            yq = (b * 4 + j) % 2
            for half in range(2):
                pd, kd_ = next_mm()
                op("pe", [mm(pd, hidT[:, hc, j * 128:(j + 1) * 128], wd_bf[q][:, hc, half * 512:(half + 1) * 512],
                             start=(hc == 0), stop=(hc == 3)) for hc in range(4)],
                   reads=hid_keys + [("wd", q, 0), ("wd", q, 1)], writes=[kd_])
                wtok = idx_sb[q].bitcast(F32)[:, 2 * j + 1:2 * j + 2]
                op("act", act(ysb[yq][:, half * 512:(half + 1) * 512], pd, AF.Identity, scale=wtok),
                   reads=[kd_, ("idx", q)], writes=[("ysb", yq)])
            op("sp", dma(ybuf_d[b * BLK + j * 128:b * BLK + (j + 1) * 128, :], ysb[yq]),
               reads=[("ysb", yq)], writes=["ybuf"], dma=True)

    sc.barrier()
    ar.reset(m3)
    F = [dict(xm=ar.f32(D), y0=ar.f32(D), y1=ar.f32(D), jk=ar.bf(D), st=ar.f32(4)) for _ in range(2)]
    for st in range(NST):
        f = F[st % 2]
        fk = ("F", st % 2)
        op("sp", dma(f["xm"], xmid_d[st * 128:(st + 1) * 128, :]), reads=["xmid_scr"], writes=[fk + ("xm",)], dma=True)
        for k in range(2):
            op("pool", lambda e, f=f, k=k, st=st: e.indirect_dma_start(
                out=f["y%d" % k], out_offset=None, in_=ybuf_d[:, :],
                in_offset=bass.IndirectOffsetOnAxis(ap=slot_c[:, st * 2 + k:st * 2 + k + 1], axis=0)),
               reads=["ybuf", "slot_c"], writes=[fk + ("y%d" % k,)], dma=True)
        op("pool", tt(f["y0"], f["y0"], f["y1"], ALU.add), reads=[fk + ("y0",), fk + ("y1",)], writes=[fk + ("y0",)])
        op("dve", tt(f["xm"], f["xm"], f["y0"], ALU.add), reads=[fk + ("xm",), fk + ("y0",)], writes=[fk + ("xm",)])
        op("act", act(f["jk"], f["xm"], AF.Square, accum_out=f["st"][:, 0:1]), reads=[fk + ("xm",)], writes=[fk + ("st",), fk + ("jk",)])
        op("act", act(f["st"][:, 1:2], f["st"][:, 0:1], AF.Sqrt, bias=EPS, scale=1.0 / D), reads=[fk + ("st",)], writes=[fk + ("st",)])
        op("dve", lambda e, f=f: e.reciprocal(out=f["st"][:, 2:3], in_=f["st"][:, 1:2]), reads=[fk + ("st",)], writes=[fk + ("st",)])
        op("dve", stt(f["y1"], f["xm"], f["st"][:, 2:3], gf_bc, ALU.mult, ALU.mult),
           reads=[fk + ("xm",), fk + ("st",), "gf_bc"], writes=[fk + ("y1",)])
        op("sp", dma(out_d[st * 128:(st + 1) * 128, :], f["y1"]), reads=[fk + ("y1",)], writes=["out"], dma=True)

    sc.emit()
    return nc


def host_consts(S):
    NST = S // 128
    cst = np.zeros((128, CS_N), np.float32)
    cst[:, CS_ID:CS_ID + 128] = np.eye(128, dtype=np.float32)
    p = np.arange(128)
    cst[:, CS_U:CS_U + 128] = (p[:, None] < p[None, :]).astype(np.float32)
    cst[:, CS_CM:CS_CM + 128] = (p[:, None] <= p[None, :]).astype(np.float32)
    cst[:, CS_HM] = (p < 64).astype(np.float32)
    cst[:, CS_HM + 1] = (p >= 64).astype(np.float32)
    cst[:, CS_TH:CS_TH + NTH] = (np.arange(NTH) * BLK).astype(np.float32)[None, :]
    rm = np.ones((512,), np.float32)
    rm[::128] = 0.0
    cst[:, CS_RM:CS_RM + 512] = rm[None, :]
    cst[:, CS_IO:CS_IO + 32] = np.arange(32, dtype=np.float32)[None, :]
    cst[:, CS_B5] = (p * BLK).astype(np.float32)
    tokid = (np.arange(NST)[None, :] * 128 + p[:, None]).astype(np.int32)
    return cst, tokid


def fm(v, n):
    return np.ascontiguousarray(np.asarray(v, np.float32).reshape(n, 128).T)


def host_inputs(inp, b, S):
    L = 0
    tab = np.zeros((128, TB_N), np.float32)
    tab[:, TB_BADA:TB_BADA + 48] = fm(inp["b_ada"][L], 48)
    tab[:, TB_GMIX:TB_GMIX + 8] = fm(inp["g_mix"][L], 8)
    tab[:, TB_GFFN:TB_GFFN + 8] = fm(inp["g_ffn"][L], 8)
    cw = np.asarray(inp["conv_w"][L], np.float32)
    for c in range(4):
        tab[:, TB_CW + c * 4:TB_CW + c * 4 + 4] = cw[:, c * 128:(c + 1) * 128].T
    tab[:, TB_CB:TB_CB + 4] = fm(inp["conv_b"][L], 4)
    tab[:, TB_BR:TB_BR + 4] = fm(inp["lru_br"][L], 4)
    tab[:, TB_BI:TB_BI + 4] = fm(inp["lru_bi"][L], 4)
    tab[:, TB_LAM:TB_LAM + 4] = fm(inp["lru_lambda"][L], 4)
    tab[:, TB_BA:TB_BA + 2] = fm(inp["gla_ba"][L], 2)
    tab[:, TB_GN] = np.asarray(inp["gla_gnorm"][L], np.float32)
    ba = np.asarray(inp["b_ada"][L], np.float32)
    tab[:, TB_BADAP:TB_BADAP + 8] = ba[3 * D:4 * D].reshape(128, 8)
    tab[:, TB_BADAP + 8:TB_BADAP + 16] = ba[4 * D:5 * D].reshape(128, 8)
    tab[:, TB_GFFNP:TB_GFFNP + 8] = np.asarray(inp["g_ffn"][L], np.float32).reshape(128, 8)

    def bd(w):
        w = np.asarray(w, np.float32)
        o = np.zeros((128, 4, 128), np.float32)
        for c in range(4):
            for hh in range(2):
                o[hh * 64:(hh + 1) * 64, c, hh * 64:(hh + 1) * 64] = w[2 * c + hh]
        return o.reshape(128, 512)

    cst, tokid = host_consts(S)
    m = {
        "x": np.ascontiguousarray(np.asarray(inp["x"][b], np.float32)),
        "cT": fm(inp["c"][b], 8),
        "w_ada": np.ascontiguousarray(np.asarray(inp["w_ada"][L], np.float32)),
        "tab": tab,
        "cst": cst,
        "tokid": tokid,
        "gf_bc": np.ascontiguousarray(np.broadcast_to(np.asarray(inp["g_final"], np.float32)[None, :], (128, D))),
        "br_bc": np.ascontiguousarray(np.broadcast_to(
            np.concatenate([np.asarray(inp["b_coarse"][L], np.float32),
                            np.asarray(inp["b_fine"][L], np.float32)])[None, :], (128, 36))),
        "w_in": np.ascontiguousarray(np.asarray(inp["w_in"][L], np.float32)),
        "w_out": np.ascontiguousarray(np.asarray(inp["w_out"][L], np.float32)),
        "bd_r": bd(inp["lru_wr"][L]),
        "bd_i": bd(inp["lru_wi"][L]),
        "wa2": np.ascontiguousarray(np.asarray(inp["gla_wa2"][L], np.float32)),
        "w_r": np.ascontiguousarray(np.concatenate([np.asarray(inp["w_coarse"][L], np.float32),
                                                    np.asarray(inp["w_fine"][L], np.float32)], axis=1)),
        "w_gate": np.ascontiguousarray(np.asarray(inp["w_gate"][L], np.float32)),
        "w_up": np.ascontiguousarray(np.asarray(inp["w_up"][L], np.float32)),
        "w_down": np.ascontiguousarray(np.asarray(inp["w_down"][L], np.float32)),
    }
    return m


def kernel(**inputs):
    B, S = inputs["x"].shape[0], inputs["x"].shape[1]
    nc = build(S)
    in_maps = [host_inputs(inputs, b, S) for b in range(B)]
    res = run_bass_kernel_spmd(nc, in_maps, core_ids=list(range(B)))
    return np.stack([np.asarray(r["out"]) for r in res.results], axis=0).astype(np.float32)
```

```python
import numpy as np
import concourse.bass as bass
import concourse.mybir as mybir
from concourse.bass_utils import run_bass_kernel_spmd

F32 = mybir.dt.float32
BF16 = mybir.dt.bfloat16
I32 = mybir.dt.int32
U32 = mybir.dt.uint32
AF = mybir.ActivationFunctionType
ALU = mybir.AluOpType
AX = mybir.AxisListType

D = 1024
NPROJ = 2576
NE = 32
CAP = 8192 + 512
EPS = 1e-6
T = 256
BLK = 512


class Sched:
    EPOCH = 6000

    def __init__(self, nc, same_engine_sync=True):
        self.nc = nc
        self.ops = []
        self.lw = {}
        self.rd = {}
        self.same = same_engine_sync
        self.stack = []

    def rec_begin(self):
        self.stack.append([])

    def rec_end(self):
        return self.stack.pop()

    def play(self, lst):
        for a in lst:
            self.op(*a)

    @staticmethod
    def merge(lists):
        lists = [l for l in lists if l]
        out = []
        n = [len(l) for l in lists]
        pos = [0] * len(lists)
        total = sum(n)
        for _ in range(total):
            bi, bv = -1, 2.0
            for i in range(len(lists)):
                if pos[i] < n[i]:
                    v = pos[i] / n[i]
                    if v < bv:
                        bi, bv = i, v
            out.append(lists[bi][pos[bi]])
            pos[bi] += 1
        return out

    def op(self, eng, fns, reads=(), writes=(), dma=False):
        if self.stack:
            self.stack[-1].append((eng, fns, list(reads), list(writes), dma))
            return
        if callable(fns):
            fns = [fns]
        reads, writes = list(reads), list(writes)
        for k in reads:
            if isinstance(k, str) and k.startswith("ps") and k[2:].isdigit() and k not in writes:
                writes.append(k)
        idx = len(self.ops)
        deps = set()
        for k in reads:
            if k in self.lw:
                deps.add(self.lw[k])
        for k in writes:
            if k in self.lw:
                deps.add(self.lw[k])
            r = self.rd.get(k)
            if r:
                deps.update(r[0].values())
                deps.update(r[1])
        self.ops.append(dict(eng=eng, fns=fns, deps=deps, dma=dma, tag=(list(reads), list(writes))))
        for k in writes:
            self.lw[k] = idx
            self.rd[k] = ({}, [])
        for k in reads:
            r = self.rd.setdefault(k, ({}, []))
            if dma:
                r[1].append(idx)
            else:
                r[0][eng] = idx
        return idx

    def barrier(self):
        last = {}
        dmas = []
        for i, o in enumerate(self.ops):
            if o["dma"]:
                dmas.append(i)
            else:
                last[o["eng"]] = i
        deps = set(last.values()) | set(dmas)
        for eng in ("pe", "act", "dve", "pool", "sp"):
            idx = len(self.ops)
            self.ops.append(dict(eng=eng, fns=[lambda e: e.nop()], deps=set(deps), dma=False))
            last[eng] = idx
        self._pending_dma_done = True
        self.lw = {}
        self.rd = {}
        self._bar = dict(last)
        for eng, i in last.items():
            self.lw[("__bar__", eng)] = i

    def emit(self):
        import os
        lim = int(os.environ.get("OPLIMIT", "0"))
        print("total ops", len(self.ops))
        if lim:
            self.ops = self.ops[:lim]
            o = self.ops[-1]
            print("last op", o["eng"], o.get("tag"))
        nc = self.nc
        engs = ("pe", "act", "dve", "pool", "sp")
        count = {e: 0 for e in engs}
        prog = {e: [] for e in engs}
        dma_pool = {}
        dma_rr = {e: 0 for e in engs}
        dma_val = {}
        NPOOL = {"sp": 10, "pool": 8, "act": 4, "dve": 2, "pe": 2}
        tokens = []
        pre_wait = []
        for o in self.ops:
            e = o["eng"]
            if o["dma"]:
                pl = dma_pool.setdefault(e, [])
                if len(pl) < NPOOL[e]:
                    s = nc.alloc_semaphore(name=f"dq_{e}_{len(pl)}")
                    pl.append(s)
                    dma_val[id(s)] = 0
                s = pl[dma_rr[e] % len(pl)] if len(pl) == NPOOL[e] else pl[-1]
                dma_rr[e] += 1
                prev = dma_val[id(s)]
                dma_val[id(s)] = prev + 16
                tokens.append((s, prev + 16))
                pre_wait.append((s, prev) if prev > 0 else None)
            else:
                c = count[e]
                ep = c // self.EPOCH
                while len(prog[e]) <= ep:
                    prog[e].append(nc.alloc_semaphore(name=f"pg_{e}_{len(prog[e])}"))
                tokens.append((prog[e][ep], c % self.EPOCH + 1))
                pre_wait.append(None)
                count[e] = c + 1
        waited = {e: {} for e in engs}
        streams = {e: [] for e in engs}
        for i, o in enumerate(self.ops):
            e = o["eng"]
            ws = []
            cand = []
            if pre_wait[i] is not None:
                cand.append(pre_wait[i])
            for d in sorted(o["deps"]):
                od = self.ops[d]
                if (not od["dma"]) and od["eng"] == e and (e == "pe" or not self.same):
                    continue
                cand.append(tokens[d])
            for (s, v) in cand:
                w = waited[e]
                if w.get(id(s), 0) >= v:
                    continue
                w[id(s)] = v
                ws.append((s, v))
            streams[e].append((ws, o["fns"], tokens[i], o["dma"]))
        final_waits = []
        for e, pl in dma_pool.items():
            for s in pl:
                if dma_val[id(s)] > 0:
                    final_waits.append((s, dma_val[id(s)]))
        for e in engs:
            if count[e] > 0:
                c = count[e] - 1
                final_waits.append((prog[e][c // self.EPOCH], c % self.EPOCH + 1))

        def run(engine, name):
            for ws, fns, tok, is_dma in streams[name]:
                for (s, v) in ws:
                    engine.wait_ge(s, v)
                ins = None
                for f in fns:
                    ins = f(engine)
                ins.then_inc(tok[0], 16 if is_dma else 1)

        with nc.Block() as block:
            @block.tensor
            def _(eng):
                run(eng, "pe")

            @block.scalar
            def _(eng):
                run(eng, "act")

            @block.vector
            def _(eng):
                run(eng, "dve")

            @block.gpsimd
            def _(eng):
                run(eng, "pool")

            @block.sync
            def _(eng):
                run(eng, "sp")
                for (s, v) in final_waits:
                    eng.wait_ge(s, v)


def act(out, in_, func, bias=None, scale=None, accum_out=None):
    def f(e):
        kw = {}
        if bias is not None:
            kw["bias"] = bias
        if scale is not None:
            kw["scale"] = scale
        if accum_out is not None:
            kw["accum_out"] = accum_out
        return e.activation(out=out, in_=in_, func=func, **kw)
    return f


def ts(out, in0, s1, op0, s2=None, op1=None, accum_out=None):
    def f(e):
        kw = {}
        if op1 is not None:
            kw["op1"] = op1
        if accum_out is not None:
            kw["accum_out"] = accum_out
        return e.tensor_scalar(out=out, in0=in0, scalar1=s1, scalar2=s2, op0=op0, **kw)
    return f


def tt(out, in0, in1, op):
    return lambda e: e.tensor_tensor(out=out, in0=in0, in1=in1, op=op)


def stt(out, in0, scalar, in1, op0, op1):
    return lambda e: e.scalar_tensor_tensor(out=out, in0=in0, scalar=scalar, in1=in1, op0=op0, op1=op1)


def mm(out, lhsT, rhs, start=True, stop=True):
    return lambda e: e.matmul(out, lhsT, rhs, start=start, stop=stop)


def tr(out, in_, ident):
    return lambda e: e.transpose(out, in_, ident)


def dma(out, in_, **kw):
    return lambda e: e.dma_start(out=out, in_=in_, **kw)


def cp(out, in_):
    return lambda e: e.tensor_copy(out=out, in_=in_)


def acp(out, in_):
    return lambda e: e.activation(out=out, in_=in_, func=AF.Copy)


def scan(out, d0, d1, init, op0, op1):
    return lambda e: e.tensor_tensor_scan(out=out, data0=d0, data1=d1, initial=init, op0=op0, op1=op1)


def red(out, in_, op):
    return lambda e: e.tensor_reduce(out=out, in_=in_, axis=AX.X, op=op)


def mset(ap, v):
    return lambda e: e.memset(ap, v)


TB_BADA = 0
TB_GMIX = 48
TB_GFFN = 56
TB_CW = 64
TB_CB = 80
TB_BR = 84
TB_BI = 88
TB_LAM = 92
TB_BA = 96
TB_GN = 98
TB_BADAP = 99
TB_GFFNP = 115
TB_N = 123

CS_ID = 0
CS_U = 128
CS_CM = 256
CS_RM = 384
CS_IO = 896
CS_B5 = 928
CS_HM = 929
CS_TH = 931
NTH = 17
CS_N = 948


class Arena:
    def __init__(self, ap, nwords):
        self.ap = ap
        self.n = nwords
        self.off = 0

    def f32(self, n):
        n = (n + 1) // 2 * 2
        assert self.off + n <= self.n, ("arena overflow", self.off, n, self.n)
        v = self.ap[:, self.off:self.off + n]
        self.off += n
        return v

    def bf(self, nel):
        w = (nel + 1) // 2
        w = (w + 1) // 2 * 2
        v = self.f32(w).bitcast(BF16)
        return v[:, 0:nel]

    def mark(self):
        return self.off

    def reset(self, m):
        self.off = m


def build(S, stop_after=None, dbg=False):
    NT = S // T
    NST = S // 128
    NB = (2 * S) // BLK + NE
    NROWS = NE * CAP + BLK
    NULLSTART = NE * CAP
    nc = bass.Bass("TRN2", target_bir_lowering=False)

    def din(name, shape, dt=F32):
        return nc.dram_tensor(name, shape, dt, kind="ExternalInput").ap()

    x_d = din("x", [S, D])
    cT_d = din("cT", [128, 8])
    wada_d = din("w_ada", [D, 6 * D])
    tab_d = din("tab", [128, TB_N])
    cst_d = din("cst", [128, CS_N])
    tok_d = din("tokid", [128, NST], I32)
    gf_d = din("gf_bc", [128, D])
    brb_d = din("br_bc", [128, 36])
    win_d = din("w_in", [D, NPROJ])
    wout_d = din("w_out", [D, D])
    bdr_d = din("bd_r", [128, 512])
    bdi_d = din("bd_i", [128, 512])
    wa2_d = din("wa2", [16, 256])
    wr_d = din("w_r", [D, 36])
    wg_d = din("w_gate", [NE, D, 512])
    wu_d = din("w_up", [NE, D, 512])
    wd_d = din("w_down", [NE, 512, D])
    out_d = nc.dram_tensor("out", [S, D], F32, kind="ExternalOutput").ap()

    xmid_d = nc.dram_tensor("xmid_scr", [S, D], F32, kind="Internal").ap()
    xn2_d = nc.dram_tensor("xn2_scr", [S, D], BF16, kind="Internal").ap()
    sidx_d = nc.dram_tensor("sidx_scr", [NROWS, 2], I32, kind="Internal").ap()
    ybuf_d = nc.dram_tensor("ybuf_scr", [NB * BLK, D], F32, kind="Internal").ap()
    dbg_d = None
    if dbg:
        dbg_d = nc.dram_tensor("dbg", [S, D], F32, kind="ExternalOutput").ap()
        dbg2_d = nc.dram_tensor("dbg2", [128, 4096], F32, kind="ExternalOutput").ap()

    sc = Sched(nc)
    op = sc.op

    def sb(name, shape, dt=F32):
        return nc.alloc_sbuf_tensor(name + "_sb", shape, dt)[:]

    tab = sb("tab", [128, TB_N])
    cst = sb("cst", [128, CS_N])
    tokid = sb("tokid", [128, NST], I32)
    gf_bc = sb("gf_bc", [128, D])
    gt1_bc = sb("gt1_bc", [128, D])
    gt2_bc = sb("gt2_bc", [128, D])
    brb = sb("brb", [128, 36])
    modT = sb("modT", [128, 48])
    scale1 = sb("scale1", [128, 8])
    scale2 = sb("scale2", [128, 8])
    scale2p = sb("scale2p", [128, 8])
    modP = sb("modP", [128, 16])
    widx = sb("widx", [128, 128], I32)
    sidx4 = sb("sidx4", [128, 4 * 128], I32)
    ltab = sb("ltab", [128, 16])
    ident_bf = sb("ident_bf", [128, 128], BF16)
    ones_bf = sb("ones_bf", [128, 128], BF16)
    ones_f = sb("ones_f", [128, 128])
    ohs = sb("ohs", [128, NST * 2 * 32], BF16)
    pos_all = sb("pos_all", [128, NST * 2])
    eid_all = sb("eid_all", [128, NST * 2])
    wts_all = sb("wts_all", [128, NST * 2])
    Ocum = sb("Ocum", [128, 32])
    pay_i = sb("pay", [128, NST * 4], I32)
    pay_f = pay_i.bitcast(F32)
    slot_u = sb("slot_u", [128, NST * 2], I32)
    slot_c = sb("slot_c", [128, NST * 2], I32)
    tbl_i = sb("tbl_i", [1, 256], I32)
    idx_sb = [sb(f"idx_sb{q}", [128, 8], I32) for q in range(2)]
    ident_f = cst[:, CS_ID:CS_ID + 128]
    Umat = cst[:, CS_U:CS_U + 128]
    cmask = cst[:, CS_CM:CS_CM + 128]
    hmask = cst[:, CS_HM:CS_HM + 2]
    rmask = cst[:, CS_RM:CS_RM + 512]
    iota32 = cst[:, CS_IO:CS_IO + 32]
    blk512 = cst[:, CS_B5:CS_B5 + 1]

    rem = nc.sbuf_bytes_remaining
    rem = rem() if callable(rem) else rem
    ARW = (int(rem) // 4) - 64
    ARW = ARW // 2 * 2
    arena_ap = sb("arena", [128, ARW])
    ar = Arena(arena_ap, ARW)

    ps = [nc.alloc_psum_tensor(f"ps{i}", [128, 512], F32)[:] for i in range(8)]

    m0 = ar.mark()
    op("sp", dma(tab, tab_d), writes=["tab"], dma=True)
    op("sp", dma(cst, cst_d), writes=["cst"], dma=True)
    cT = ar.f32(8)
    op("sp", dma(cT, cT_d), writes=["cT"], dma=True)
    op("sp", dma(tokid, tok_d), writes=["tokid"], dma=True)
    op("sp", dma(gf_bc, gf_d), writes=["gf_bc"], dma=True)
    op("sp", dma(brb, brb_d), writes=["brb"], dma=True)
    sgc = ar.f32(8)
    scT = ar.f32(8)
    op("act", act(sgc, cT, AF.Sigmoid), reads=["cT"], writes=["sgc"])
    op("dve", tt(scT, cT, sgc, ALU.mult), reads=["cT", "sgc"], writes=["scT"])
    op("dve", mset(ones_f, 1.0), writes=["ones_f"])
    op("dve", mset(ones_bf, 1.0), writes=["ones_bf"])
    op("dve", cp(ident_bf, ident_f), reads=["cst"], writes=["ident_bf"])

    zt = ar.f32(2048)
    op("pool", mset(zt, 0.0), writes=["zt"])
    zt_i = zt.bitcast(I32)
    rows_per = 128 * 1024
    r0 = 0
    while r0 < NROWS:
        n = min(rows_per, NROWS - r0)
        np_ = n // 1024
        if np_ > 0:
            op("sp", dma(sidx_d[r0:r0 + np_ * 1024, :].rearrange("(p r) c -> p (r c)", p=np_),
                         zt_i[0:np_, :]), reads=["zt"], writes=["sidx_scr"], dma=True)
            r0 += np_ * 1024
        else:
            op("sp", dma(sidx_d[r0:r0 + n, :].rearrange("(p r) c -> p (r c)", p=1),
                         zt_i[0:1, 0:2 * n]), reads=["zt"], writes=["sidx_scr"], dma=True)
            r0 += n

    wst = [ar.f32(8 * 1024).rearrange("p (k f) -> p k f", k=8) for _ in range(2)]
    for blk in range(6):
        b = blk % 2
        op("sp", dma(wst[b], wada_d[:, blk * 1024:(blk + 1) * 1024].rearrange("(k p) f -> p k f", p=128)),
           writes=[("wst", b)], dma=True)
        fns = []
        for fj in range(8):
            for kc in range(8):
                fns.append(mm(ps[0][:, blk * 8 + fj: blk * 8 + fj + 1],
                              wst[b][:, kc, fj * 128:(fj + 1) * 128], scT[:, kc:kc + 1],
                              start=(kc == 0), stop=(kc == 7)))
        if blk in (3, 4):
            for kk in range(8):
                for kc in range(8):
                    fns.append(mm(ps[0][:, 48 + (blk - 3) * 8 + kk: 48 + (blk - 3) * 8 + kk + 1],
                                  wst[b][:, kc, :].rearrange("p (m kk) -> p kk m", kk=8)[:, kk, :],
                                  scT[:, kc:kc + 1], start=(kc == 0), stop=(kc == 7)))
        op("pe", fns, reads=[("wst", b), "scT"], writes=["ps0"])
    op("dve", tt(modT, ps[0][:, 0:48], tab[:, TB_BADA:TB_BADA + 48], ALU.add),
       reads=["ps0", "tab"], writes=["modT"])
    op("dve", tt(modP, ps[0][:, 48:64], tab[:, TB_BADAP:TB_BADAP + 16], ALU.add),
       reads=["ps0", "tab"], writes=["modP"])
    op("dve", stt(scale2p, modP[:, 8:16], 1.0, tab[:, TB_GFFNP:TB_GFFNP + 8], ALU.add, ALU.mult),
       reads=["modP", "tab"], writes=["scale2p"])
    op("dve", stt(scale1, modT[:, 8:16], 1.0, tab[:, TB_GMIX:TB_GMIX + 8], ALU.add, ALU.mult),
       reads=["modT", "tab"], writes=["scale1"])
    op("dve", stt(scale2, modT[:, 32:40], 1.0, tab[:, TB_GFFN:TB_GFFN + 8], ALU.add, ALU.mult),
       reads=["modT", "tab"], writes=["scale2"])
    bias1 = modT[:, 0:8]
    bias2 = modT[:, 24:32]
    bias2p = modP[:, 0:8]
    Gt = ar.f32(8 * 128).rearrange("p (k f) -> p k f", k=8)
    for (col0, dst, nm) in ((16, gt1_bc, "gt1_bc"), (40, gt2_bc, "gt2_bc")):
        for kc in range(8):
            op("dve", ts(Gt[:, kc, :], ones_f, modT[:, col0 + kc:col0 + kc + 1], ALU.mult),
               reads=["modT", "ones_f"], writes=[("Gt", kc)])
        for half in range(2):
            fns = [mm(ps[1 + half][:, k4 * 128:(k4 + 1) * 128], Gt[:, half * 4 + k4, :], ident_f)
                   for k4 in range(4)]
            op("pe", fns, reads=[("Gt", half * 4 + k4) for k4 in range(4)] + ["cst"],
               writes=[f"ps{1 + half}"])
            op("act", acp(dst[:, half * 512:(half + 1) * 512], ps[1 + half]),
               reads=[f"ps{1 + half}"], writes=[nm])
    t4 = ar.f32(4)
    op("act", act(t4, tab[:, TB_LAM:TB_LAM + 4], AF.Exp, scale=-1.0), reads=["tab"], writes=["t4"])
    op("act", act(t4, t4, AF.Ln, bias=1.0), reads=["t4"], writes=["t4"])
    op("dve", ts(ltab[:, 0:4], t4, -8.0, ALU.mult), reads=["t4"], writes=["ltab"])
    op("dve", ts(ltab[:, 4:8], t4, -16.0, ALU.mult), reads=["t4"], writes=["ltab"])
    op("dve", ts(ltab[:, 8:10], tab[:, TB_BA:TB_BA + 2], -1.0, ALU.mult), reads=["tab"], writes=["ltab"])
    cl = ltab[:, 0:4]
    c2l = ltab[:, 4:8]
    nba = ltab[:, 8:10]
    m_setup_tmp = ar.mark()

    ar.reset(m0)
    w_in_bf = ar.bf(8 * NPROJ).rearrange("p (k f) -> p k f", k=8)
    w_out_s = ar.bf(8 * D).rearrange("p (k f) -> p k f", k=8)
    bdr_bf = ar.bf(512).rearrange("p (c m) -> p c m", c=4)
    bdi_bf = ar.bf(512).rearrange("p (c m) -> p c m", c=4)
    wa2_bf = ar.bf(256)
    wr_bf = ar.bf(8 * 36).rearrange("p (k n) -> p k n", k=8)
    m_w = ar.mark()
    sc.barrier()
    stg = [ar.f32(NPROJ) for _ in range(2)]
    cast_engs = ["dve", "pool", "act"]
    ci = 0
    for kc in range(8):
        b = kc % 2
        op("sp", dma(stg[b], win_d[kc * 128:(kc + 1) * 128, :]), writes=[("stg", b)], dma=True)
        e = cast_engs[ci % 3]
        ci += 1
        op(e, (acp if e == "act" else cp)(w_in_bf[:, kc, :], stg[b]), reads=[("stg", b)], writes=["w_in_bf"])
    for kc in range(8):
        b = kc % 2
        op("sp", dma(stg[b][:, 0:D], wout_d[kc * 128:(kc + 1) * 128, :]), writes=[("stg", b)], dma=True)
        op("dve", tt(w_out_s[:, kc, :], stg[b][:, 0:D], gt1_bc, ALU.mult),
           reads=[("stg", b), "gt1_bc"], writes=["w_out_s"])
    for (src, dst, nm) in ((bdr_d, bdr_bf, "bdr"), (bdi_d, bdi_bf, "bdi")):
        op("sp", dma(stg[0][:, 0:512], src), writes=[("stg", 0)], dma=True)
        op("dve", cp(dst.rearrange("p c m -> p (c m)"), stg[0][:, 0:512]), reads=[("stg", 0)], writes=[nm])
    op("sp", dma(stg[1][0:16, 0:256], wa2_d), writes=[("stg", 1)], dma=True)
    op("dve", cp(wa2_bf[0:16, :], stg[1][0:16, 0:256]), reads=[("stg", 1)], writes=["wa2_bf"])
    op("sp", dma(stg[0][:, 0:288].rearrange("p (k n) -> p k n", k=8),
                 wr_d.rearrange("(k p) n -> p k n", p=128)), writes=[("stg", 0)], dma=True)
    op("dve", cp(wr_bf.rearrange("p k n -> p (k n)"), stg[0][:, 0:288]), reads=[("stg", 0)], writes=["wr_bf"])
    sc.barrier()
    ar.reset(m_w)

    if stop_after == "setup":
        op("sp", dma(dbg2_d[:, 0:48], modT), reads=["modT"], writes=["dbg2"], dma=True)
        op("sp", dma(dbg2_d[:, 1024:2048], gt1_bc), reads=["gt1_bc"], writes=["dbg2"], dma=True)
        op("sp", dma(dbg2_d[:, 64:80], ltab), reads=["ltab"], writes=["dbg2"], dma=True)
        sc.emit()
        return nc
    NJ = T // 128
    NCH = T // 64
    xs = [ar.f32(NJ * D).rearrange("p (j d) -> p j d", j=NJ) for _ in range(2)]
    xn = ar.bf(NJ * D).rearrange("p (j d) -> p j d", j=NJ)
    hT = ar.bf(8 * T).rearrange("p (k t) -> p k t", k=8)
    xc = ar.f32(4 * (T + 4)).rearrange("p (c t) -> p c t", c=4)
    yb = ar.f32(4 * T).rearrange("p (c t) -> p c t", c=4)
    qf = ar.f32(2 * T).rearrange("p (c t) -> p c t", c=2)
    kf = ar.f32(2 * T).rearrange("p (c t) -> p c t", c=2)
    gfb = ar.f32(4 * T).rearrange("p (c t) -> p c t", c=4)
    glT = ar.bf(T)
    vb = ar.bf(NJ * 512).rearrange("p (j e) -> p j e", j=NJ)
    catT = ar.bf(8 * T).rearrange("p (k t) -> p k t", k=8)
    L = []
    for _ in range(2):
        L.append(dict(cv=ar.f32(T), cvb=ar.bf(T), r=ar.f32(T), i=ar.f32(T), a=ar.f32(T),
                      s=ar.f32(T), h=ar.f32(T), t1=ar.f32(T), t2=ar.f32(T)))
    hprev = ar.f32(4)
    lf = ar.f32(2 * T).rearrange("p (c t) -> p c t", c=2)
    cs = ar.f32(2 * T).rearrange("p (c t) -> p c t", c=2)
    eb = ar.f32(2 * T).rearrange("p (c t) -> p c t", c=2)
    enb = ar.f32(2 * T).rearrange("p (c t) -> p c t", c=2)
    dec = ar.f32(2 * NCH).rearrange("p (c n) -> p c n", c=2)
    qd = ar.bf(2 * T).rearrange("p (c t) -> p c t", c=2)
    kd = ar.bf(2 * T).rearrange("p (c t) -> p c t", c=2)
    ke = ar.bf(2 * T).rearrange("p (c t) -> p c t", c=2)
    keT = ar.bf(NJ * 256).rearrange("p (j f) -> p j f", j=NJ)
    scT_sb = ar.bf(512)
    Sf = ar.f32(256).rearrange("p (c e) -> p c e", c=2)
    Sb = [ar.bf(256).rearrange("p (c e) -> p c e", c=2) for _ in range(2)]
    osq = ar.bf(T)
    sd = ar.f32(T)
    rs = ar.f32(T)
    sg = ar.f32(T)
    gs = ar.f32(T)
    to = ar.f32(T)
    stat = ar.f32(16)
    lg = ar.f32(NJ * 36).rearrange("p (j n) -> p j n", j=NJ)
    rt = ar.f32(NJ * 64).rearrange("p (j n) -> p j n", j=NJ)
    mf = ar.f32(NJ * 32).rearrange("p (j n) -> p j n", j=NJ)
    m8 = ar.f32(NJ * 8).rearrange("p (j n) -> p j n", j=NJ)
    oh = ar.f32(NJ * 64).rearrange("p (j n) -> p j n", j=NJ)
    Ot = ar.f32(NJ * 32).rearrange("p (j n) -> p j n", j=NJ)
    tmp32 = ar.f32(NJ * 64).rearrange("p (j n) -> p j n", j=NJ)
    print("arena used (words):", ar.off, "of", ARW)

    for c in range(4):
        op("pool", mset(xc[:, c, 0:4], 0.0), writes=[("xc", c)])
    op("pool", mset(hprev, 0.0), writes=["hprev"])
    op("pool", mset(Sf.rearrange("p c e -> p (c e)"), 0.0), writes=["Sf"])
    op("pool", mset(Sb[0].rearrange("p c e -> p (c e)"), 0.0), writes=[("Sb", 0)])
    op("pool", mset(Sb[1].rearrange("p c e -> p (c e)"), 0.0), writes=[("Sb", 1)])
    op("pool", mset(Ocum, 0.0), writes=["Ocum"])

    mmslot = [0]

    mmgroup = ["all"]
    MMB = {"all": [2, 3, 4], "front": [2, 3], "back": [4], "lru0": [2], "lru1": [3], "gla": [5]}

    def next_mm():
        banks = MMB[mmgroup[0]]
        bk = banks[mmslot[0] % len(banks)]
        mmslot[0] += 1
        return ps[bk], f"ps{bk}"

    psTb = [ps[0].bitcast(BF16), ps[1].bitcast(BF16)]
    psS = ps[5]
    psKV = ps[5]
    psO = [ps[6], ps[7]]
    qz = [[ar.bf(T) for _ in range(2)] for _ in range(2)]
    osq2 = ar.bf(2 * T)
    sd2 = ar.f32(2 * T)
    rs2 = ar.f32(2 * T)
    sg2 = ar.f32(2 * T)
    gs2 = ar.f32(2 * T)
    to2 = ar.f32(2 * T)
    print("arena used (words):", ar.off, "of", ARW)

    xnB = ar.bf(NJ * D).rearrange("p (j d) -> p j d", j=NJ)
    hTB = ar.bf(8 * T).rearrange("p (k t) -> p k t", k=8)
    catT2 = [catT, ar.bf(8 * T).rearrange("p (k t) -> p k t", k=8)]
    statB = ar.f32(16)
    print("arena used (words):", ar.off, "of", ARW)

    def norm_stats(xsrc_key, xbuf, xn_, st_, tag):
        sk = "stat" + tag
        for j in range(NJ):
            op("act", act(xn_[:, j, :], xbuf[:, j, :], AF.Square, accum_out=st_[:, j:j + 1]),
               reads=[xsrc_key], writes=[("xn" + tag, j), sk])
        op("act", act(st_[:, 2:4], st_[:, 0:2], AF.Sqrt, bias=EPS, scale=1.0 / D),
           reads=[sk], writes=[sk])
        op("dve", lambda e: e.reciprocal(out=st_[:, 4:6], in_=st_[:, 2:4]),
           reads=[sk], writes=[sk])
        for j in range(NJ):
            op("dve", ts(xn_[:, j, :], xbuf[:, j, :], st_[:, 4 + j:5 + j], ALU.mult),
               reads=[xsrc_key, sk], writes=[("xn" + tag, j)])

    def transposes_to_hT(scale_t, bias_t, xn_, hT_, tag, tb):
        for hb_ in range(2):
            hb = tb
            fns = []
            for k4 in range(4):
                kc = hb_ * 4 + k4
                for j in range(NJ):
                    fns.append(tr(psTb[hb][:, k4 * T + j * 128: k4 * T + (j + 1) * 128],
                                  xn_[:, j, kc * 128:(kc + 1) * 128], ident_bf))
            op("pe", fns, reads=[("xn" + tag, j) for j in range(NJ)] + ["ident_bf"], writes=[f"ps{hb}"])
            for k4 in range(4):
                kc = hb_ * 4 + k4
                src = psTb[hb][:, k4 * T:(k4 + 1) * T]
                op("act", act(hT_[:, kc, :], src, AF.Identity, bias=bias_t[:, kc:kc + 1],
                              scale=scale_t[:, kc:kc + 1]),
                   reads=[f"ps{hb}", "scale1", "scale2", "modT"], writes=[("hT" + tag, kc)])

    hT_keys = [("hTF", kc) for kc in range(8)]
    hTB_keys = [("hTB", kc) for kc in range(8)]
    ev_i = [0]

    def evac(dst, src, skey, dkey):
        e = "act" if (ev_i[0] // 2) % 2 == 0 else "dve"
        ev_i[0] += 1
        op(e, (acp if e == "act" else cp)(dst, src), reads=[skey], writes=[dkey])

    def load_x(t_):
        op("sp", dma(xs[t_ % 2], x_d[t_ * T:(t_ + 1) * T, :].rearrange("(j p) d -> p j d", p=128)),
           writes=[("xs", t_ % 2)], dma=True)

    def front(it):
        xb = it % 2
        X = xs[xb]
        xk = ("xs", xb)
        catT = catT2[it % 2]
        ctag = "catT%d" % (it % 2)
        norm_stats(xk, X, xn, stat, "F")
        mmgroup[0] = "front"
        transposes_to_hT(scale1, bias1, xn, hT, "F", 0)

        fm_list = []
        for c in range(4):
            fm_list.append((c * 128, xc[:, c, 4:4 + T], ("xc", c)))
        for c in range(4):
            fm_list.append((512 + c * 128, yb[:, c, :], ("yb", c)))
        for i in range(2):
            fm_list.append((1024 + i * 128, qf[:, i, :], ("qf", i)))
        for i in range(2):
            fm_list.append((1280 + i * 128, kf[:, i, :], ("kf", i)))
        for h in range(4):
            fm_list.append((2048 + h * 128, gfb[:, h, :], ("gf", h)))
        for pi_ in range(0, 16, 2):
            pso, key = next_mm()
            fns = []
            for u in range(2):
                col0 = fm_list[pi_ + u][0]
                fns += [mm(pso[:, u * T:(u + 1) * T], w_in_bf[:, kc, col0:col0 + 128], hT[:, kc, :],
                           start=(kc == 0), stop=(kc == 7)) for kc in range(8)]
            op("pe", fns, reads=hT_keys + ["w_in_bf"], writes=[key])
            for u in range(2):
                _, dst, dkey = fm_list[pi_ + u]
                evac(dst, pso[:, u * T:(u + 1) * T], key, dkey)
        pso, key = next_mm()
        fns = [mm(pso[0:16, 0:T], w_in_bf[:, kc, 2560:2576], hT[:, kc, :], start=(kc == 0), stop=(kc == 7))
               for kc in range(8)]
        op("pe", fns, reads=hT_keys + ["w_in_bf"], writes=[key])
        evac(glT[0:16, :], pso[0:16, 0:T], key, "glT")
        ev_i[0] += 1
        for j in range(NJ):
            pso, key = next_mm()
            fns = [mm(pso, hT[:, kc, j * 128:(j + 1) * 128], w_in_bf[:, kc, 1536:2048],
                      start=(kc == 0), stop=(kc == 7)) for kc in range(8)]
            op("pe", fns, reads=hT_keys + ["w_in_bf"], writes=[key])
            evac(vb[:, j, :], pso, key, ("vb", j))
            ev_i[0] += 1

        lru_lists = {0: [], 1: []}
        for c in range(4):
            mmgroup[0] = "lru%d" % (c % 2)
            sc.rec_begin()
            B = L[c % 2]
            lk = ("L", c % 2)
            cw = tab[:, TB_CW + c * 4:TB_CW + c * 4 + 4]
            op("dve", ts(B["cv"], xc[:, c, 4:4 + T], cw[:, 3:4], ALU.mult, tab[:, TB_CB + c:TB_CB + c + 1], ALU.add),
               reads=[("xc", c), "tab"], writes=[lk + ("cv",)])
            for k in range(3):
                op("dve", stt(B["cv"], xc[:, c, 1 + k:1 + k + T], cw[:, k:k + 1], B["cv"], ALU.mult, ALU.add),
                   reads=[("xc", c), "tab", lk + ("cv",)], writes=[lk + ("cv",)])
            op("pool", cp(B["cvb"], B["cv"]), reads=[lk + ("cv",)], writes=[lk + ("cvb",)])
            op("pool", cp(xc[:, c, 0:4], xc[:, c, T:T + 4]), reads=[("xc", c)], writes=[("xc", c)])
            pg, kg = next_mm()
            op("pe", [mm(pg[:, 0:T], bdr_bf[:, c, :], B["cvb"]), mm(pg[:, T:2 * T], bdi_bf[:, c, :], B["cvb"])],
               reads=[lk + ("cvb",), "bdr", "bdi"], writes=[kg])
            op("act", act(B["r"], pg[:, 0:T], AF.Sigmoid, bias=tab[:, TB_BR + c:TB_BR + c + 1]),
               reads=[kg, "tab"], writes=[lk + ("r",)])
            op("act", act(B["i"], pg[:, T:2 * T], AF.Sigmoid, bias=tab[:, TB_BI + c:TB_BI + c + 1]),
               reads=[kg, "tab"], writes=[lk + ("i",)])
            op("act", act(B["a"], B["r"], AF.Exp, scale=cl[:, c:c + 1]),
               reads=[lk + ("r",), "ltab"], writes=[lk + ("a",)])
            op("act", act(B["s"], B["r"], AF.Exp, scale=c2l[:, c:c + 1]),
               reads=[lk + ("r",), "ltab"], writes=[lk + ("s",)])
            op("act", act(B["s"], B["s"], AF.Sqrt, bias=1.0, scale=-1.0),
               reads=[lk + ("s",)], writes=[lk + ("s",)])
            op("pool", tt(B["i"], B["i"], B["cv"], ALU.mult), reads=[lk + ("i",), lk + ("cv",)], writes=[lk + ("i",)])
            op("pool", tt(B["i"], B["i"], B["s"], ALU.mult), reads=[lk + ("i",), lk + ("s",)], writes=[lk + ("i",)])
            op("dve", scan(B["h"], B["a"], B["i"], hprev[:, c:c + 1], ALU.mult, ALU.add),
               reads=[lk + ("a",), lk + ("i",), "hprev"], writes=[lk + ("h",)])
            op("pool", cp(hprev[:, c:c + 1], B["h"][:, T - 1:T]), reads=[lk + ("h",)], writes=["hprev"])
            Y = yb[:, c, :]
            op("pool", tt(B["t1"], Y, Y, ALU.mult), reads=[("yb", c)], writes=[lk + ("t1",)])
            op("pool", ts(B["t1"], B["t1"], 0.044715, ALU.mult, 1.0, ALU.add),
               reads=[lk + ("t1",)], writes=[lk + ("t1",)])
            op("pool", tt(B["t1"], B["t1"], Y, ALU.mult), reads=[lk + ("t1",), ("yb", c)], writes=[lk + ("t1",)])
            op("act", act(B["t2"], B["t1"], AF.Sigmoid, scale=1.5957691216057308),
               reads=[lk + ("t1",)], writes=[lk + ("t2",)])
            op("pool", tt(B["t2"], B["t2"], Y, ALU.mult), reads=[lk + ("t2",), ("yb", c)], writes=[lk + ("t2",)])
            op("dve", tt(catT[:, c, :], B["h"], B["t2"], ALU.mult),
               reads=[lk + ("h",), lk + ("t2",)], writes=[(ctag, c)])
            lru_lists[c % 2] += sc.rec_end()
        mmgroup[0] = "gla"
        sc.rec_begin()

        pz, kz = next_mm()
        op("pe", [mm(pz[:, i * T:(i + 1) * T], wa2_bf[0:16, i * 128:(i + 1) * 128], glT[0:16, :]) for i in range(2)],
           reads=["glT", "wa2_bf"], writes=[kz])
        for i in range(2):
            op("act", act(lf[:, i, :], pz[:, i * T:(i + 1) * T], AF.Exp, bias=nba[:, i:i + 1], scale=-1.0),
               reads=[kz, "ltab"], writes=[("lf", i)])
            op("act", act(lf[:, i, :], lf[:, i, :], AF.Ln, bias=1.0), reads=[("lf", i)], writes=[("lf", i)])
            op("dve", scan(cs[:, i, :], rmask[:, 0:T], lf[:, i, :], 0.0, ALU.mult, ALU.add),
               reads=[("lf", i), "cst"], writes=[("cs", i)])
            op("act", act(eb[:, i, :], cs[:, i, :], AF.Exp, scale=-1.0 / 16), reads=[("cs", i)], writes=[("eb", i)])
            op("act", act(enb[:, i, :], cs[:, i, :], AF.Exp, scale=1.0 / 16), reads=[("cs", i)], writes=[("enb", i)])
            op("act", act(dec[:, i, 0:NJ], cs[:, i, :].rearrange("p (n t) -> p n t", t=128)[:, :, 127],
                          AF.Exp, scale=-1.0 / 16), reads=[("cs", i)], writes=[("dec", i)])
            op("dve", stt(qd[:, i, :], qf[:, i, :], 0.125, eb[:, i, :], ALU.mult, ALU.mult),
               reads=[("qf", i), ("eb", i)], writes=[("qd", i)])
            for hh in range(2):
                op("pool", ts(qz[i][hh], qd[:, i, :], hmask[:, hh:hh + 1], ALU.mult),
                   reads=[("qd", i), "cst"], writes=[("qz", i, hh)])
            op("dve", tt(kd[:, i, :], kf[:, i, :], enb[:, i, :], ALU.mult),
               reads=[("kf", i), ("enb", i)], writes=[("kd", i)])
            op("pool", tt(ke[:, i, :].rearrange("p (n t) -> p n t", t=128),
                          kd[:, i, :].rearrange("p (n t) -> p n t", t=128),
                          dec[:, i, 0:NJ].unsqueeze(2).to_broadcast([128, NJ, 128]), ALU.mult),
               reads=[("kd", i), ("dec", i)], writes=[("ke", i)])
        fns = []
        for j in range(NJ):
            for i in range(2):
                fns.append(tr(psTb[0][:, (j * 2 + i) * 128:(j * 2 + i + 1) * 128], ke[:, i, j * 128:(j + 1) * 128], ident_bf))
        op("pe", fns, reads=[("ke", 0), ("ke", 1), "ident_bf"], writes=["ps0"])
        op("act", acp(keT.rearrange("p j f -> p (j f)"), psTb[0][:, 0:NJ * 256]), reads=["ps0"], writes=["keT"])
        for j in range(NJ):
            par = (it * NJ + j) % 2
            tsl = slice(j * 128, (j + 1) * 128)
            fns = []
            for h in range(4):
                i, hh = h // 2, h % 2
                fns.append(mm(psS[:, h * 128:(h + 1) * 128], kd[:, i, tsl], qz[i][hh][:, tsl]))
            op("pe", fns, reads=[("kd", 0), ("kd", 1)] + [("qz", i, hh) for i in range(2) for hh in range(2)],
               writes=["ps5"])
            op("dve", tt(scT_sb.rearrange("p (a c) -> p a c", c=128),
                         psS.rearrange("p (a c) -> p a c", c=128),
                         cmask.unsqueeze(1).to_broadcast([128, 4, 128]), ALU.mult),
               reads=["ps5", "cst"], writes=["scT"])
            for i in range(2):
                fns = []
                for hh in range(2):
                    h = 2 * i + hh
                    o_ap = psO[i][:, hh * T + j * 128: hh * T + (j + 1) * 128]
                    fns.append(mm(o_ap, vb[:, j, h * 128:(h + 1) * 128], scT_sb[:, h * 128:(h + 1) * 128],
                                  start=True, stop=False))
                    fns.append(mm(o_ap, Sb[par][:, i, :], qz[i][hh][:, tsl], start=False, stop=True))
                op("pe", fns, reads=[("vb", j), "scT", ("Sb", par), ("qz", i, 0), ("qz", i, 1)], writes=[f"ps{6 + i}"])
            fns = []
            for h in range(4):
                i = h // 2
                fns.append(mm(psKV[:, h * 128:(h + 1) * 128], keT[:, j, i * 128:(i + 1) * 128],
                              vb[:, j, h * 128:(h + 1) * 128]))
            op("pe", fns, reads=["keT", ("vb", j)], writes=["ps5"])
            for h in range(4):
                i, hh = h // 2, h % 2
                r0, r1 = hh * 64, (hh + 1) * 64
                op("dve", stt(Sf[r0:r1, i, :], Sf[r0:r1, i, :], dec[r0:r1, i, j:j + 1],
                              psKV[r0:r1, h * 128:(h + 1) * 128], ALU.mult, ALU.add),
                   reads=["Sf", ("dec", i), "ps5"], writes=["Sf"])
            op("act", acp(Sb[1 - par].rearrange("p c e -> p (c e)"), Sf.rearrange("p c e -> p (c e)")),
               reads=["Sf"], writes=[("Sb", 1 - par)])
        for i in range(2):
            O = psO[i]
            ok = f"ps{6 + i}"
            op("act", act(osq2, O, AF.Square), reads=[ok], writes=["osq"])
            pss, kss = next_mm()
            op("pe", mm(pss, ones_bf, osq2), reads=["osq", "ones_bf"], writes=[kss])
            op("act", act(sd2, pss, AF.Sqrt, bias=EPS, scale=1.0 / 128), reads=[kss], writes=["sd"])
            op("dve", lambda e: e.reciprocal(out=rs2, in_=sd2), reads=["sd"], writes=["rs"])
            G = gfb[:, 2 * i:2 * i + 2, :].rearrange("p c t -> p (c t)")
            gk = [("gf", 2 * i), ("gf", 2 * i + 1)]
            op("act", act(sg2, G, AF.Sigmoid), reads=gk, writes=["sg"])
            op("dve", stt(gs2, G, tab[:, TB_GN:TB_GN + 1], sg2, ALU.mult, ALU.mult),
               reads=gk + ["sg", "tab"], writes=["gs"])
            op("dve", tt(to2, O, rs2, ALU.mult), reads=[ok, "rs"], writes=["to"])
            op("dve", tt(catT[:, 4 + 2 * i:6 + 2 * i, :].rearrange("p c t -> p (c t)"), to2, gs2, ALU.mult),
               reads=["to", "gs"], writes=[(ctag, 4 + 2 * i), (ctag, 5 + 2 * i)])

        gla_list = sc.rec_end()
        sc.play(Sched.merge([lru_lists[0], lru_lists[1], gla_list]))

    def back(it):
        xb = it % 2
        X = xs[xb]
        xk = ("xs", xb)
        catT = catT2[it % 2]
        ctag = "catT%d" % (it % 2)
        mmgroup[0] = "back"
        cat_keys = [(ctag, k) for k in range(8)]
        for j in range(NJ):
            for half in range(2):
                pso, key = next_mm()
                fns = [mm(pso, catT[:, kc, j * 128:(j + 1) * 128], w_out_s[:, kc, half * 512:(half + 1) * 512],
                          start=(kc == 0), stop=(kc == 7)) for kc in range(8)]
                op("pe", fns, reads=cat_keys + ["w_out_s"], writes=[key])
                op("dve", tt(X[:, j, half * 512:(half + 1) * 512], X[:, j, half * 512:(half + 1) * 512], pso, ALU.add),
                   reads=[key, xk], writes=[xk])
        op("sp", dma(xmid_d[it * T:(it + 1) * T, :].rearrange("(j p) d -> p j d", p=128), X),
           reads=[xk], writes=["xmid_scr"], dma=True)
        if dbg and stop_after == "mixer":
            op("sp", dma(dbg_d[it * T:(it + 1) * T, :].rearrange("(j p) d -> p j d", p=128), X),
               reads=[xk], writes=["dbg"], dma=True)
            if it + 2 < NT:
                load_x(it + 2)
            return

        norm_stats(xk, X, xnB, statB, "B")
        op("sp", dma(xn2_d[it * T:(it + 1) * T, :].rearrange("(j p) d -> p j d", p=128), xnB),
           reads=[("xnB", j) for j in range(NJ)], writes=["xn2_scr"], dma=True)
        if it + 2 < NT:
            load_x(it + 2)
        transposes_to_hT(scale2, bias2, xnB, hTB, "B", 1)
        pso, key = next_mm()
        fns = []
        for j in range(NJ):
            for kc in range(8):
                fns.append(mm(pso[:, j * 64:j * 64 + 36], hTB[:, kc, j * 128:(j + 1) * 128], wr_bf[:, kc, :],
                              start=(kc == 0), stop=(kc == 7)))
        op("pe", fns, reads=hTB_keys + ["wr_bf"], writes=[key])
        for j in range(NJ):
            op("dve", tt(lg[:, j, :], pso[:, j * 64:j * 64 + 36], brb, ALU.add), reads=[key, "brb"], writes=[("rt", j)])
        for j in range(NJ):
            st = it * NJ + j
            rk = ("rt", j)
            op("dve", red(rt[:, j, 0:1], lg[:, j, 0:4], ALU.max), reads=[rk], writes=[rk])
            op("dve", ts(rt[:, j, 4:8], lg[:, j, 0:4], rt[:, j, 0:1], ALU.is_equal), reads=[rk], writes=[rk])
            op("dve", ts(rt[:, j, 8:12], lg[:, j, 0:4], rt[:, j, 0:1], ALU.subtract), reads=[rk], writes=[rk])
            op("act", act(rt[:, j, 8:12], rt[:, j, 8:12], AF.Exp, accum_out=rt[:, j, 1:2]), reads=[rk], writes=[rk])
            op("dve", lambda e, j=j: e.reciprocal(out=rt[:, j, 2:3], in_=rt[:, j, 1:2]), reads=[rk], writes=[rk])
            op("dve", ts(rt[:, j, 12:16], rt[:, j, 4:8], 1e30, ALU.mult, -1e30, ALU.add), reads=[rk], writes=[rk])
            op("dve", tt(mf[:, j, :].rearrange("p (g e) -> p g e", g=4),
                         lg[:, j, 4:36].rearrange("p (g e) -> p g e", g=4),
                         rt[:, j, 12:16].unsqueeze(2).to_broadcast([128, 4, 8]), ALU.add), reads=[rk], writes=[rk])
            op("dve", lambda e, j=j: e.max(out=m8[:, j, :], in_=mf[:, j, :]), reads=[rk], writes=[rk])
            op("dve", ts(oh[:, j, 0:32], mf[:, j, :], m8[:, j, 0:1], ALU.is_equal), reads=[rk], writes=[rk])
            op("dve", ts(oh[:, j, 32:64], mf[:, j, :], m8[:, j, 1:2], ALU.is_equal), reads=[rk], writes=[rk])
            op("dve", cp(ohs[:, st * 64:(st + 1) * 64], oh[:, j, :]), reads=[rk], writes=["ohs"])
            op("dve", tt(rt[:, j, 16:17], m8[:, j, 1:2], m8[:, j, 0:1], ALU.subtract), reads=[rk], writes=[rk])
            op("act", act(rt[:, j, 17:18], rt[:, j, 16:17], AF.Exp), reads=[rk], writes=[rk])
            op("dve", ts(rt[:, j, 18:19], rt[:, j, 17:18], 1.0, ALU.add), reads=[rk], writes=[rk])
            op("dve", lambda e, j=j: e.reciprocal(out=rt[:, j, 19:20], in_=rt[:, j, 18:19]), reads=[rk], writes=[rk])
            op("dve", tt(wts_all[:, st * 2:st * 2 + 1], rt[:, j, 19:20], rt[:, j, 2:3], ALU.mult),
               reads=[rk], writes=["wts_all"])
            op("dve", tt(wts_all[:, st * 2 + 1:st * 2 + 2], wts_all[:, st * 2:st * 2 + 1], rt[:, j, 17:18], ALU.mult),
               reads=[rk, "wts_all"], writes=["wts_all"])
            op("dve", tt(Ot[:, j, :], oh[:, j, 0:32], oh[:, j, 32:64], ALU.add), reads=[rk], writes=[("Ot", j)])
            pp, kp = next_mm()
            op("pe", [mm(pp[:, 0:32], Umat, Ot[:, j, :], start=True, stop=False),
                      mm(pp[:, 0:32], ones_f, Ocum, start=False, stop=True)],
               reads=[("Ot", j), "Ocum", "cst", "ones_f"], writes=[kp])
            op("dve", tt(Ocum, Ocum, Ot[:, j, :], ALU.add), reads=[("Ot", j), "Ocum"], writes=["Ocum"])
            for k in range(2):
                o_k = oh[:, j, k * 32:(k + 1) * 32]
                op("dve", tt(tmp32[:, j, 0:32], o_k, pp[:, 0:32], ALU.mult), reads=[rk, kp], writes=[("tmp32", j)])
                op("dve", red(pos_all[:, st * 2 + k:st * 2 + k + 1], tmp32[:, j, 0:32], ALU.add),
                   reads=[("tmp32", j)], writes=["pos_all"])
                op("dve", tt(tmp32[:, j, 32:64], o_k, iota32, ALU.mult), reads=[rk, "cst"], writes=[("tmp32", j)])
                op("dve", red(eid_all[:, st * 2 + k:st * 2 + k + 1], tmp32[:, j, 32:64], ALU.add),
                   reads=[("tmp32", j)], writes=["eid_all"])
                op("dve", stt(rt[:, j, 20 + k:21 + k], eid_all[:, st * 2 + k:st * 2 + k + 1], float(CAP),
                              pos_all[:, st * 2 + k:st * 2 + k + 1], ALU.mult, ALU.add),
                   reads=["eid_all", "pos_all", rk], writes=[rk])
                op("dve", cp(slot_u[:, st * 2 + k:st * 2 + k + 1], rt[:, j, 20 + k:21 + k]), reads=[rk], writes=["slot_u"])
                op("pool", cp(pay_i[:, (st * 2 + k) * 2:(st * 2 + k) * 2 + 1], tokid[:, st:st + 1]),
                   reads=["tokid"], writes=[("pay", st, k)])
                op("pool", cp(pay_f[:, (st * 2 + k) * 2 + 1:(st * 2 + k) * 2 + 2], wts_all[:, st * 2 + k:st * 2 + k + 1]),
                   reads=["wts_all", ("pay", st, k)], writes=[("pay", st, k)])
                sl = slot_u[:, st * 2 + k:st * 2 + k + 1]
                py = pay_i[:, (st * 2 + k) * 2:(st * 2 + k) * 2 + 2]
                op("pool", lambda e, sl=sl, py=py: e.indirect_dma_start(
                    out=sidx_d[:, :], out_offset=bass.IndirectOffsetOnAxis(ap=sl, axis=0),
                    in_=py, in_offset=None), reads=["slot_u", ("pay", st, k)], writes=["sidx_scr"], dma=True)

    load_x(0)
    if NT > 1:
        load_x(1)
    front(0)
    for it in range(NT):
        lists = []
        if it + 1 < NT:
            sc.rec_begin()
            front(it + 1)
            lists.append(sc.rec_end())
        sc.rec_begin()
        back(it)
        lists.append(sc.rec_end())
        sc.play(Sched.merge(lists))
    mmgroup[0] = "all"

    if stop_after in ("mixer", "route"):
        if dbg and stop_after == "route":
            op("sp", dma(dbg2_d[:, 0:NST * 2], pos_all), reads=["pos_all"], writes=["dbg2"], dma=True)
            op("sp", dma(dbg2_d[:, 1024:1024 + NST * 2], eid_all), reads=["eid_all"], writes=["dbg2"], dma=True)
            op("sp", dma(dbg2_d[:, 2048:2048 + NST * 2], wts_all), reads=["wts_all"], writes=["dbg2"], dma=True)
        sc.emit()
        return nc

    sc.barrier()
    ar.reset(m0)
    thr = cst[:, CS_TH:CS_TH + NTH]
    cnt = ar.f32(32)
    big_full = ar.f32(max(NST * 2 * 32, 32 * NTH))
    big = big_full[:, 0:NST * 2 * 32]
    nblk = ar.f32(32)
    padded = ar.f32(32)
    pends = ar.f32(32)
    ebase = ar.f32(32)
    bt = ar.f32(64)
    slc_f = ar.f32(NST * 2)
    op("pe", mm(ps[2][:, 0:32], ones_f, Ocum), reads=["Ocum", "ones_f"], writes=["ps2"])
    op("dve", cp(cnt, ps[2][:, 0:32]), reads=["ps2"], writes=["cnt"])
    op("dve", tt(big_full[:, 0:32 * NTH].rearrange("p (e m) -> p e m", m=NTH),
                 cnt.unsqueeze(2).to_broadcast([128, 32, NTH]),
                 thr.unsqueeze(1).to_broadcast([128, 32, NTH]), ALU.is_gt),
       reads=["cnt", "cst"], writes=["big"])
    op("dve", red(nblk, big_full[:, 0:32 * NTH].rearrange("p (e m) -> p e m", m=NTH), ALU.add),
       reads=["big"], writes=["nblk"])
    op("dve", ts(padded, nblk, float(BLK), ALU.mult), reads=["nblk"], writes=["padded"])
    op("dve", scan(pends, ones_f[:, 0:32], padded, 0.0, ALU.mult, ALU.add), reads=["padded", "ones_f"], writes=["pends"])
    op("dve", tt(ebase, pends, padded, ALU.subtract), reads=["pends", "padded"], writes=["ebase"])
    op("dve", ts(bt[:, 0:32], pends, blk512, ALU.is_le), reads=["pends", "cst"], writes=["bt"])
    op("dve", red(bt[:, 32:33], bt[:, 0:32], ALU.add), reads=["bt"], writes=["bt"])
    op("dve", ts(bt[:, 32:33], bt[:, 32:33], float(NE - 1), ALU.min), reads=["bt"], writes=["bt"])
    op("dve", ts(bt[:, 0:32], iota32, bt[:, 32:33], ALU.is_equal), reads=["bt", "cst"], writes=["bt"])
    op("dve", tt(bt[:, 0:32], bt[:, 0:32], ebase, ALU.mult), reads=["bt", "ebase"], writes=["bt"])
    op("dve", red(bt[:, 33:34], bt[:, 0:32], ALU.add), reads=["bt"], writes=["bt"])
    op("dve", ts(bt[:, 34:35], blk512, pends[:, 31:32], ALU.is_lt), reads=["pends", "cst"], writes=["bt"])
    op("dve", stt(bt[:, 35:36], bt[:, 32:33], float(CAP), blk512, ALU.mult, ALU.add), reads=["bt", "cst"], writes=["bt"])
    op("dve", tt(bt[:, 35:36], bt[:, 35:36], bt[:, 33:34], ALU.subtract), reads=["bt"], writes=["bt"])
    op("dve", ts(bt[:, 35:36], bt[:, 35:36], float(-NULLSTART), ALU.add), reads=["bt"], writes=["bt"])
    op("dve", tt(bt[:, 35:36], bt[:, 35:36], bt[:, 34:35], ALU.mult), reads=["bt"], writes=["bt"])
    op("dve", ts(bt[:, 35:36], bt[:, 35:36], float(NULLSTART), ALU.add), reads=["bt"], writes=["bt"])
    Gb = ar.f32(256).rearrange("p (a m) -> p a m", a=2)
    bcf = ar.f32(256).rearrange("p (a m) -> p a m", a=2)
    pcol = ar.f32(2)
    op("dve", ts(Gb[:, 0, :], ones_f, bt[:, 32:33], ALU.mult), reads=["bt", "ones_f"], writes=["Gb"])
    op("dve", ts(Gb[:, 1, :], ones_f, bt[:, 35:36], ALU.mult), reads=["bt", "ones_f"], writes=["Gb"])
    op("pe", [mm(ps[3][:, 0:128], Gb[:, 0, :], ident_f), mm(ps[3][:, 128:256], Gb[:, 1, :], ident_f)],
       reads=["Gb", "cst"], writes=["ps3"])
    op("dve", cp(bcf.rearrange("p a m -> p (a m)"), ps[3][:, 0:256]), reads=["ps3"], writes=["bcf"])
    op("dve", ts(pcol[:, 0:1], blk512, 1.0 / BLK, ALU.mult), reads=["cst"], writes=["pcol"])
    op("dve", ts(bcf[:, 0, :], bcf[:, 0, :], 128.0, ALU.mult, pcol[:, 0:1], ALU.add), reads=["bcf", "pcol"], writes=["bcf"])
    op("dve", cp(widx, bcf[:, 0, :]), reads=["bcf"], writes=["widx"])
    op("dve", ts(bcf[:, 1, :], bcf[:, 1, :], pcol[:, 0:1], ALU.add), reads=["bcf", "pcol"], writes=["bcf"])
    for j in range(4):
        op("dve", ts(Gb[:, 0, :], bcf[:, 1, :], float(j * 128), ALU.add), reads=["bcf", "Gb"], writes=["Gb"])
        op("dve", cp(sidx4[:, j * 128:(j + 1) * 128], Gb[:, 0, :]), reads=["Gb"], writes=["sidx4"])
    op("dve", tt(big.rearrange("p (a e) -> p a e", e=32), ohs.rearrange("p (a e) -> p a e", e=32),
                 ebase.unsqueeze(1).to_broadcast([128, NST * 2, 32]), ALU.mult),
       reads=["ohs", "ebase", "big"], writes=["big"])
    op("dve", red(slc_f, big.rearrange("p (a e) -> p a e", e=32), ALU.add), reads=["big"], writes=["slc_f"])
    op("dve", tt(slc_f, slc_f, pos_all, ALU.add), reads=["slc_f", "pos_all"], writes=["slc_f"])
    op("dve", cp(slot_c, slc_f), reads=["slc_f"], writes=["slot_c"])
    if dbg and stop_after == "tables":
        op("sp", dma(dbg2_d[:, 0:128], widx.bitcast(F32)), reads=["widx"], writes=["dbg2"], dma=True)
        op("sp", dma(dbg2_d[:, 512:1024], sidx4.bitcast(F32)), reads=["sidx4"], writes=["dbg2"], dma=True)
        op("sp", dma(dbg2_d[:, 1024:1024 + NST * 2], slot_c.bitcast(F32)), reads=["slot_c"], writes=["dbg2"], dma=True)
        op("sp", dma(dbg2_d[:, 256:288], pends), reads=["pends"], writes=["dbg2"], dma=True)
        sc.emit()
        return nc

    m3 = ar.mark()
    NSTG = 3
    stg3 = [ar.f32(4096) for _ in range(NSTG)]
    wg_bf = [ar.bf(8 * 512).rearrange("p (k f) -> p k f", k=8) for _ in range(2)]
    wu_bf = [ar.bf(8 * 512).rearrange("p (k f) -> p k f", k=8) for _ in range(2)]
    wd_bf = [ar.bf(4 * 1024).rearrange("p (k f) -> p k f", k=4) for _ in range(2)]
    Xg = [ar.bf(4 * D).rearrange("p (j d) -> p j d", j=4) for _ in range(2)]
    h2T = ar.bf(8 * BLK).rearrange("p (k t) -> p k t", k=8)
    hidT = ar.bf(4 * BLK).rearrange("p (k t) -> p k t", k=4)
    sgb = [ar.f32(BLK) for _ in range(2)]
    ysb = [ar.f32(D) for _ in range(2)]
    print("arena used phase3 (words):", ar.off, "of", ARW)
    stg_i = [0]
    cast_i = [0]
    wgv = wg_d.rearrange("e (p kk) f -> (e p) (kk f)", kk=8)
    wuv = wu_d.rearrange("e (p kk) f -> (e p) (kk f)", kk=8)
    wdv = wd_d.rearrange("e (p kk) f -> (e p) (kk f)", kk=4)

    def issue_loads(b):
        q = b % 2
        for j in range(4):
            op("pool", lambda e, j=j, q=q, b=b: e.indirect_dma_start(
                out=idx_sb[q][:, 2 * j:2 * j + 2], out_offset=None, in_=sidx_d[:, :],
                in_offset=bass.IndirectOffsetOnAxis(ap=sidx4[:, j * 128 + b:j * 128 + b + 1], axis=0)),
               reads=["sidx4", "sidx_scr"], writes=[("idx", q, j)], dma=True)
        casts = []
        for (wv, dst, nm) in ((wgv, wg_bf[q], "wg"), (wuv, wu_bf[q], "wu"), (wdv, wd_bf[q], "wd")):
            sgi = stg_i[0] % NSTG
            stg_i[0] += 1
            op("pool", lambda e, wv=wv, sgi=sgi, b=b: e.indirect_dma_start(
                out=stg3[sgi], out_offset=None, in_=wv,
                in_offset=bass.IndirectOffsetOnAxis(ap=widx[:, b:b + 1], axis=0)),
               reads=["widx"], writes=[("stg3", sgi)], dma=True)
            casts.append((dst, nm, sgi))
        for j in range(4):
            op("pool", lambda e, j=j, q=q: e.indirect_dma_start(
                out=Xg[q][:, j, :], out_offset=None, in_=xn2_d[:, :],
                in_offset=bass.IndirectOffsetOnAxis(ap=idx_sb[q][:, 2 * j:2 * j + 1], axis=0)),
               reads=[("idx", q, j), "xn2_scr"], writes=[("Xg", q, j)], dma=True)
        for (dst, nm, sgi) in casts:
            dflat = dst.rearrange("p k f -> p (k f)")
            for hf in range(2):
                sl = slice(hf * 2048, (hf + 1) * 2048)
                if nm != "wd":
                    ce = ("act", "dve")[cast_i[0] % 2]
                    cast_i[0] += 1
                    op(ce, (acp if ce == "act" else cp)(dflat[:, sl], stg3[sgi][:, sl]),
                       reads=[("stg3", sgi)], writes=[(nm, q, hf)])
                else:
                    op("dve", tt(dflat[:, sl].rearrange("p (k f) -> p k f", k=2),
                                 stg3[sgi][:, sl].rearrange("p (k f) -> p k f", k=2),
                                 gt2_bc.unsqueeze(1).to_broadcast([128, 2, D]), ALU.mult),
                       reads=[("stg3", sgi), "gt2_bc"], writes=[(nm, q, hf)])

    issue_loads(0)
    for b in range(NB):
        q = b % 2
        if b + 1 < NB:
            issue_loads(b + 1)
        for r4 in range(4):
            hb = r4 % 2
            fns = []
            for u in range(2):
                kc = r4 * 2 + u
                for j in range(4):
                    fns.append(tr(psTb[hb][:, u * BLK + j * 128:u * BLK + (j + 1) * 128],
                                  Xg[q][:, j, :].rearrange("p (m kk) -> p kk m", kk=8)[:, kc, :], ident_bf))
            op("pe", fns, reads=[("Xg", q, j) for j in range(4)] + ["ident_bf"], writes=[f"ps{hb}"])
            for u in range(2):
                kc = r4 * 2 + u
                op("act", act(h2T[:, kc, :], psTb[hb][:, u * BLK:(u + 1) * BLK], AF.Identity,
                              bias=bias2p[:, kc:kc + 1], scale=scale2p[:, kc:kc + 1]),
                   reads=[f"ps{hb}", "scale2p", "modP"], writes=[("h2T", kc)])
        h2_keys = [("h2T", kc) for kc in range(8)]
        for hc in range(4):
            pg, kg = next_mm()
            op("pe", [mm(pg, wg_bf[q][:, kc, :].rearrange("p (m c) -> p c m", c=4)[:, hc, :], h2T[:, kc, :], start=(kc == 0), stop=(kc == 7))
                      for kc in range(8)], reads=h2_keys + [("wg", q, 0), ("wg", q, 1)], writes=[kg])
            pu, ku = next_mm()
            op("pe", [mm(pu, wu_bf[q][:, kc, :].rearrange("p (m c) -> p c m", c=4)[:, hc, :], h2T[:, kc, :], start=(kc == 0), stop=(kc == 7))
                      for kc in range(8)], reads=h2_keys + [("wu", q, 0), ("wu", q, 1)], writes=[ku])
            sgk = ("sgb", hc % 2)
            op("act", act(sgb[hc % 2], pg, AF.Sigmoid), reads=[kg], writes=[sgk])
            op("dve", tt(sgb[hc % 2], sgb[hc % 2], pg, ALU.mult), reads=[kg, sgk], writes=[sgk])
            op("dve", tt(hidT[:, hc, :], sgb[hc % 2], pu, ALU.mult), reads=[ku, sgk], writes=[("hidT", hc)])
        hid_keys = [("hidT", hc) for hc in range(4)]
        for j in range(4):
            yq = (b * 4 + j) % 2
            for half in range(2):
                pd, kd_ = next_mm()
                op("pe", [mm(pd, hidT[:, hc, j * 128:(j + 1) * 128], wd_bf[q][:, hc, half * 512:(half + 1) * 512],
                             start=(hc == 0), stop=(hc == 3)) for hc in range(4)],
                   reads=hid_keys + [("wd", q, 0), ("wd", q, 1)], writes=[kd_])
                wtok = idx_sb[q].bitcast(F32)[:, 2 * j + 1:2 * j + 2]
                op("act", act(ysb[yq][:, half * 512:(half + 1) * 512], pd, AF.Identity, scale=wtok),
                   reads=[kd_, ("idx", q, j)], writes=[("ysb", yq)])
            op("sp", dma(ybuf_d[b * BLK + j * 128:b * BLK + (j + 1) * 128, :], ysb[yq]),
               reads=[("ysb", yq)], writes=["ybuf"], dma=True)

    sc.barrier()
    ar.reset(m3)
    F = [dict(xm=ar.f32(D), y0=ar.f32(D), y1=ar.f32(D), jk=ar.bf(D), st=ar.f32(4)) for _ in range(2)]
    for st in range(NST):
        f = F[st % 2]
        fk = ("F", st % 2)
        op("sp", dma(f["xm"], xmid_d[st * 128:(st + 1) * 128, :]), reads=["xmid_scr"], writes=[fk + ("xm",)], dma=True)
        for k in range(2):
            op("pool", lambda e, f=f, k=k, st=st: e.indirect_dma_start(
                out=f["y%d" % k], out_offset=None, in_=ybuf_d[:, :],
                in_offset=bass.IndirectOffsetOnAxis(ap=slot_c[:, st * 2 + k:st * 2 + k + 1], axis=0)),
               reads=["ybuf", "slot_c"], writes=[fk + ("y%d" % k,)], dma=True)
        op("pool", tt(f["y0"], f["y0"], f["y1"], ALU.add), reads=[fk + ("y0",), fk + ("y1",)], writes=[fk + ("y0",)])
        op("dve", tt(f["xm"], f["xm"], f["y0"], ALU.add), reads=[fk + ("xm",), fk + ("y0",)], writes=[fk + ("xm",)])
        op("act", act(f["jk"], f["xm"], AF.Square, accum_out=f["st"][:, 0:1]), reads=[fk + ("xm",)], writes=[fk + ("st",), fk + ("jk",)])
        op("act", act(f["st"][:, 1:2], f["st"][:, 0:1], AF.Sqrt, bias=EPS, scale=1.0 / D), reads=[fk + ("st",)], writes=[fk + ("st",)])
        op("dve", lambda e, f=f: e.reciprocal(out=f["st"][:, 2:3], in_=f["st"][:, 1:2]), reads=[fk + ("st",)], writes=[fk + ("st",)])
        op("dve", stt(f["y1"], f["xm"], f["st"][:, 2:3], gf_bc, ALU.mult, ALU.mult),
           reads=[fk + ("xm",), fk + ("st",), "gf_bc"], writes=[fk + ("y1",)])
        op("sp", dma(out_d[st * 128:(st + 1) * 128, :], f["y1"]), reads=[fk + ("y1",)], writes=["out"], dma=True)

    sc.emit()
    return nc


def host_consts(S):
    NST = S // 128
    cst = np.zeros((128, CS_N), np.float32)
    cst[:, CS_ID:CS_ID + 128] = np.eye(128, dtype=np.float32)
    p = np.arange(128)
    cst[:, CS_U:CS_U + 128] = (p[:, None] < p[None, :]).astype(np.float32)
    cst[:, CS_CM:CS_CM + 128] = (p[:, None] <= p[None, :]).astype(np.float32)
    cst[:, CS_HM] = (p < 64).astype(np.float32)
    cst[:, CS_HM + 1] = (p >= 64).astype(np.float32)
    cst[:, CS_TH:CS_TH + NTH] = (np.arange(NTH) * BLK).astype(np.float32)[None, :]
    rm = np.ones((512,), np.float32)
    rm[::128] = 0.0
    cst[:, CS_RM:CS_RM + 512] = rm[None, :]
    cst[:, CS_IO:CS_IO + 32] = np.arange(32, dtype=np.float32)[None, :]
    cst[:, CS_B5] = (p * BLK).astype(np.float32)
    tokid = (np.arange(NST)[None, :] * 128 + p[:, None]).astype(np.int32)
    return cst, tokid


def fm(v, n):
    return np.ascontiguousarray(np.asarray(v, np.float32).reshape(n, 128).T)


def host_inputs(inp, b, S):
    L = 0
    tab = np.zeros((128, TB_N), np.float32)
    tab[:, TB_BADA:TB_BADA + 48] = fm(inp["b_ada"][L], 48)
    tab[:, TB_GMIX:TB_GMIX + 8] = fm(inp["g_mix"][L], 8)
    tab[:, TB_GFFN:TB_GFFN + 8] = fm(inp["g_ffn"][L], 8)
    cw = np.asarray(inp["conv_w"][L], np.float32)
    for c in range(4):
        tab[:, TB_CW + c * 4:TB_CW + c * 4 + 4] = cw[:, c * 128:(c + 1) * 128].T
    tab[:, TB_CB:TB_CB + 4] = fm(inp["conv_b"][L], 4)
    tab[:, TB_BR:TB_BR + 4] = fm(inp["lru_br"][L], 4)
    tab[:, TB_BI:TB_BI + 4] = fm(inp["lru_bi"][L], 4)
    tab[:, TB_LAM:TB_LAM + 4] = fm(inp["lru_lambda"][L], 4)
    tab[:, TB_BA:TB_BA + 2] = fm(inp["gla_ba"][L], 2)
    tab[:, TB_GN] = np.asarray(inp["gla_gnorm"][L], np.float32)
    ba = np.asarray(inp["b_ada"][L], np.float32)
    tab[:, TB_BADAP:TB_BADAP + 8] = ba[3 * D:4 * D].reshape(128, 8)
    tab[:, TB_BADAP + 8:TB_BADAP + 16] = ba[4 * D:5 * D].reshape(128, 8)
    tab[:, TB_GFFNP:TB_GFFNP + 8] = np.asarray(inp["g_ffn"][L], np.float32).reshape(128, 8)

    def bd(w):
        w = np.asarray(w, np.float32)
        o = np.zeros((128, 4, 128), np.float32)
        for c in range(4):
            for hh in range(2):
                o[hh * 64:(hh + 1) * 64, c, hh * 64:(hh + 1) * 64] = w[2 * c + hh]
        return o.reshape(128, 512)

    cst, tokid = host_consts(S)
    m = {
        "x": np.ascontiguousarray(np.asarray(inp["x"][b], np.float32)),
        "cT": fm(inp["c"][b], 8),
        "w_ada": np.ascontiguousarray(np.asarray(inp["w_ada"][L], np.float32)),
        "tab": tab,
        "cst": cst,
        "tokid": tokid,
        "gf_bc": np.ascontiguousarray(np.broadcast_to(np.asarray(inp["g_final"], np.float32)[None, :], (128, D))),
        "br_bc": np.ascontiguousarray(np.broadcast_to(
            np.concatenate([np.asarray(inp["b_coarse"][L], np.float32),
                            np.asarray(inp["b_fine"][L], np.float32)])[None, :], (128, 36))),
        "w_in": np.ascontiguousarray(np.asarray(inp["w_in"][L], np.float32)),
        "w_out": np.ascontiguousarray(np.asarray(inp["w_out"][L], np.float32)),
        "bd_r": bd(inp["lru_wr"][L]),
        "bd_i": bd(inp["lru_wi"][L]),
        "wa2": np.ascontiguousarray(np.asarray(inp["gla_wa2"][L], np.float32)),
        "w_r": np.ascontiguousarray(np.concatenate([np.asarray(inp["w_coarse"][L], np.float32),
                                                    np.asarray(inp["w_fine"][L], np.float32)], axis=1)),
        "w_gate": np.ascontiguousarray(np.asarray(inp["w_gate"][L], np.float32)),
        "w_up": np.ascontiguousarray(np.asarray(inp["w_up"][L], np.float32)),
        "w_down": np.ascontiguousarray(np.asarray(inp["w_down"][L], np.float32)),
    }
    return m


def kernel(**inputs):
    B, S = inputs["x"].shape[0], inputs["x"].shape[1]
    nc = build(S)
    in_maps = [host_inputs(inputs, b, S) for b in range(B)]
    res = run_bass_kernel_spmd(nc, in_maps, core_ids=list(range(B)))
    return np.stack([np.asarray(r["out"]) for r in res.results], axis=0).astype(np.float32)
```

```python
import numpy as np
import concourse.bass as bass
import concourse.mybir as mybir
from concourse.bass_utils import run_bass_kernel_spmd

F32 = mybir.dt.float32
BF16 = mybir.dt.bfloat16
I32 = mybir.dt.int32
U32 = mybir.dt.uint32
AF = mybir.ActivationFunctionType
ALU = mybir.AluOpType
AX = mybir.AxisListType

D = 1024
NPROJ = 2576
NE = 32
CAP = 8192 + 512
EPS = 1e-6
T = 256
BLK = 512


class Sched:
    EPOCH = 6000

    def __init__(self, nc, same_engine_sync=True):
        self.nc = nc
        self.ops = []
        self.lw = {}
        self.rd = {}
        self.same = same_engine_sync
        self.stack = []

    def rec_begin(self):
        self.stack.append([])

    def rec_end(self):
        return self.stack.pop()

    def play(self, lst):
        for a in lst:
            self.op(*a)

    @staticmethod
    def merge(lists):
        lists = [l for l in lists if l]
        out = []
        n = [len(l) for l in lists]
        pos = [0] * len(lists)
        total = sum(n)
        for _ in range(total):
            bi, bv = -1, 2.0
            for i in range(len(lists)):
                if pos[i] < n[i]:
                    v = pos[i] / n[i]
                    if v < bv:
                        bi, bv = i, v
            out.append(lists[bi][pos[bi]])
            pos[bi] += 1
        return out

    def op(self, eng, fns, reads=(), writes=(), dma=False):
        if self.stack:
            self.stack[-1].append((eng, fns, list(reads), list(writes), dma))
            return
        if callable(fns):
            fns = [fns]
        reads, writes = list(reads), list(writes)
        for k in reads:
            if isinstance(k, str) and k.startswith("ps") and k[2:].isdigit() and k not in writes:
                writes.append(k)
        idx = len(self.ops)
        deps = set()
        for k in reads:
            if k in self.lw:
                deps.add(self.lw[k])
        for k in writes:
            if k in self.lw:
                deps.add(self.lw[k])
            r = self.rd.get(k)
            if r:
                deps.update(r[0].values())
                deps.update(r[1])
        self.ops.append(dict(eng=eng, fns=fns, deps=deps, dma=dma, tag=(list(reads), list(writes))))
        for k in writes:
            self.lw[k] = idx
            self.rd[k] = ({}, [])
        for k in reads:
            r = self.rd.setdefault(k, ({}, []))
            if dma:
                r[1].append(idx)
            else:
                r[0][eng] = idx
        return idx

    def barrier(self):
        last = {}
        dmas = []
        for i, o in enumerate(self.ops):
            if o["dma"]:
                dmas.append(i)
            else:
                last[o["eng"]] = i
        deps = set(last.values()) | set(dmas)
        for eng in ("pe", "act", "dve", "pool", "sp"):
            idx = len(self.ops)
            self.ops.append(dict(eng=eng, fns=[lambda e: e.nop()], deps=set(deps), dma=False))
            last[eng] = idx
        self._pending_dma_done = True
        self.lw = {}
        self.rd = {}
        self._bar = dict(last)
        for eng, i in last.items():
            self.lw[("__bar__", eng)] = i

    def emit(self):
        import os
        lim = int(os.environ.get("OPLIMIT", "0"))
        print("total ops", len(self.ops))
        if lim:
            self.ops = self.ops[:lim]
            o = self.ops[-1]
            print("last op", o["eng"], o.get("tag"))
        nc = self.nc
        engs = ("pe", "act", "dve", "pool", "sp")
        count = {e: 0 for e in engs}
        prog = {e: [] for e in engs}
        dma_pool = {}
        dma_rr = {e: 0 for e in engs}
        dma_val = {}
        NPOOL = {"sp": 10, "pool": 8, "act": 4, "dve": 2, "pe": 2}
        tokens = []
        pre_wait = []
        for o in self.ops:
            e = o["eng"]
            if o["dma"]:
                pl = dma_pool.setdefault(e, [])
                if len(pl) < NPOOL[e]:
                    s = nc.alloc_semaphore(name=f"dq_{e}_{len(pl)}")
                    pl.append(s)
                    dma_val[id(s)] = 0
                s = pl[dma_rr[e] % len(pl)] if len(pl) == NPOOL[e] else pl[-1]
                dma_rr[e] += 1
                prev = dma_val[id(s)]
                dma_val[id(s)] = prev + 16
                tokens.append((s, prev + 16))
                pre_wait.append((s, prev) if prev > 0 else None)
            else:
                c = count[e]
                ep = c // self.EPOCH
                while len(prog[e]) <= ep:
                    prog[e].append(nc.alloc_semaphore(name=f"pg_{e}_{len(prog[e])}"))
                tokens.append((prog[e][ep], c % self.EPOCH + 1))
                pre_wait.append(None)
                count[e] = c + 1
        waited = {e: {} for e in engs}
        streams = {e: [] for e in engs}
        for i, o in enumerate(self.ops):
            e = o["eng"]
            ws = []
            cand = []
            if pre_wait[i] is not None:
                cand.append(pre_wait[i])
            for d in sorted(o["deps"]):
                od = self.ops[d]
                if (not od["dma"]) and od["eng"] == e and (e == "pe" or not self.same):
                    continue
                cand.append(tokens[d])
            for (s, v) in cand:
                w = waited[e]
                if w.get(id(s), 0) >= v:
                    continue
                w[id(s)] = v
                ws.append((s, v))
            streams[e].append((ws, o["fns"], tokens[i], o["dma"]))
        final_waits = []
        for e, pl in dma_pool.items():
            for s in pl:
                if dma_val[id(s)] > 0:
                    final_waits.append((s, dma_val[id(s)]))
        for e in engs:
            if count[e] > 0:
                c = count[e] - 1
                final_waits.append((prog[e][c // self.EPOCH], c % self.EPOCH + 1))

        def run(engine, name):
            for ws, fns, tok, is_dma in streams[name]:
                for (s, v) in ws:
                    engine.wait_ge(s, v)
                ins = None
                for f in fns:
                    ins = f(engine)
                ins.then_inc(tok[0], 16 if is_dma else 1)

        with nc.Block() as block:
            @block.tensor
            def _(eng):
                run(eng, "pe")

            @block.scalar
            def _(eng):
                run(eng, "act")

            @block.vector
            def _(eng):
                run(eng, "dve")

            @block.gpsimd
            def _(eng):
                run(eng, "pool")

            @block.sync
            def _(eng):
                run(eng, "sp")
                for (s, v) in final_waits:
                    eng.wait_ge(s, v)


def act(out, in_, func, bias=None, scale=None, accum_out=None):
    def f(e):
        kw = {}
        if bias is not None:
            kw["bias"] = bias
        if scale is not None:
            kw["scale"] = scale
        if accum_out is not None:
            kw["accum_out"] = accum_out
        return e.activation(out=out, in_=in_, func=func, **kw)
    return f


def ts(out, in0, s1, op0, s2=None, op1=None, accum_out=None):
    def f(e):
        kw = {}
        if op1 is not None:
            kw["op1"] = op1
        if accum_out is not None:
            kw["accum_out"] = accum_out
        return e.tensor_scalar(out=out, in0=in0, scalar1=s1, scalar2=s2, op0=op0, **kw)
    return f


def tt(out, in0, in1, op):
    return lambda e: e.tensor_tensor(out=out, in0=in0, in1=in1, op=op)


def stt(out, in0, scalar, in1, op0, op1):
    return lambda e: e.scalar_tensor_tensor(out=out, in0=in0, scalar=scalar, in1=in1, op0=op0, op1=op1)


def mm(out, lhsT, rhs, start=True, stop=True):
    return lambda e: e.matmul(out, lhsT, rhs, start=start, stop=stop)


def tr(out, in_, ident):
    return lambda e: e.transpose(out, in_, ident)


def dma(out, in_, **kw):
    return lambda e: e.dma_start(out=out, in_=in_, **kw)


def cp(out, in_):
    return lambda e: e.tensor_copy(out=out, in_=in_)


def acp(out, in_):
    return lambda e: e.activation(out=out, in_=in_, func=AF.Copy)


def scan(out, d0, d1, init, op0, op1):
    return lambda e: e.tensor_tensor_scan(out=out, data0=d0, data1=d1, initial=init, op0=op0, op1=op1)


def red(out, in_, op):
    return lambda e: e.tensor_reduce(out=out, in_=in_, axis=AX.X, op=op)


def mset(ap, v):
    return lambda e: e.memset(ap, v)


TB_BADA = 0
TB_GMIX = 48
TB_GFFN = 56
TB_CW = 64
TB_CB = 80
TB_BR = 84
TB_BI = 88
TB_LAM = 92
TB_BA = 96
TB_GN = 98
TB_BADAP = 99
TB_GFFNP = 115
TB_N = 123

CS_ID = 0
CS_U = 128
CS_CM = 256
CS_RM = 384
CS_IO = 896
CS_B5 = 928
CS_HM = 929
CS_TH = 931
NTH = 17
CS_N = 948


class Arena:
    def __init__(self, ap, nwords):
        self.ap = ap
        self.n = nwords
        self.off = 0

    def f32(self, n):
        n = (n + 1) // 2 * 2
        assert self.off + n <= self.n, ("arena overflow", self.off, n, self.n)
        v = self.ap[:, self.off:self.off + n]
        self.off += n
        return v

    def bf(self, nel):
        w = (nel + 1) // 2
        w = (w + 1) // 2 * 2
        v = self.f32(w).bitcast(BF16)
        return v[:, 0:nel]

    def mark(self):
        return self.off

    def reset(self, m):
        self.off = m


def build(S, stop_after=None, dbg=False):
    NT = S // T
    NST = S // 128
    NB = (2 * S) // BLK + NE
    NROWS = NE * CAP + BLK
    NULLSTART = NE * CAP
    nc = bass.Bass("TRN2", target_bir_lowering=False)

    def din(name, shape, dt=F32):
        return nc.dram_tensor(name, shape, dt, kind="ExternalInput").ap()

    x_d = din("x", [S, D])
    cT_d = din("cT", [128, 8])
    wada_d = din("w_ada", [D, 6 * D])
    tab_d = din("tab", [128, TB_N])
    cst_d = din("cst", [128, CS_N])
    tok_d = din("tokid", [128, NST], I32)
    gf_d = din("gf_bc", [128, D])
    brb_d = din("br_bc", [128, 36])
    win_d = din("w_in", [D, NPROJ])
    wout_d = din("w_out", [D, D])
    bdr_d = din("bd_r", [128, 512])
    bdi_d = din("bd_i", [128, 512])
    wa2_d = din("wa2", [16, 256])
    wr_d = din("w_r", [D, 36])
    wg_d = din("w_gate", [NE, D, 512])
    wu_d = din("w_up", [NE, D, 512])
    wd_d = din("w_down", [NE, 512, D])
    out_d = nc.dram_tensor("out", [S, D], F32, kind="ExternalOutput").ap()

    xmid_d = nc.dram_tensor("xmid_scr", [S, D], F32, kind="Internal").ap()
    xn2_d = nc.dram_tensor("xn2_scr", [S, D], BF16, kind="Internal").ap()
    sidx_d = nc.dram_tensor("sidx_scr", [NROWS, 2], I32, kind="Internal").ap()
    ybuf_d = nc.dram_tensor("ybuf_scr", [NB * BLK, D], F32, kind="Internal").ap()
    dbg_d = None
    if dbg:
        dbg_d = nc.dram_tensor("dbg", [S, D], F32, kind="ExternalOutput").ap()
        dbg2_d = nc.dram_tensor("dbg2", [128, 4096], F32, kind="ExternalOutput").ap()

    sc = Sched(nc)
    op = sc.op

    def sb(name, shape, dt=F32):
        return nc.alloc_sbuf_tensor(name + "_sb", shape, dt)[:]

    tab = sb("tab", [128, TB_N])
    cst = sb("cst", [128, CS_N])
    tokid = sb("tokid", [128, NST], I32)
    gf_bc = sb("gf_bc", [128, D])
    gt1_bc = sb("gt1_bc", [128, D])
    gt2_bc = sb("gt2_bc", [128, D])
    brb = sb("brb", [128, 36])
    modT = sb("modT", [128, 48])
    scale1 = sb("scale1", [128, 8])
    scale2 = sb("scale2", [128, 8])
    scale2p = sb("scale2p", [128, 8])
    modP = sb("modP", [128, 16])
    widx = sb("widx", [128, 128], I32)
    sidx4 = sb("sidx4", [128, 4 * 128], I32)
    ltab = sb("ltab", [128, 16])
    ident_bf = sb("ident_bf", [128, 128], BF16)
    ones_bf = sb("ones_bf", [128, 128], BF16)
    ones_f = sb("ones_f", [128, 128])
    ohs = sb("ohs", [128, NST * 2 * 32], BF16)
    pos_all = sb("pos_all", [128, NST * 2])
    eid_all = sb("eid_all", [128, NST * 2])
    wts_all = sb("wts_all", [128, NST * 2])
    Ocum = sb("Ocum", [128, 32])
    pay_i = sb("pay", [128, NST * 4], I32)
    pay_f = pay_i.bitcast(F32)
    slot_u = sb("slot_u", [128, NST * 2], I32)
    slot_c = sb("slot_c", [128, NST * 2], I32)
    tbl_i = sb("tbl_i", [1, 256], I32)
    idx_sb = [sb(f"idx_sb{q}", [128, 8], I32) for q in range(2)]
    ident_f = cst[:, CS_ID:CS_ID + 128]
    Umat = cst[:, CS_U:CS_U + 128]
    cmask = cst[:, CS_CM:CS_CM + 128]
    hmask = cst[:, CS_HM:CS_HM + 2]
    rmask = cst[:, CS_RM:CS_RM + 512]
    iota32 = cst[:, CS_IO:CS_IO + 32]
    blk512 = cst[:, CS_B5:CS_B5 + 1]

    rem = nc.sbuf_bytes_remaining
    rem = rem() if callable(rem) else rem
    ARW = (int(rem) // 4) - 64
    ARW = ARW // 2 * 2
    arena_ap = sb("arena", [128, ARW])
    ar = Arena(arena_ap, ARW)

    ps = [nc.alloc_psum_tensor(f"ps{i}", [128, 512], F32)[:] for i in range(8)]

    m0 = ar.mark()
    op("sp", dma(tab, tab_d), writes=["tab"], dma=True)
    op("sp", dma(cst, cst_d), writes=["cst"], dma=True)
    cT = ar.f32(8)
    op("sp", dma(cT, cT_d), writes=["cT"], dma=True)
    op("sp", dma(tokid, tok_d), writes=["tokid"], dma=True)
    op("sp", dma(gf_bc, gf_d), writes=["gf_bc"], dma=True)
    op("sp", dma(brb, brb_d), writes=["brb"], dma=True)
    sgc = ar.f32(8)
    scT = ar.f32(8)
    op("act", act(sgc, cT, AF.Sigmoid), reads=["cT"], writes=["sgc"])
    op("dve", tt(scT, cT, sgc, ALU.mult), reads=["cT", "sgc"], writes=["scT"])
    op("dve", mset(ones_f, 1.0), writes=["ones_f"])
    op("dve", mset(ones_bf, 1.0), writes=["ones_bf"])
    op("dve", cp(ident_bf, ident_f), reads=["cst"], writes=["ident_bf"])

    zt = ar.f32(2048)
    op("pool", mset(zt, 0.0), writes=["zt"])
    zt_i = zt.bitcast(I32)
    rows_per = 128 * 1024
    r0 = 0
    while r0 < NROWS:
        n = min(rows_per, NROWS - r0)
        np_ = n // 1024
        if np_ > 0:
            op("sp", dma(sidx_d[r0:r0 + np_ * 1024, :].rearrange("(p r) c -> p (r c)", p=np_),
                         zt_i[0:np_, :]), reads=["zt"], writes=["sidx_scr"], dma=True)
            r0 += np_ * 1024
        else:
            op("sp", dma(sidx_d[r0:r0 + n, :].rearrange("(p r) c -> p (r c)", p=1),
                         zt_i[0:1, 0:2 * n]), reads=["zt"], writes=["sidx_scr"], dma=True)
            r0 += n

    wst = [ar.f32(8 * 1024).rearrange("p (k f) -> p k f", k=8) for _ in range(2)]
    for blk in range(6):
        b = blk % 2
        op("sp", dma(wst[b], wada_d[:, blk * 1024:(blk + 1) * 1024].rearrange("(k p) f -> p k f", p=128)),
           writes=[("wst", b)], dma=True)
        fns = []
        for fj in range(8):
            for kc in range(8):
                fns.append(mm(ps[0][:, blk * 8 + fj: blk * 8 + fj + 1],
                              wst[b][:, kc, fj * 128:(fj + 1) * 128], scT[:, kc:kc + 1],
                              start=(kc == 0), stop=(kc == 7)))
        if blk in (3, 4):
            for kk in range(8):
                for kc in range(8):
                    fns.append(mm(ps[0][:, 48 + (blk - 3) * 8 + kk: 48 + (blk - 3) * 8 + kk + 1],
                                  wst[b][:, kc, :].rearrange("p (m kk) -> p kk m", kk=8)[:, kk, :],
                                  scT[:, kc:kc + 1], start=(kc == 0), stop=(kc == 7)))
        op("pe", fns, reads=[("wst", b), "scT"], writes=["ps0"])
    op("dve", tt(modT, ps[0][:, 0:48], tab[:, TB_BADA:TB_BADA + 48], ALU.add),
       reads=["ps0", "tab"], writes=["modT"])
    op("dve", tt(modP, ps[0][:, 48:64], tab[:, TB_BADAP:TB_BADAP + 16], ALU.add),
       reads=["ps0", "tab"], writes=["modP"])
    op("dve", stt(scale2p, modP[:, 8:16], 1.0, tab[:, TB_GFFNP:TB_GFFNP + 8], ALU.add, ALU.mult),
       reads=["modP", "tab"], writes=["scale2p"])
    op("dve", stt(scale1, modT[:, 8:16], 1.0, tab[:, TB_GMIX:TB_GMIX + 8], ALU.add, ALU.mult),
       reads=["modT", "tab"], writes=["scale1"])
    op("dve", stt(scale2, modT[:, 32:40], 1.0, tab[:, TB_GFFN:TB_GFFN + 8], ALU.add, ALU.mult),
       reads=["modT", "tab"], writes=["scale2"])
    bias1 = modT[:, 0:8]
    bias2 = modT[:, 24:32]
    bias2p = modP[:, 0:8]
    Gt = ar.f32(8 * 128).rearrange("p (k f) -> p k f", k=8)
    for (col0, dst, nm) in ((16, gt1_bc, "gt1_bc"), (40, gt2_bc, "gt2_bc")):
        for kc in range(8):
            op("dve", ts(Gt[:, kc, :], ones_f, modT[:, col0 + kc:col0 + kc + 1], ALU.mult),
               reads=["modT", "ones_f"], writes=[("Gt", kc)])
        for half in range(2):
            fns = [mm(ps[1 + half][:, k4 * 128:(k4 + 1) * 128], Gt[:, half * 4 + k4, :], ident_f)
                   for k4 in range(4)]
            op("pe", fns, reads=[("Gt", half * 4 + k4) for k4 in range(4)] + ["cst"],
               writes=[f"ps{1 + half}"])
            op("act", acp(dst[:, half * 512:(half + 1) * 512], ps[1 + half]),
               reads=[f"ps{1 + half}"], writes=[nm])
    t4 = ar.f32(4)
    op("act", act(t4, tab[:, TB_LAM:TB_LAM + 4], AF.Exp, scale=-1.0), reads=["tab"], writes=["t4"])
    op("act", act(t4, t4, AF.Ln, bias=1.0), reads=["t4"], writes=["t4"])
    op("dve", ts(ltab[:, 0:4], t4, -8.0, ALU.mult), reads=["t4"], writes=["ltab"])
    op("dve", ts(ltab[:, 4:8], t4, -16.0, ALU.mult), reads=["t4"], writes=["ltab"])
    op("dve", ts(ltab[:, 8:10], tab[:, TB_BA:TB_BA + 2], -1.0, ALU.mult), reads=["tab"], writes=["ltab"])
    cl = ltab[:, 0:4]
    c2l = ltab[:, 4:8]
    nba = ltab[:, 8:10]
    m_setup_tmp = ar.mark()

    ar.reset(m0)
    w_in_bf = ar.bf(8 * NPROJ).rearrange("p (k f) -> p k f", k=8)
    w_out_s = ar.bf(8 * D).rearrange("p (k f) -> p k f", k=8)
    bdr_bf = ar.bf(512).rearrange("p (c m) -> p c m", c=4)
    bdi_bf = ar.bf(512).rearrange("p (c m) -> p c m", c=4)
    wa2_bf = ar.bf(256)
    wr_bf = ar.bf(8 * 36).rearrange("p (k n) -> p k n", k=8)
    m_w = ar.mark()
    sc.barrier()
    stg = [ar.f32(NPROJ) for _ in range(2)]
    cast_engs = ["dve", "pool", "act"]
    ci = 0
    for kc in range(8):
        b = kc % 2
        op("sp", dma(stg[b], win_d[kc * 128:(kc + 1) * 128, :]), writes=[("stg", b)], dma=True)
        e = cast_engs[ci % 3]
        ci += 1
        op(e, (acp if e == "act" else cp)(w_in_bf[:, kc, :], stg[b]), reads=[("stg", b)], writes=["w_in_bf"])
    for kc in range(8):
        b = kc % 2
        op("sp", dma(stg[b][:, 0:D], wout_d[kc * 128:(kc + 1) * 128, :]), writes=[("stg", b)], dma=True)
        op("dve", tt(w_out_s[:, kc, :], stg[b][:, 0:D], gt1_bc, ALU.mult),
           reads=[("stg", b), "gt1_bc"], writes=["w_out_s"])
    for (src, dst, nm) in ((bdr_d, bdr_bf, "bdr"), (bdi_d, bdi_bf, "bdi")):
        op("sp", dma(stg[0][:, 0:512], src), writes=[("stg", 0)], dma=True)
        op("dve", cp(dst.rearrange("p c m -> p (c m)"), stg[0][:, 0:512]), reads=[("stg", 0)], writes=[nm])
    op("sp", dma(stg[1][0:16, 0:256], wa2_d), writes=[("stg", 1)], dma=True)
    op("dve", cp(wa2_bf[0:16, :], stg[1][0:16, 0:256]), reads=[("stg", 1)], writes=["wa2_bf"])
    op("sp", dma(stg[0][:, 0:288].rearrange("p (k n) -> p k n", k=8),
                 wr_d.rearrange("(k p) n -> p k n", p=128)), writes=[("stg", 0)], dma=True)
    op("dve", cp(wr_bf.rearrange("p k n -> p (k n)"), stg[0][:, 0:288]), reads=[("stg", 0)], writes=["wr_bf"])
    sc.barrier()
    ar.reset(m_w)

    if stop_after == "setup":
        op("sp", dma(dbg2_d[:, 0:48], modT), reads=["modT"], writes=["dbg2"], dma=True)
        op("sp", dma(dbg2_d[:, 1024:2048], gt1_bc), reads=["gt1_bc"], writes=["dbg2"], dma=True)
        op("sp", dma(dbg2_d[:, 64:80], ltab), reads=["ltab"], writes=["dbg2"], dma=True)
        sc.emit()
        return nc
    NJ = T // 128
    NCH = T // 64
    xs = [ar.f32(NJ * D).rearrange("p (j d) -> p j d", j=NJ) for _ in range(2)]
    xn = ar.bf(NJ * D).rearrange("p (j d) -> p j d", j=NJ)
    hT = ar.bf(8 * T).rearrange("p (k t) -> p k t", k=8)
    xc = ar.f32(4 * (T + 4)).rearrange("p (c t) -> p c t", c=4)
    yb = ar.f32(4 * T).rearrange("p (c t) -> p c t", c=4)
    qf = ar.f32(2 * T).rearrange("p (c t) -> p c t", c=2)
    kf = ar.f32(2 * T).rearrange("p (c t) -> p c t", c=2)
    gfb = ar.f32(4 * T).rearrange("p (c t) -> p c t", c=4)
    glT = ar.bf(T)
    vb = ar.bf(NJ * 512).rearrange("p (j e) -> p j e", j=NJ)
    catT = ar.bf(8 * T).rearrange("p (k t) -> p k t", k=8)
    L = []
    for _ in range(2):
        L.append(dict(cv=ar.f32(T), cvb=ar.bf(T), r=ar.f32(T), i=ar.f32(T), a=ar.f32(T),
                      s=ar.f32(T), h=ar.f32(T), t1=ar.f32(T), t2=ar.f32(T)))
    hprev = ar.f32(4)
    lf = ar.f32(2 * T).rearrange("p (c t) -> p c t", c=2)
    cs = ar.f32(2 * T).rearrange("p (c t) -> p c t", c=2)
    eb = ar.f32(2 * T).rearrange("p (c t) -> p c t", c=2)
    enb = ar.f32(2 * T).rearrange("p (c t) -> p c t", c=2)
    dec = ar.f32(2 * NCH).rearrange("p (c n) -> p c n", c=2)
    qd = ar.bf(2 * T).rearrange("p (c t) -> p c t", c=2)
    kd = ar.bf(2 * T).rearrange("p (c t) -> p c t", c=2)
    ke = ar.bf(2 * T).rearrange("p (c t) -> p c t", c=2)
    keT = ar.bf(NJ * 256).rearrange("p (j f) -> p j f", j=NJ)
    scT_sb = ar.bf(512)
    Sf = ar.f32(256).rearrange("p (c e) -> p c e", c=2)
    Sb = [ar.bf(256).rearrange("p (c e) -> p c e", c=2) for _ in range(2)]
    osq = ar.bf(T)
    sd = ar.f32(T)
    rs = ar.f32(T)
    sg = ar.f32(T)
    gs = ar.f32(T)
    to = ar.f32(T)
    stat = ar.f32(16)
    lg = ar.f32(NJ * 36).rearrange("p (j n) -> p j n", j=NJ)
    rt = ar.f32(NJ * 64).rearrange("p (j n) -> p j n", j=NJ)
    mf = ar.f32(NJ * 32).rearrange("p (j n) -> p j n", j=NJ)
    m8 = ar.f32(NJ * 8).rearrange("p (j n) -> p j n", j=NJ)
    oh = ar.f32(NJ * 64).rearrange("p (j n) -> p j n", j=NJ)
    Ot = ar.f32(NJ * 32).rearrange("p (j n) -> p j n", j=NJ)
    tmp32 = ar.f32(NJ * 64).rearrange("p (j n) -> p j n", j=NJ)
    print("arena used (words):", ar.off, "of", ARW)

    for c in range(4):
        op("pool", mset(xc[:, c, 0:4], 0.0), writes=[("xc", c)])
    op("pool", mset(hprev, 0.0), writes=["hprev"])
    op("pool", mset(Sf.rearrange("p c e -> p (c e)"), 0.0), writes=["Sf"])
    op("pool", mset(Sb[0].rearrange("p c e -> p (c e)"), 0.0), writes=[("Sb", 0)])
    op("pool", mset(Sb[1].rearrange("p c e -> p (c e)"), 0.0), writes=[("Sb", 1)])
    op("pool", mset(Ocum, 0.0), writes=["Ocum"])

    mmslot = [0]

    mmgroup = ["all"]
    MMB = {"all": [2, 3, 4], "front": [2, 3], "back": [4], "lru0": [2], "lru1": [3], "gla": [5]}

    def next_mm():
        banks = MMB[mmgroup[0]]
        bk = banks[mmslot[0] % len(banks)]
        mmslot[0] += 1
        return ps[bk], f"ps{bk}"

    psTb = [ps[0].bitcast(BF16), ps[1].bitcast(BF16)]
    psS = ps[5]
    psKV = ps[5]
    psO = [ps[6], ps[7]]
    qz = [[ar.bf(T) for _ in range(2)] for _ in range(2)]
    osq2 = ar.bf(2 * T)
    sd2 = ar.f32(2 * T)
    rs2 = ar.f32(2 * T)
    sg2 = ar.f32(2 * T)
    gs2 = ar.f32(2 * T)
    to2 = ar.f32(2 * T)
    print("arena used (words):", ar.off, "of", ARW)

    xnB = ar.bf(NJ * D).rearrange("p (j d) -> p j d", j=NJ)
    hTB = ar.bf(8 * T).rearrange("p (k t) -> p k t", k=8)
    catT2 = [catT, ar.bf(8 * T).rearrange("p (k t) -> p k t", k=8)]
    statB = ar.f32(16)
    print("arena used (words):", ar.off, "of", ARW)

    def norm_stats(xsrc_key, xbuf, xn_, st_, tag):
        sk = "stat" + tag
        for j in range(NJ):
            op("act", act(xn_[:, j, :], xbuf[:, j, :], AF.Square, accum_out=st_[:, j:j + 1]),
               reads=[xsrc_key], writes=[("xn" + tag, j), sk])
        op("act", act(st_[:, 2:4], st_[:, 0:2], AF.Sqrt, bias=EPS, scale=1.0 / D),
           reads=[sk], writes=[sk])
        op("dve", lambda e: e.reciprocal(out=st_[:, 4:6], in_=st_[:, 2:4]),
           reads=[sk], writes=[sk])
        for j in range(NJ):
            op("dve", ts(xn_[:, j, :], xbuf[:, j, :], st_[:, 4 + j:5 + j], ALU.mult),
               reads=[xsrc_key, sk], writes=[("xn" + tag, j)])

    def transposes_to_hT(scale_t, bias_t, xn_, hT_, tag, tb):
        for hb_ in range(2):
            hb = tb
            fns = []
            for k4 in range(4):
                kc = hb_ * 4 + k4
                for j in range(NJ):
                    fns.append(tr(psTb[hb][:, k4 * T + j * 128: k4 * T + (j + 1) * 128],
                                  xn_[:, j, kc * 128:(kc + 1) * 128], ident_bf))
            op("pe", fns, reads=[("xn" + tag, j) for j in range(NJ)] + ["ident_bf"], writes=[f"ps{hb}"])
            for k4 in range(4):
                kc = hb_ * 4 + k4
                src = psTb[hb][:, k4 * T:(k4 + 1) * T]
                op("act", act(hT_[:, kc, :], src, AF.Identity, bias=bias_t[:, kc:kc + 1],
                              scale=scale_t[:, kc:kc + 1]),
                   reads=[f"ps{hb}", "scale1", "scale2", "modT"], writes=[("hT" + tag, kc)])

    hT_keys = [("hTF", kc) for kc in range(8)]
    hTB_keys = [("hTB", kc) for kc in range(8)]
    ev_i = [0]

    def evac(dst, src, skey, dkey):
        e = "act" if (ev_i[0] // 2) % 2 == 0 else "dve"
        ev_i[0] += 1
        op(e, (acp if e == "act" else cp)(dst, src), reads=[skey], writes=[dkey])

    def load_x(t_):
        op("sp", dma(xs[t_ % 2], x_d[t_ * T:(t_ + 1) * T, :].rearrange("(j p) d -> p j d", p=128)),
           writes=[("xs", t_ % 2)], dma=True)

    def front(it):
        xb = it % 2
        X = xs[xb]
        xk = ("xs", xb)
        catT = catT2[it % 2]
        ctag = "catT%d" % (it % 2)
        norm_stats(xk, X, xn, stat, "F")
        mmgroup[0] = "front"
        transposes_to_hT(scale1, bias1, xn, hT, "F", 0)

        fm_list = []
        for c in range(4):
            fm_list.append((c * 128, xc[:, c, 4:4 + T], ("xc", c)))
        for c in range(4):
            fm_list.append((512 + c * 128, yb[:, c, :], ("yb", c)))
        for i in range(2):
            fm_list.append((1024 + i * 128, qf[:, i, :], ("qf", i)))
        for i in range(2):
            fm_list.append((1280 + i * 128, kf[:, i, :], ("kf", i)))
        for h in range(4):
            fm_list.append((2048 + h * 128, gfb[:, h, :], ("gf", h)))
        for pi_ in range(0, 16, 2):
            pso, key = next_mm()
            fns = []
            for u in range(2):
                col0 = fm_list[pi_ + u][0]
                fns += [mm(pso[:, u * T:(u + 1) * T], w_in_bf[:, kc, col0:col0 + 128], hT[:, kc, :],
                           start=(kc == 0), stop=(kc == 7)) for kc in range(8)]
            op("pe", fns, reads=hT_keys + ["w_in_bf"], writes=[key])
            for u in range(2):
                _, dst, dkey = fm_list[pi_ + u]
                evac(dst, pso[:, u * T:(u + 1) * T], key, dkey)
        pso, key = next_mm()
        fns = [mm(pso[0:16, 0:T], w_in_bf[:, kc, 2560:2576], hT[:, kc, :], start=(kc == 0), stop=(kc == 7))
               for kc in range(8)]
        op("pe", fns, reads=hT_keys + ["w_in_bf"], writes=[key])
        evac(glT[0:16, :], pso[0:16, 0:T], key, "glT")
        ev_i[0] += 1
        for j in range(NJ):
            pso, key = next_mm()
            fns = [mm(pso, hT[:, kc, j * 128:(j + 1) * 128], w_in_bf[:, kc, 1536:2048],
                      start=(kc == 0), stop=(kc == 7)) for kc in range(8)]
            op("pe", fns, reads=hT_keys + ["w_in_bf"], writes=[key])
            evac(vb[:, j, :], pso, key, ("vb", j))
            ev_i[0] += 1

        lru_lists = {0: [], 1: []}
        for c in range(4):
            mmgroup[0] = "lru%d" % (c % 2)
            sc.rec_begin()
            B = L[c % 2]
            lk = ("L", c % 2)
            cw = tab[:, TB_CW + c * 4:TB_CW + c * 4 + 4]
            op("dve", ts(B["cv"], xc[:, c, 4:4 + T], cw[:, 3:4], ALU.mult, tab[:, TB_CB + c:TB_CB + c + 1], ALU.add),
               reads=[("xc", c), "tab"], writes=[lk + ("cv",)])
            for k in range(3):
                op("dve", stt(B["cv"], xc[:, c, 1 + k:1 + k + T], cw[:, k:k + 1], B["cv"], ALU.mult, ALU.add),
                   reads=[("xc", c), "tab", lk + ("cv",)], writes=[lk + ("cv",)])
            op("pool", cp(B["cvb"], B["cv"]), reads=[lk + ("cv",)], writes=[lk + ("cvb",)])
            op("pool", cp(xc[:, c, 0:4], xc[:, c, T:T + 4]), reads=[("xc", c)], writes=[("xc", c)])
            pg, kg = next_mm()
            op("pe", [mm(pg[:, 0:T], bdr_bf[:, c, :], B["cvb"]), mm(pg[:, T:2 * T], bdi_bf[:, c, :], B["cvb"])],
               reads=[lk + ("cvb",), "bdr", "bdi"], writes=[kg])
            op("act", act(B["r"], pg[:, 0:T], AF.Sigmoid, bias=tab[:, TB_BR + c:TB_BR + c + 1]),
               reads=[kg, "tab"], writes=[lk + ("r",)])
            op("act", act(B["i"], pg[:, T:2 * T], AF.Sigmoid, bias=tab[:, TB_BI + c:TB_BI + c + 1]),
               reads=[kg, "tab"], writes=[lk + ("i",)])
            op("act", act(B["a"], B["r"], AF.Exp, scale=cl[:, c:c + 1]),
               reads=[lk + ("r",), "ltab"], writes=[lk + ("a",)])
            op("act", act(B["s"], B["r"], AF.Exp, scale=c2l[:, c:c + 1]),
               reads=[lk + ("r",), "ltab"], writes=[lk + ("s",)])
            op("act", act(B["s"], B["s"], AF.Sqrt, bias=1.0, scale=-1.0),
               reads=[lk + ("s",)], writes=[lk + ("s",)])
            op("pool", tt(B["i"], B["i"], B["cv"], ALU.mult), reads=[lk + ("i",), lk + ("cv",)], writes=[lk + ("i",)])
            op("pool", tt(B["i"], B["i"], B["s"], ALU.mult), reads=[lk + ("i",), lk + ("s",)], writes=[lk + ("i",)])
            op("dve", scan(B["h"], B["a"], B["i"], hprev[:, c:c + 1], ALU.mult, ALU.add),
               reads=[lk + ("a",), lk + ("i",), "hprev"], writes=[lk + ("h",)])
            op("pool", cp(hprev[:, c:c + 1], B["h"][:, T - 1:T]), reads=[lk + ("h",)], writes=["hprev"])
            Y = yb[:, c, :]
            op("pool", tt(B["t1"], Y, Y, ALU.mult), reads=[("yb", c)], writes=[lk + ("t1",)])
            op("pool", ts(B["t1"], B["t1"], 0.044715, ALU.mult, 1.0, ALU.add),
               reads=[lk + ("t1",)], writes=[lk + ("t1",)])
            op("pool", tt(B["t1"], B["t1"], Y, ALU.mult), reads=[lk + ("t1",), ("yb", c)], writes=[lk + ("t1",)])
            op("act", act(B["t2"], B["t1"], AF.Sigmoid, scale=1.5957691216057308),
               reads=[lk + ("t1",)], writes=[lk + ("t2",)])
            op("pool", tt(B["t2"], B["t2"], Y, ALU.mult), reads=[lk + ("t2",), ("yb", c)], writes=[lk + ("t2",)])
            op("dve", tt(catT[:, c, :], B["h"], B["t2"], ALU.mult),
               reads=[lk + ("h",), lk + ("t2",)], writes=[(ctag, c)])
            lru_lists[c % 2] += sc.rec_end()
        mmgroup[0] = "gla"
        sc.rec_begin()

        pz, kz = next_mm()
        op("pe", [mm(pz[:, i * T:(i + 1) * T], wa2_bf[0:16, i * 128:(i + 1) * 128], glT[0:16, :]) for i in range(2)],
           reads=["glT", "wa2_bf"], writes=[kz])
        for i in range(2):
            op("act", act(lf[:, i, :], pz[:, i * T:(i + 1) * T], AF.Exp, bias=nba[:, i:i + 1], scale=-1.0),
               reads=[kz, "ltab"], writes=[("lf", i)])
            op("act", act(lf[:, i, :], lf[:, i, :], AF.Ln, bias=1.0), reads=[("lf", i)], writes=[("lf", i)])
            op("dve", scan(cs[:, i, :], rmask[:, 0:T], lf[:, i, :], 0.0, ALU.mult, ALU.add),
               reads=[("lf", i), "cst"], writes=[("cs", i)])
            op("act", act(eb[:, i, :], cs[:, i, :], AF.Exp, scale=-1.0 / 16), reads=[("cs", i)], writes=[("eb", i)])
            op("act", act(enb[:, i, :], cs[:, i, :], AF.Exp, scale=1.0 / 16), reads=[("cs", i)], writes=[("enb", i)])
            op("act", act(dec[:, i, 0:NJ], cs[:, i, :].rearrange("p (n t) -> p n t", t=128)[:, :, 127],
                          AF.Exp, scale=-1.0 / 16), reads=[("cs", i)], writes=[("dec", i)])
            op("dve", stt(qd[:, i, :], qf[:, i, :], 0.125, eb[:, i, :], ALU.mult, ALU.mult),
               reads=[("qf", i), ("eb", i)], writes=[("qd", i)])
            for hh in range(2):
                op("pool", ts(qz[i][hh], qd[:, i, :], hmask[:, hh:hh + 1], ALU.mult),
                   reads=[("qd", i), "cst"], writes=[("qz", i, hh)])
            op("dve", tt(kd[:, i, :], kf[:, i, :], enb[:, i, :], ALU.mult),
               reads=[("kf", i), ("enb", i)], writes=[("kd", i)])
            op("pool", tt(ke[:, i, :].rearrange("p (n t) -> p n t", t=128),
                          kd[:, i, :].rearrange("p (n t) -> p n t", t=128),
                          dec[:, i, 0:NJ].unsqueeze(2).to_broadcast([128, NJ, 128]), ALU.mult),
               reads=[("kd", i), ("dec", i)], writes=[("ke", i)])
        fns = []
        for j in range(NJ):
            for i in range(2):
                fns.append(tr(psTb[0][:, (j * 2 + i) * 128:(j * 2 + i + 1) * 128], ke[:, i, j * 128:(j + 1) * 128], ident_bf))
        op("pe", fns, reads=[("ke", 0), ("ke", 1), "ident_bf"], writes=["ps0"])
        op("act", acp(keT.rearrange("p j f -> p (j f)"), psTb[0][:, 0:NJ * 256]), reads=["ps0"], writes=["keT"])
        for j in range(NJ):
            par = (it * NJ + j) % 2
            tsl = slice(j * 128, (j + 1) * 128)
            fns = []
            for h in range(4):
                i, hh = h // 2, h % 2
                fns.append(mm(psS[:, h * 128:(h + 1) * 128], kd[:, i, tsl], qz[i][hh][:, tsl]))
            op("pe", fns, reads=[("kd", 0), ("kd", 1)] + [("qz", i, hh) for i in range(2) for hh in range(2)],
               writes=["ps5"])
            op("dve", tt(scT_sb.rearrange("p (a c) -> p a c", c=128),
                         psS.rearrange("p (a c) -> p a c", c=128),
                         cmask.unsqueeze(1).to_broadcast([128, 4, 128]), ALU.mult),
               reads=["ps5", "cst"], writes=["scT"])
            for i in range(2):
                fns = []
                for hh in range(2):
                    h = 2 * i + hh
                    o_ap = psO[i][:, hh * T + j * 128: hh * T + (j + 1) * 128]
                    fns.append(mm(o_ap, vb[:, j, h * 128:(h + 1) * 128], scT_sb[:, h * 128:(h + 1) * 128],
                                  start=True, stop=False))
                    fns.append(mm(o_ap, Sb[par][:, i, :], qz[i][hh][:, tsl], start=False, stop=True))
                op("pe", fns, reads=[("vb", j), "scT", ("Sb", par), ("qz", i, 0), ("qz", i, 1)], writes=[f"ps{6 + i}"])
            fns = []
            for h in range(4):
                i = h // 2
                fns.append(mm(psKV[:, h * 128:(h + 1) * 128], keT[:, j, i * 128:(i + 1) * 128],
                              vb[:, j, h * 128:(h + 1) * 128]))
            op("pe", fns, reads=["keT", ("vb", j)], writes=["ps5"])
            for h in range(4):
                i, hh = h // 2, h % 2
                r0, r1 = hh * 64, (hh + 1) * 64
                op("dve", stt(Sf[r0:r1, i, :], Sf[r0:r1, i, :], dec[r0:r1, i, j:j + 1],
                              psKV[r0:r1, h * 128:(h + 1) * 128], ALU.mult, ALU.add),
                   reads=["Sf", ("dec", i), "ps5"], writes=["Sf"])
            op("act", acp(Sb[1 - par].rearrange("p c e -> p (c e)"), Sf.rearrange("p c e -> p (c e)")),
               reads=["Sf"], writes=[("Sb", 1 - par)])
        for i in range(2):
            O = psO[i]
            ok = f"ps{6 + i}"
            op("act", act(osq2, O, AF.Square), reads=[ok], writes=["osq"])
            pss, kss = next_mm()
            op("pe", mm(pss, ones_bf, osq2), reads=["osq", "ones_bf"], writes=[kss])
            op("act", act(sd2, pss, AF.Sqrt, bias=EPS, scale=1.0 / 128), reads=[kss], writes=["sd"])
            op("dve", lambda e: e.reciprocal(out=rs2, in_=sd2), reads=["sd"], writes=["rs"])
            G = gfb[:, 2 * i:2 * i + 2, :].rearrange("p c t -> p (c t)")
            gk = [("gf", 2 * i), ("gf", 2 * i + 1)]
            op("act", act(sg2, G, AF.Sigmoid), reads=gk, writes=["sg"])
            op("dve", stt(gs2, G, tab[:, TB_GN:TB_GN + 1], sg2, ALU.mult, ALU.mult),
               reads=gk + ["sg", "tab"], writes=["gs"])
            op("dve", tt(to2, O, rs2, ALU.mult), reads=[ok, "rs"], writes=["to"])
            op("dve", tt(catT[:, 4 + 2 * i:6 + 2 * i, :].rearrange("p c t -> p (c t)"), to2, gs2, ALU.mult),
               reads=["to", "gs"], writes=[(ctag, 4 + 2 * i), (ctag, 5 + 2 * i)])

        gla_list = sc.rec_end()
        sc.play(Sched.merge([lru_lists[0], lru_lists[1], gla_list]))

    def back(it):
        xb = it % 2
        X = xs[xb]
        xk = ("xs", xb)
        catT = catT2[it % 2]
        ctag = "catT%d" % (it % 2)
        mmgroup[0] = "back"
        cat_keys = [(ctag, k) for k in range(8)]
        for j in range(NJ):
            for half in range(2):
                pso, key = next_mm()
                fns = [mm(pso, catT[:, kc, j * 128:(j + 1) * 128], w_out_s[:, kc, half * 512:(half + 1) * 512],
                          start=(kc == 0), stop=(kc == 7)) for kc in range(8)]
                op("pe", fns, reads=cat_keys + ["w_out_s"], writes=[key])
                op("dve", tt(X[:, j, half * 512:(half + 1) * 512], X[:, j, half * 512:(half + 1) * 512], pso, ALU.add),
                   reads=[key, xk], writes=[xk])
        op("sp", dma(xmid_d[it * T:(it + 1) * T, :].rearrange("(j p) d -> p j d", p=128), X),
           reads=[xk], writes=["xmid_scr"], dma=True)
        if dbg and stop_after == "mixer":
            op("sp", dma(dbg_d[it * T:(it + 1) * T, :].rearrange("(j p) d -> p j d", p=128), X),
               reads=[xk], writes=["dbg"], dma=True)
            if it + 2 < NT:
                load_x(it + 2)
            return

        norm_stats(xk, X, xnB, statB, "B")
        op("sp", dma(xn2_d[it * T:(it + 1) * T, :].rearrange("(j p) d -> p j d", p=128), xnB),
           reads=[("xnB", j) for j in range(NJ)], writes=["xn2_scr"], dma=True)
        if it + 2 < NT:
            load_x(it + 2)
        transposes_to_hT(scale2, bias2, xnB, hTB, "B", 1)
        pso, key = next_mm()
        fns = []
        for j in range(NJ):
            for kc in range(8):
                fns.append(mm(pso[:, j * 64:j * 64 + 36], hTB[:, kc, j * 128:(j + 1) * 128], wr_bf[:, kc, :],
                              start=(kc == 0), stop=(kc == 7)))
        op("pe", fns, reads=hTB_keys + ["wr_bf"], writes=[key])
        for j in range(NJ):
            op("dve", tt(lg[:, j, :], pso[:, j * 64:j * 64 + 36], brb, ALU.add), reads=[key, "brb"], writes=[("rt", j)])
        for j in range(NJ):
            st = it * NJ + j
            rk = ("rt", j)
            op("dve", red(rt[:, j, 0:1], lg[:, j, 0:4], ALU.max), reads=[rk], writes=[rk])
            op("dve", ts(rt[:, j, 4:8], lg[:, j, 0:4], rt[:, j, 0:1], ALU.is_equal), reads=[rk], writes=[rk])
            op("dve", ts(rt[:, j, 8:12], lg[:, j, 0:4], rt[:, j, 0:1], ALU.subtract), reads=[rk], writes=[rk])
            op("act", act(rt[:, j, 8:12], rt[:, j, 8:12], AF.Exp, accum_out=rt[:, j, 1:2]), reads=[rk], writes=[rk])
            op("dve", lambda e, j=j: e.reciprocal(out=rt[:, j, 2:3], in_=rt[:, j, 1:2]), reads=[rk], writes=[rk])
            op("dve", ts(rt[:, j, 12:16], rt[:, j, 4:8], 1e30, ALU.mult, -1e30, ALU.add), reads=[rk], writes=[rk])
            op("dve", tt(mf[:, j, :].rearrange("p (g e) -> p g e", g=4),
                         lg[:, j, 4:36].rearrange("p (g e) -> p g e", g=4),
                         rt[:, j, 12:16].unsqueeze(2).to_broadcast([128, 4, 8]), ALU.add), reads=[rk], writes=[rk])
            op("dve", lambda e, j=j: e.max(out=m8[:, j, :], in_=mf[:, j, :]), reads=[rk], writes=[rk])
            op("dve", ts(oh[:, j, 0:32], mf[:, j, :], m8[:, j, 0:1], ALU.is_equal), reads=[rk], writes=[rk])
            op("dve", ts(oh[:, j, 32:64], mf[:, j, :], m8[:, j, 1:2], ALU.is_equal), reads=[rk], writes=[rk])
            op("dve", cp(ohs[:, st * 64:(st + 1) * 64], oh[:, j, :]), reads=[rk], writes=["ohs"])
            op("dve", tt(rt[:, j, 16:17], m8[:, j, 1:2], m8[:, j, 0:1], ALU.subtract), reads=[rk], writes=[rk])
            op("act", act(rt[:, j, 17:18], rt[:, j, 16:17], AF.Exp), reads=[rk], writes=[rk])
            op("dve", ts(rt[:, j, 18:19], rt[:, j, 17:18], 1.0, ALU.add), reads=[rk], writes=[rk])
            op("dve", lambda e, j=j: e.reciprocal(out=rt[:, j, 19:20], in_=rt[:, j, 18:19]), reads=[rk], writes=[rk])
            op("dve", tt(wts_all[:, st * 2:st * 2 + 1], rt[:, j, 19:20], rt[:, j, 2:3], ALU.mult),
               reads=[rk], writes=["wts_all"])
            op("dve", tt(wts_all[:, st * 2 + 1:st * 2 + 2], wts_all[:, st * 2:st * 2 + 1], rt[:, j, 17:18], ALU.mult),
               reads=[rk, "wts_all"], writes=["wts_all"])
            op("dve", tt(Ot[:, j, :], oh[:, j, 0:32], oh[:, j, 32:64], ALU.add), reads=[rk], writes=[("Ot", j)])
            pp, kp = next_mm()
            op("pe", [mm(pp[:, 0:32], Umat, Ot[:, j, :], start=True, stop=False),
                      mm(pp[:, 0:32], ones_f, Ocum, start=False, stop=True)],
               reads=[("Ot", j), "Ocum", "cst", "ones_f"], writes=[kp])
            op("dve", tt(Ocum, Ocum, Ot[:, j, :], ALU.add), reads=[("Ot", j), "Ocum"], writes=["Ocum"])
            for k in range(2):
                o_k = oh[:, j, k * 32:(k + 1) * 32]
                op("dve", tt(tmp32[:, j, 0:32], o_k, pp[:, 0:32], ALU.mult), reads=[rk, kp], writes=[("tmp32", j)])
                op("dve", red(pos_all[:, st * 2 + k:st * 2 + k + 1], tmp32[:, j, 0:32], ALU.add),
                   reads=[("tmp32", j)], writes=["pos_all"])
                op("dve", tt(tmp32[:, j, 32:64], o_k, iota32, ALU.mult), reads=[rk, "cst"], writes=[("tmp32", j)])
                op("dve", red(eid_all[:, st * 2 + k:st * 2 + k + 1], tmp32[:, j, 32:64], ALU.add),
                   reads=[("tmp32", j)], writes=["eid_all"])
                op("dve", stt(rt[:, j, 20 + k:21 + k], eid_all[:, st * 2 + k:st * 2 + k + 1], float(CAP),
                              pos_all[:, st * 2 + k:st * 2 + k + 1], ALU.mult, ALU.add),
                   reads=["eid_all", "pos_all", rk], writes=[rk])
                op("dve", cp(slot_u[:, st * 2 + k:st * 2 + k + 1], rt[:, j, 20 + k:21 + k]), reads=[rk], writes=["slot_u"])
                op("pool", cp(pay_i[:, (st * 2 + k) * 2:(st * 2 + k) * 2 + 1], tokid[:, st:st + 1]),
                   reads=["tokid"], writes=[("pay", st, k)])
                op("pool", cp(pay_f[:, (st * 2 + k) * 2 + 1:(st * 2 + k) * 2 + 2], wts_all[:, st * 2 + k:st * 2 + k + 1]),
                   reads=["wts_all", ("pay", st, k)], writes=[("pay", st, k)])
                sl = slot_u[:, st * 2 + k:st * 2 + k + 1]
                py = pay_i[:, (st * 2 + k) * 2:(st * 2 + k) * 2 + 2]
                op("pool", lambda e, sl=sl, py=py: e.indirect_dma_start(
                    out=sidx_d[:, :], out_offset=bass.IndirectOffsetOnAxis(ap=sl, axis=0),
                    in_=py, in_offset=None), reads=["slot_u", ("pay", st, k)], writes=["sidx_scr"], dma=True)

    load_x(0)
    if NT > 1:
        load_x(1)
    front(0)
    for it in range(NT):
        lists = []
        if it + 1 < NT:
            sc.rec_begin()
            front(it + 1)
            lists.append(sc.rec_end())
        sc.rec_begin()
        back(it)
        lists.append(sc.rec_end())
        sc.play(Sched.merge(lists))
    mmgroup[0] = "all"

    if stop_after in ("mixer", "route"):
        if dbg and stop_after == "route":
            op("sp", dma(dbg2_d[:, 0:NST * 2], pos_all), reads=["pos_all"], writes=["dbg2"], dma=True)
            op("sp", dma(dbg2_d[:, 1024:1024 + NST * 2], eid_all), reads=["eid_all"], writes=["dbg2"], dma=True)
            op("sp", dma(dbg2_d[:, 2048:2048 + NST * 2], wts_all), reads=["wts_all"], writes=["dbg2"], dma=True)
        sc.emit()
        return nc

    sc.barrier()
    ar.reset(m0)
    thr = cst[:, CS_TH:CS_TH + NTH]
    cnt = ar.f32(32)
    big_full = ar.f32(max(NST * 2 * 32, 32 * NTH))
    big = big_full[:, 0:NST * 2 * 32]
    nblk = ar.f32(32)
    padded = ar.f32(32)
    pends = ar.f32(32)
    ebase = ar.f32(32)
    bt = ar.f32(64)
    slc_f = ar.f32(NST * 2)
    op("pe", mm(ps[2][:, 0:32], ones_f, Ocum), reads=["Ocum", "ones_f"], writes=["ps2"])
    op("dve", cp(cnt, ps[2][:, 0:32]), reads=["ps2"], writes=["cnt"])
    op("dve", tt(big_full[:, 0:32 * NTH].rearrange("p (e m) -> p e m", m=NTH),
                 cnt.unsqueeze(2).to_broadcast([128, 32, NTH]),
                 thr.unsqueeze(1).to_broadcast([128, 32, NTH]), ALU.is_gt),
       reads=["cnt", "cst"], writes=["big"])
    op("dve", red(nblk, big_full[:, 0:32 * NTH].rearrange("p (e m) -> p e m", m=NTH), ALU.add),
       reads=["big"], writes=["nblk"])
    op("dve", ts(padded, nblk, float(BLK), ALU.mult), reads=["nblk"], writes=["padded"])
    op("dve", scan(pends, ones_f[:, 0:32], padded, 0.0, ALU.mult, ALU.add), reads=["padded", "ones_f"], writes=["pends"])
    op("dve", tt(ebase, pends, padded, ALU.subtract), reads=["pends", "padded"], writes=["ebase"])
    op("dve", ts(bt[:, 0:32], pends, blk512, ALU.is_le), reads=["pends", "cst"], writes=["bt"])
    op("dve", red(bt[:, 32:33], bt[:, 0:32], ALU.add), reads=["bt"], writes=["bt"])
    op("dve", ts(bt[:, 32:33], bt[:, 32:33], float(NE - 1), ALU.min), reads=["bt"], writes=["bt"])
    op("dve", ts(bt[:, 0:32], iota32, bt[:, 32:33], ALU.is_equal), reads=["bt", "cst"], writes=["bt"])
    op("dve", tt(bt[:, 0:32], bt[:, 0:32], ebase, ALU.mult), reads=["bt", "ebase"], writes=["bt"])
    op("dve", red(bt[:, 33:34], bt[:, 0:32], ALU.add), reads=["bt"], writes=["bt"])
    op("dve", ts(bt[:, 34:35], blk512, pends[:, 31:32], ALU.is_lt), reads=["pends", "cst"], writes=["bt"])
    op("dve", stt(bt[:, 35:36], bt[:, 32:33], float(CAP), blk512, ALU.mult, ALU.add), reads=["bt", "cst"], writes=["bt"])
    op("dve", tt(bt[:, 35:36], bt[:, 35:36], bt[:, 33:34], ALU.subtract), reads=["bt"], writes=["bt"])
    op("dve", ts(bt[:, 35:36], bt[:, 35:36], float(-NULLSTART), ALU.add), reads=["bt"], writes=["bt"])
    op("dve", tt(bt[:, 35:36], bt[:, 35:36], bt[:, 34:35], ALU.mult), reads=["bt"], writes=["bt"])
    op("dve", ts(bt[:, 35:36], bt[:, 35:36], float(NULLSTART), ALU.add), reads=["bt"], writes=["bt"])
    Gb = ar.f32(256).rearrange("p (a m) -> p a m", a=2)
    bcf = ar.f32(256).rearrange("p (a m) -> p a m", a=2)
    pcol = ar.f32(2)
    op("dve", ts(Gb[:, 0, :], ones_f, bt[:, 32:33], ALU.mult), reads=["bt", "ones_f"], writes=["Gb"])
    op("dve", ts(Gb[:, 1, :], ones_f, bt[:, 35:36], ALU.mult), reads=["bt", "ones_f"], writes=["Gb"])
    op("pe", [mm(ps[3][:, 0:128], Gb[:, 0, :], ident_f), mm(ps[3][:, 128:256], Gb[:, 1, :], ident_f)],
       reads=["Gb", "cst"], writes=["ps3"])
    op("dve", cp(bcf.rearrange("p a m -> p (a m)"), ps[3][:, 0:256]), reads=["ps3"], writes=["bcf"])
    op("dve", ts(pcol[:, 0:1], blk512, 1.0 / BLK, ALU.mult), reads=["cst"], writes=["pcol"])
    op("dve", ts(bcf[:, 0, :], bcf[:, 0, :], 128.0, ALU.mult, pcol[:, 0:1], ALU.add), reads=["bcf", "pcol"], writes=["bcf"])
    op("dve", cp(widx, bcf[:, 0, :]), reads=["bcf"], writes=["widx"])
    op("dve", ts(bcf[:, 1, :], bcf[:, 1, :], 0.25, ALU.mult, pcol[:, 0:1], ALU.add), reads=["bcf", "pcol"], writes=["bcf"])
    op("dve", cp(sidx4[:, 0:128], bcf[:, 1, :]), reads=["bcf"], writes=["sidx4"])
    op("dve", tt(big.rearrange("p (a e) -> p a e", e=32), ohs.rearrange("p (a e) -> p a e", e=32),
                 ebase.unsqueeze(1).to_broadcast([128, NST * 2, 32]), ALU.mult),
       reads=["ohs", "ebase", "big"], writes=["big"])
    op("dve", red(slc_f, big.rearrange("p (a e) -> p a e", e=32), ALU.add), reads=["big"], writes=["slc_f"])
    op("dve", tt(slc_f, slc_f, pos_all, ALU.add), reads=["slc_f", "pos_all"], writes=["slc_f"])
    op("dve", cp(slot_c, slc_f), reads=["slc_f"], writes=["slot_c"])
    if dbg and stop_after == "tables":
        op("sp", dma(dbg2_d[:, 0:128], widx.bitcast(F32)), reads=["widx"], writes=["dbg2"], dma=True)
        op("sp", dma(dbg2_d[:, 512:1024], sidx4.bitcast(F32)), reads=["sidx4"], writes=["dbg2"], dma=True)
        op("sp", dma(dbg2_d[:, 1024:1024 + NST * 2], slot_c.bitcast(F32)), reads=["slot_c"], writes=["dbg2"], dma=True)
        op("sp", dma(dbg2_d[:, 256:288], pends), reads=["pends"], writes=["dbg2"], dma=True)
        sc.emit()
        return nc

    m3 = ar.mark()
    NSTG = 3
    stg3 = [ar.f32(4096) for _ in range(NSTG)]
    wg_bf = [ar.bf(8 * 512).rearrange("p (k f) -> p k f", k=8) for _ in range(2)]
    wu_bf = [ar.bf(8 * 512).rearrange("p (k f) -> p k f", k=8) for _ in range(2)]
    wd_bf = [ar.bf(4 * 1024).rearrange("p (k f) -> p k f", k=4) for _ in range(2)]
    Xg = [ar.bf(4 * D).rearrange("p (j d) -> p j d", j=4) for _ in range(2)]
    h2T = ar.bf(8 * BLK).rearrange("p (k t) -> p k t", k=8)
    hidT = ar.bf(4 * BLK).rearrange("p (k t) -> p k t", k=4)
    sgb = [ar.f32(BLK) for _ in range(2)]
    ysb = [ar.f32(D) for _ in range(2)]
    print("arena used phase3 (words):", ar.off, "of", ARW)
    stg_i = [0]
    cast_i = [0]
    wgv = wg_d.rearrange("e (p kk) f -> (e p) (kk f)", kk=8)
    wuv = wu_d.rearrange("e (p kk) f -> (e p) (kk f)", kk=8)
    wdv = wd_d.rearrange("e (p kk) f -> (e p) (kk f)", kk=4)

    def issue_loads(b):
        q = b % 2
        op("pool", lambda e, q=q, b=b: e.indirect_dma_start(
            out=idx_sb[q][:, 0:8], out_offset=None, in_=sidx_d.rearrange("(r f) c -> r (f c)", f=4),
            in_offset=bass.IndirectOffsetOnAxis(ap=sidx4[:, b:b + 1], axis=0)),
           reads=["sidx4", "sidx_scr"], writes=[("idx", q)], dma=True)
        casts = []
        for (wv, dst, nm) in ((wgv, wg_bf[q], "wg"), (wuv, wu_bf[q], "wu"), (wdv, wd_bf[q], "wd")):
            sgi = stg_i[0] % NSTG
            stg_i[0] += 1
            op("pool", lambda e, wv=wv, sgi=sgi, b=b: e.indirect_dma_start(
                out=stg3[sgi], out_offset=None, in_=wv,
                in_offset=bass.IndirectOffsetOnAxis(ap=widx[:, b:b + 1], axis=0)),
               reads=["widx"], writes=[("stg3", sgi)], dma=True)
            casts.append((dst, nm, sgi))
        for j in range(4):
            op("pool", lambda e, j=j, q=q: e.indirect_dma_start(
                out=Xg[q][:, j, :], out_offset=None, in_=xn2_d[:, :],
                in_offset=bass.IndirectOffsetOnAxis(ap=idx_sb[q][:, 2 * j:2 * j + 1], axis=0)),
               reads=[("idx", q), "xn2_scr"], writes=[("Xg", q, j)], dma=True)
        sc.rec_begin()
        for (dst, nm, sgi) in casts:
            dflat = dst.rearrange("p k f -> p (k f)")
            for hf in range(2):
                sl = slice(hf * 2048, (hf + 1) * 2048)
                if nm != "wd":
                    ce = ("act", "dve")[cast_i[0] % 2]
                    cast_i[0] += 1
                    op(ce, (acp if ce == "act" else cp)(dflat[:, sl], stg3[sgi][:, sl]),
                       reads=[("stg3", sgi)], writes=[(nm, q, hf)])
                else:
                    ce = ("dve", "pool")[hf]
                    op(ce, tt(dflat[:, sl].rearrange("p (k f) -> p k f", k=2),
                              stg3[sgi][:, sl].rearrange("p (k f) -> p k f", k=2),
                              gt2_bc.unsqueeze(1).to_broadcast([128, 2, D]), ALU.mult),
                       reads=[("stg3", sgi), "gt2_bc"], writes=[(nm, q, hf)])
        return sc.rec_end()

    h2_keys = [("h2T", kc) for kc in range(8)]
    hid_keys = [("hidT", hc) for hc in range(4)]

    def stageA(b):
        q = b % 2
        for r4 in range(4):
            hb = r4 % 2
            fns = []
            for u in range(2):
                kc = r4 * 2 + u
                for j in range(4):
                    fns.append(tr(psTb[hb][:, u * BLK + j * 128:u * BLK + (j + 1) * 128],
                                  Xg[q][:, j, :].rearrange("p (m kk) -> p kk m", kk=8)[:, kc, :], ident_bf))
            op("pe", fns, reads=[("Xg", q, j) for j in range(4)] + ["ident_bf"], writes=[f"ps{hb}"])
            for u in range(2):
                kc = r4 * 2 + u
                op("act", act(h2T[:, kc, :], psTb[hb][:, u * BLK:(u + 1) * BLK], AF.Identity,
                              bias=bias2p[:, kc:kc + 1], scale=scale2p[:, kc:kc + 1]),
                   reads=[f"ps{hb}", "scale2p", "modP"], writes=[("h2T", kc)])

    def stageG(b):
        q = b % 2
        for hc in range(4):
            pg, kg = next_mm()
            op("pe", [mm(pg, wg_bf[q][:, kc, :].rearrange("p (m c) -> p c m", c=4)[:, hc, :], h2T[:, kc, :], start=(kc == 0), stop=(kc == 7))
                      for kc in range(8)], reads=h2_keys + [("wg", q, 0), ("wg", q, 1)], writes=[kg])
            pu, ku = next_mm()
            op("pe", [mm(pu, wu_bf[q][:, kc, :].rearrange("p (m c) -> p c m", c=4)[:, hc, :], h2T[:, kc, :], start=(kc == 0), stop=(kc == 7))
                      for kc in range(8)], reads=h2_keys + [("wu", q, 0), ("wu", q, 1)], writes=[ku])
            sgk = ("sgb", hc % 2)
            op("act", act(sgb[hc % 2], pg, AF.Sigmoid), reads=[kg], writes=[sgk])
            op("dve", tt(sgb[hc % 2], sgb[hc % 2], pg, ALU.mult), reads=[kg, sgk], writes=[sgk])
            op("dve", tt(hidT[:, hc, :], sgb[hc % 2], pu, ALU.mult), reads=[ku, sgk], writes=[("hidT", hc)])

    def stageD(b):
        q = b % 2
        for j in range(4):
            yq = (b * 4 + j) % 2
            for half in range(2):
                pd, kd_ = next_mm()
                op("pe", [mm(pd, hidT[:, hc, j * 128:(j + 1) * 128], wd_bf[q][:, hc, half * 512:(half + 1) * 512],
                             start=(hc == 0), stop=(hc == 3)) for hc in range(4)],
                   reads=hid_keys + [("wd", q, 0), ("wd", q, 1)], writes=[kd_])
                wtok = idx_sb[q].bitcast(F32)[:, 2 * j + 1:2 * j + 2]
                op("act", act(ysb[yq][:, half * 512:(half + 1) * 512], pd, AF.Identity, scale=wtok),
                   reads=[kd_, ("idx", q)], writes=[("ysb", yq)])
            op("sp", dma(ybuf_d[b * BLK:(b + 1) * BLK, :].rearrange("(p j) d -> p j d", j=4)[:, j, :], ysb[yq]),
               reads=[("ysb", yq)], writes=[("ybuf", b, j)], dma=True)

    sc.play(issue_loads(0))
    stageA(0)
    for b in range(NB):
        lists = []
        if b + 1 < NB:
            lists.append(issue_loads(b + 1))
        stageG(b)
        if b + 1 < NB:
            sc.rec_begin()
            stageA(b + 1)
            lists.append(sc.rec_end())
        sc.rec_begin()
        stageD(b)
        lists.append(sc.rec_end())
        sc.play(Sched.merge(lists))

    sc.barrier()
    ar.reset(m3)
    F = [dict(xm=ar.f32(D), y0=ar.f32(D), y1=ar.f32(D), jk=ar.bf(D), st=ar.f32(4)) for _ in range(2)]
    for st in range(NST):
        f = F[st % 2]
        fk = ("F", st % 2)
        op("sp", dma(f["xm"], xmid_d[st * 128:(st + 1) * 128, :]), reads=["xmid_scr"], writes=[fk + ("xm",)], dma=True)
        for k in range(2):
            op("pool", lambda e, f=f, k=k, st=st: e.indirect_dma_start(
                out=f["y%d" % k], out_offset=None, in_=ybuf_d[:, :],
                in_offset=bass.IndirectOffsetOnAxis(ap=slot_c[:, st * 2 + k:st * 2 + k + 1], axis=0)),
               reads=["slot_c"], writes=[fk + ("y%d" % k,)], dma=True)
        op("pool", tt(f["y0"], f["y0"], f["y1"], ALU.add), reads=[fk + ("y0",), fk + ("y1",)], writes=[fk + ("y0",)])
        op("dve", tt(f["xm"], f["xm"], f["y0"], ALU.add), reads=[fk + ("xm",), fk + ("y0",)], writes=[fk + ("xm",)])
        op("act", act(f["jk"], f["xm"], AF.Square, accum_out=f["st"][:, 0:1]), reads=[fk + ("xm",)], writes=[fk + ("st",), fk + ("jk",)])
        op("act", act(f["st"][:, 1:2], f["st"][:, 0:1], AF.Sqrt, bias=EPS, scale=1.0 / D), reads=[fk + ("st",)], writes=[fk + ("st",)])
        op("dve", lambda e, f=f: e.reciprocal(out=f["st"][:, 2:3], in_=f["st"][:, 1:2]), reads=[fk + ("st",)], writes=[fk + ("st",)])
        op("dve", stt(f["y1"], f["xm"], f["st"][:, 2:3], gf_bc, ALU.mult, ALU.mult),
           reads=[fk + ("xm",), fk + ("st",), "gf_bc"], writes=[fk + ("y1",)])
        op("sp", dma(out_d[st * 128:(st + 1) * 128, :], f["y1"]), reads=[fk + ("y1",)], writes=["out"], dma=True)

    sc.emit()
    return nc


def host_consts(S):
    NST = S // 128
    cst = np.zeros((128, CS_N), np.float32)
    cst[:, CS_ID:CS_ID + 128] = np.eye(128, dtype=np.float32)
    p = np.arange(128)
    cst[:, CS_U:CS_U + 128] = (p[:, None] < p[None, :]).astype(np.float32)
    cst[:, CS_CM:CS_CM + 128] = (p[:, None] <= p[None, :]).astype(np.float32)
    cst[:, CS_HM] = (p < 64).astype(np.float32)
    cst[:, CS_HM + 1] = (p >= 64).astype(np.float32)
    cst[:, CS_TH:CS_TH + NTH] = (np.arange(NTH) * BLK).astype(np.float32)[None, :]
    rm = np.ones((512,), np.float32)
    rm[::128] = 0.0
    cst[:, CS_RM:CS_RM + 512] = rm[None, :]
    cst[:, CS_IO:CS_IO + 32] = np.arange(32, dtype=np.float32)[None, :]
    cst[:, CS_B5] = (p * BLK).astype(np.float32)
    tokid = (np.arange(NST)[None, :] * 128 + p[:, None]).astype(np.int32)
    return cst, tokid


def fm(v, n):
    return np.ascontiguousarray(np.asarray(v, np.float32).reshape(n, 128).T)


def host_inputs(inp, b, S):
    L = 0
    tab = np.zeros((128, TB_N), np.float32)
    tab[:, TB_BADA:TB_BADA + 48] = fm(inp["b_ada"][L], 48)
    tab[:, TB_GMIX:TB_GMIX + 8] = fm(inp["g_mix"][L], 8)
    tab[:, TB_GFFN:TB_GFFN + 8] = fm(inp["g_ffn"][L], 8)
    cw = np.asarray(inp["conv_w"][L], np.float32)
    for c in range(4):
        tab[:, TB_CW + c * 4:TB_CW + c * 4 + 4] = cw[:, c * 128:(c + 1) * 128].T
    tab[:, TB_CB:TB_CB + 4] = fm(inp["conv_b"][L], 4)
    tab[:, TB_BR:TB_BR + 4] = fm(inp["lru_br"][L], 4)
    tab[:, TB_BI:TB_BI + 4] = fm(inp["lru_bi"][L], 4)
    tab[:, TB_LAM:TB_LAM + 4] = fm(inp["lru_lambda"][L], 4)
    tab[:, TB_BA:TB_BA + 2] = fm(inp["gla_ba"][L], 2)
    tab[:, TB_GN] = np.asarray(inp["gla_gnorm"][L], np.float32)
    ba = np.asarray(inp["b_ada"][L], np.float32)
    tab[:, TB_BADAP:TB_BADAP + 8] = ba[3 * D:4 * D].reshape(128, 8)
    tab[:, TB_BADAP + 8:TB_BADAP + 16] = ba[4 * D:5 * D].reshape(128, 8)
    tab[:, TB_GFFNP:TB_GFFNP + 8] = np.asarray(inp["g_ffn"][L], np.float32).reshape(128, 8)

    def bd(w):
        w = np.asarray(w, np.float32)
        o = np.zeros((128, 4, 128), np.float32)
        for c in range(4):
            for hh in range(2):
                o[hh * 64:(hh + 1) * 64, c, hh * 64:(hh + 1) * 64] = w[2 * c + hh]
        return o.reshape(128, 512)

    cst, tokid = host_consts(S)
    m = {
        "x": np.ascontiguousarray(np.asarray(inp["x"][b], np.float32)),
        "cT": fm(inp["c"][b], 8),
        "w_ada": np.ascontiguousarray(np.asarray(inp["w_ada"][L], np.float32)),
        "tab": tab,
        "cst": cst,
        "tokid": tokid,
        "gf_bc": np.ascontiguousarray(np.broadcast_to(np.asarray(inp["g_final"], np.float32)[None, :], (128, D))),
        "br_bc": np.ascontiguousarray(np.broadcast_to(
            np.concatenate([np.asarray(inp["b_coarse"][L], np.float32),
                            np.asarray(inp["b_fine"][L], np.float32)])[None, :], (128, 36))),
        "w_in": np.ascontiguousarray(np.asarray(inp["w_in"][L], np.float32)),
        "w_out": np.ascontiguousarray(np.asarray(inp["w_out"][L], np.float32)),
        "bd_r": bd(inp["lru_wr"][L]),
        "bd_i": bd(inp["lru_wi"][L]),
        "wa2": np.ascontiguousarray(np.asarray(inp["gla_wa2"][L], np.float32)),
        "w_r": np.ascontiguousarray(np.concatenate([np.asarray(inp["w_coarse"][L], np.float32),
                                                    np.asarray(inp["w_fine"][L], np.float32)], axis=1)),
        "w_gate": np.ascontiguousarray(np.asarray(inp["w_gate"][L], np.float32)),
        "w_up": np.ascontiguousarray(np.asarray(inp["w_up"][L], np.float32)),
        "w_down": np.ascontiguousarray(np.asarray(inp["w_down"][L], np.float32)),
    }
    return m


def kernel(**inputs):
    B, S = inputs["x"].shape[0], inputs["x"].shape[1]
    nc = build(S)
    in_maps = [host_inputs(inputs, b, S) for b in range(B)]
    res = run_bass_kernel_spmd(nc, in_maps, core_ids=list(range(B)))
    return np.stack([np.asarray(r["out"]) for r in res.results], axis=0).astype(np.float32)
```

```python
import numpy as np
import concourse.bass as bass
import concourse.mybir as mybir
from concourse.bass_utils import run_bass_kernel_spmd

F32 = mybir.dt.float32
BF16 = mybir.dt.bfloat16
I32 = mybir.dt.int32
U32 = mybir.dt.uint32
AF = mybir.ActivationFunctionType
ALU = mybir.AluOpType
AX = mybir.AxisListType

D = 1024
NPROJ = 2576
NE = 32
CAP = 8192 + 512
EPS = 1e-6
T = 256
BLK = 512


class Sched:
    EPOCH = 6000

    def __init__(self, nc, same_engine_sync=True):
        self.nc = nc
        self.ops = []
        self.lw = {}
        self.rd = {}
        self.same = same_engine_sync
        self.stack = []

    def rec_begin(self):
        self.stack.append([])

    def rec_end(self):
        return self.stack.pop()

    def play(self, lst):
        for a in lst:
            self.op(*a)

    @staticmethod
    def merge(lists):
        lists = [l for l in lists if l]
        out = []
        n = [len(l) for l in lists]
        pos = [0] * len(lists)
        total = sum(n)
        for _ in range(total):
            bi, bv = -1, 2.0
            for i in range(len(lists)):
                if pos[i] < n[i]:
                    v = pos[i] / n[i]
                    if v < bv:
                        bi, bv = i, v
            out.append(lists[bi][pos[bi]])
            pos[bi] += 1
        return out

    def op(self, eng, fns, reads=(), writes=(), dma=False):
        if self.stack:
            self.stack[-1].append((eng, fns, list(reads), list(writes), dma))
            return
        if callable(fns):
            fns = [fns]
        reads, writes = list(reads), list(writes)
        for k in reads:
            if isinstance(k, str) and k.startswith("ps") and k[2:].isdigit() and k not in writes:
                writes.append(k)
        idx = len(self.ops)
        deps = set()
        for k in reads:
            if k in self.lw:
                deps.add(self.lw[k])
        for k in writes:
            if k in self.lw:
                deps.add(self.lw[k])
            r = self.rd.get(k)
            if r:
                deps.update(r[0].values())
                deps.update(r[1])
        self.ops.append(dict(eng=eng, fns=fns, deps=deps, dma=dma, tag=(list(reads), list(writes))))
        for k in writes:
            self.lw[k] = idx
            self.rd[k] = ({}, [])
        for k in reads:
            r = self.rd.setdefault(k, ({}, []))
            if dma:
                r[1].append(idx)
            else:
                r[0][eng] = idx
        return idx

    def barrier(self):
        last = {}
        dmas = []
        for i, o in enumerate(self.ops):
            if o["dma"]:
                dmas.append(i)
            else:
                last[o["eng"]] = i
        deps = set(last.values()) | set(dmas)
        for eng in ("pe", "act", "dve", "pool", "sp"):
            idx = len(self.ops)
            self.ops.append(dict(eng=eng, fns=[lambda e: e.nop()], deps=set(deps), dma=False))
            last[eng] = idx
        self._pending_dma_done = True
        self.lw = {}
        self.rd = {}
        self._bar = dict(last)
        for eng, i in last.items():
            self.lw[("__bar__", eng)] = i

    def emit(self):
        import os
        lim = int(os.environ.get("OPLIMIT", "0"))
        print("total ops", len(self.ops))
        if lim:
            self.ops = self.ops[:lim]
            o = self.ops[-1]
            print("last op", o["eng"], o.get("tag"))
        nc = self.nc
        engs = ("pe", "act", "dve", "pool", "sp")
        count = {e: 0 for e in engs}
        prog = {e: [] for e in engs}
        dma_pool = {}
        dma_rr = {e: 0 for e in engs}
        dma_val = {}
        NPOOL = {"sp": 10, "pool": 8, "act": 4, "dve": 2, "pe": 2}
        tokens = []
        pre_wait = []
        for o in self.ops:
            e = o["eng"]
            if o["dma"]:
                pl = dma_pool.setdefault(e, [])
                if len(pl) < NPOOL[e]:
                    s = nc.alloc_semaphore(name=f"dq_{e}_{len(pl)}")
                    pl.append(s)
                    dma_val[id(s)] = 0
                s = pl[dma_rr[e] % len(pl)] if len(pl) == NPOOL[e] else pl[-1]
                dma_rr[e] += 1
                prev = dma_val[id(s)]
                dma_val[id(s)] = prev + 16
                tokens.append((s, prev + 16))
                pre_wait.append((s, prev) if prev > 0 else None)
            else:
                c = count[e]
                ep = c // self.EPOCH
                while len(prog[e]) <= ep:
                    prog[e].append(nc.alloc_semaphore(name=f"pg_{e}_{len(prog[e])}"))
                tokens.append((prog[e][ep], c % self.EPOCH + 1))
                pre_wait.append(None)
                count[e] = c + 1
        waited = {e: {} for e in engs}
        streams = {e: [] for e in engs}
        for i, o in enumerate(self.ops):
            e = o["eng"]
            ws = []
            cand = []
            if pre_wait[i] is not None:
                cand.append(pre_wait[i])
            for d in sorted(o["deps"]):
                od = self.ops[d]
                if (not od["dma"]) and od["eng"] == e and (e == "pe" or not self.same):
                    continue
                cand.append(tokens[d])
            for (s, v) in cand:
                w = waited[e]
                if w.get(id(s), 0) >= v:
                    continue
                w[id(s)] = v
                ws.append((s, v))
            streams[e].append((ws, o["fns"], tokens[i], o["dma"]))
        final_waits = []
        for e, pl in dma_pool.items():
            for s in pl:
                if dma_val[id(s)] > 0:
                    final_waits.append((s, dma_val[id(s)]))
        for e in engs:
            if count[e] > 0:
                c = count[e] - 1
                final_waits.append((prog[e][c // self.EPOCH], c % self.EPOCH + 1))

        def run(engine, name):
            for ws, fns, tok, is_dma in streams[name]:
                for (s, v) in ws:
                    engine.wait_ge(s, v)
                ins = None
                for f in fns:
                    ins = f(engine)
                ins.then_inc(tok[0], 16 if is_dma else 1)

        with nc.Block() as block:
            @block.tensor
            def _(eng):
                run(eng, "pe")

            @block.scalar
            def _(eng):
                run(eng, "act")

            @block.vector
            def _(eng):
                run(eng, "dve")

            @block.gpsimd
            def _(eng):
                run(eng, "pool")

            @block.sync
            def _(eng):
                run(eng, "sp")
                for (s, v) in final_waits:
                    eng.wait_ge(s, v)


def act(out, in_, func, bias=None, scale=None, accum_out=None):
    def f(e):
        kw = {}
        if bias is not None:
            kw["bias"] = bias
        if scale is not None:
            kw["scale"] = scale
        if accum_out is not None:
            kw["accum_out"] = accum_out
        return e.activation(out=out, in_=in_, func=func, **kw)
    return f


def ts(out, in0, s1, op0, s2=None, op1=None, accum_out=None):
    def f(e):
        kw = {}
        if op1 is not None:
            kw["op1"] = op1
        if accum_out is not None:
            kw["accum_out"] = accum_out
        return e.tensor_scalar(out=out, in0=in0, scalar1=s1, scalar2=s2, op0=op0, **kw)
    return f


def tt(out, in0, in1, op):
    return lambda e: e.tensor_tensor(out=out, in0=in0, in1=in1, op=op)


def stt(out, in0, scalar, in1, op0, op1):
    return lambda e: e.scalar_tensor_tensor(out=out, in0=in0, scalar=scalar, in1=in1, op0=op0, op1=op1)


def mm(out, lhsT, rhs, start=True, stop=True):
    return lambda e: e.matmul(out, lhsT, rhs, start=start, stop=stop)


def tr(out, in_, ident):
    return lambda e: e.transpose(out, in_, ident)


def dma(out, in_, **kw):
    return lambda e: e.dma_start(out=out, in_=in_, **kw)


def cp(out, in_):
    return lambda e: e.tensor_copy(out=out, in_=in_)


def acp(out, in_):
    return lambda e: e.activation(out=out, in_=in_, func=AF.Copy)


def scan(out, d0, d1, init, op0, op1):
    return lambda e: e.tensor_tensor_scan(out=out, data0=d0, data1=d1, initial=init, op0=op0, op1=op1)


def red(out, in_, op):
    return lambda e: e.tensor_reduce(out=out, in_=in_, axis=AX.X, op=op)


def mset(ap, v):
    return lambda e: e.memset(ap, v)


TB_BADA = 0
TB_GMIX = 48
TB_GFFN = 56
TB_CW = 64
TB_CB = 80
TB_BR = 84
TB_BI = 88
TB_LAM = 92
TB_BA = 96
TB_GN = 98
TB_BADAP = 99
TB_GFFNP = 115
TB_N = 123

CS_ID = 0
CS_U = 128
CS_CM = 256
CS_RM = 384
CS_IO = 896
CS_B5 = 928
CS_HM = 929
CS_TH = 931
NTH = 17
CS_N = 948


class Arena:
    def __init__(self, ap, nwords):
        self.ap = ap
        self.n = nwords
        self.off = 0

    def f32(self, n):
        n = (n + 1) // 2 * 2
        assert self.off + n <= self.n, ("arena overflow", self.off, n, self.n)
        v = self.ap[:, self.off:self.off + n]
        self.off += n
        return v

    def bf(self, nel):
        w = (nel + 1) // 2
        w = (w + 1) // 2 * 2
        v = self.f32(w).bitcast(BF16)
        return v[:, 0:nel]

    def mark(self):
        return self.off

    def reset(self, m):
        self.off = m


def build(S, stop_after=None, dbg=False):
    NT = S // T
    NST = S // 128
    NB = (2 * S) // BLK + NE
    NROWS = NE * CAP + BLK
    NULLSTART = NE * CAP
    nc = bass.Bass("TRN2", target_bir_lowering=False)

    def din(name, shape, dt=F32):
        return nc.dram_tensor(name, shape, dt, kind="ExternalInput").ap()

    x_d = din("x", [S, D])
    cT_d = din("cT", [128, 8])
    wada_d = din("w_ada", [D, 6 * D])
    tab_d = din("tab", [128, TB_N])
    cst_d = din("cst", [128, CS_N])
    tok_d = din("tokid", [128, NST], I32)
    gf_d = din("gf_bc", [128, D])
    brb_d = din("br_bc", [128, 36])
    win_d = din("w_in", [D, NPROJ])
    wout_d = din("w_out", [D, D])
    bdr_d = din("bd_r", [128, 512])
    bdi_d = din("bd_i", [128, 512])
    wa2_d = din("wa2", [16, 256])
    wr_d = din("w_r", [D, 36])
    wg_d = din("w_gate", [NE, D, 512])
    wu_d = din("w_up", [NE, D, 512])
    wd_d = din("w_down", [NE, 512, D])
    out_d = nc.dram_tensor("out", [S, D], F32, kind="ExternalOutput").ap()

    xmid_d = nc.dram_tensor("xmid_scr", [S, D], F32, kind="Internal").ap()
    xn2_d = nc.dram_tensor("xn2_scr", [S, D], BF16, kind="Internal").ap()
    sidx_d = nc.dram_tensor("sidx_scr", [NROWS, 2], I32, kind="Internal").ap()
    ybuf_d = nc.dram_tensor("ybuf_scr", [NB * BLK, D], F32, kind="Internal").ap()
    dbg_d = None
    if dbg:
        dbg_d = nc.dram_tensor("dbg", [S, D], F32, kind="ExternalOutput").ap()
        dbg2_d = nc.dram_tensor("dbg2", [128, 4096], F32, kind="ExternalOutput").ap()

    sc = Sched(nc)
    op = sc.op

    def sb(name, shape, dt=F32):
        return nc.alloc_sbuf_tensor(name + "_sb", shape, dt)[:]

    tab = sb("tab", [128, TB_N])
    cst = sb("cst", [128, CS_N])
    tokid = sb("tokid", [128, NST], I32)
    gf_bc = sb("gf_bc", [128, D])
    gt1_bc = sb("gt1_bc", [128, D])
    gt2_bc = sb("gt2_bc", [128, D])
    brb = sb("brb", [128, 36])
    modT = sb("modT", [128, 48])
    scale1 = sb("scale1", [128, 8])
    scale2 = sb("scale2", [128, 8])
    scale2p = sb("scale2p", [128, 8])
    modP = sb("modP", [128, 16])
    widx = sb("widx", [128, 128], I32)
    sidx4 = sb("sidx4", [128, 4 * 128], I32)
    ltab = sb("ltab", [128, 16])
    ident_bf = sb("ident_bf", [128, 128], BF16)
    ones_bf = sb("ones_bf", [128, 128], BF16)
    ones_f = sb("ones_f", [128, 128])
    ohs = sb("ohs", [128, NST * 2 * 32], BF16)
    pos_all = sb("pos_all", [128, NST * 2])
    eid_all = sb("eid_all", [128, NST * 2])
    wts_all = sb("wts_all", [128, NST * 2])
    Ocum = sb("Ocum", [128, 32])
    pay_i = sb("pay", [128, NST * 4], I32)
    pay_f = pay_i.bitcast(F32)
    slot_u = sb("slot_u", [128, NST * 2], I32)
    slot_c = sb("slot_c", [128, NST * 2], I32)
    tbl_i = sb("tbl_i", [1, 256], I32)
    idx_sb = [sb(f"idx_sb{q}", [128, 8], I32) for q in range(2)]
    ident_f = cst[:, CS_ID:CS_ID + 128]
    Umat = cst[:, CS_U:CS_U + 128]
    cmask = cst[:, CS_CM:CS_CM + 128]
    hmask = cst[:, CS_HM:CS_HM + 2]
    rmask = cst[:, CS_RM:CS_RM + 512]
    iota32 = cst[:, CS_IO:CS_IO + 32]
    blk512 = cst[:, CS_B5:CS_B5 + 1]

    rem = nc.sbuf_bytes_remaining
    rem = rem() if callable(rem) else rem
    ARW = (int(rem) // 4) - 64
    ARW = ARW // 2 * 2
    arena_ap = sb("arena", [128, ARW])
    ar = Arena(arena_ap, ARW)

    ps = [nc.alloc_psum_tensor(f"ps{i}", [128, 512], F32)[:] for i in range(8)]

    m0 = ar.mark()
    op("sp", dma(tab, tab_d), writes=["tab"], dma=True)
    op("sp", dma(cst, cst_d), writes=["cst"], dma=True)
    cT = ar.f32(8)
    op("sp", dma(cT, cT_d), writes=["cT"], dma=True)
    op("sp", dma(tokid, tok_d), writes=["tokid"], dma=True)
    op("sp", dma(gf_bc, gf_d), writes=["gf_bc"], dma=True)
    op("sp", dma(brb, brb_d), writes=["brb"], dma=True)
    sgc = ar.f32(8)
    scT = ar.f32(8)
    op("act", act(sgc, cT, AF.Sigmoid), reads=["cT"], writes=["sgc"])
    op("dve", tt(scT, cT, sgc, ALU.mult), reads=["cT", "sgc"], writes=["scT"])
    op("dve", mset(ones_f, 1.0), writes=["ones_f"])
    op("dve", mset(ones_bf, 1.0), writes=["ones_bf"])
    op("dve", cp(ident_bf, ident_f), reads=["cst"], writes=["ident_bf"])

    zt = ar.f32(2048)
    op("pool", mset(zt, 0.0), writes=["zt"])
    zt_i = zt.bitcast(I32)
    rows_per = 128 * 1024
    r0 = 0
    while r0 < NROWS:
        n = min(rows_per, NROWS - r0)
        np_ = n // 1024
        if np_ > 0:
            op("sp", dma(sidx_d[r0:r0 + np_ * 1024, :].rearrange("(p r) c -> p (r c)", p=np_),
                         zt_i[0:np_, :]), reads=["zt"], writes=["sidx_scr"], dma=True)
            r0 += np_ * 1024
        else:
            op("sp", dma(sidx_d[r0:r0 + n, :].rearrange("(p r) c -> p (r c)", p=1),
                         zt_i[0:1, 0:2 * n]), reads=["zt"], writes=["sidx_scr"], dma=True)
            r0 += n

    wst = [ar.f32(8 * 1024).rearrange("p (k f) -> p k f", k=8) for _ in range(2)]
    for blk in range(6):
        b = blk % 2
        op("sp", dma(wst[b], wada_d[:, blk * 1024:(blk + 1) * 1024].rearrange("(k p) f -> p k f", p=128)),
           writes=[("wst", b)], dma=True)
        fns = []
        for fj in range(8):
            for kc in range(8):
                fns.append(mm(ps[0][:, blk * 8 + fj: blk * 8 + fj + 1],
                              wst[b][:, kc, fj * 128:(fj + 1) * 128], scT[:, kc:kc + 1],
                              start=(kc == 0), stop=(kc == 7)))
        if blk in (3, 4):
            for kk in range(8):
                for kc in range(8):
                    fns.append(mm(ps[0][:, 48 + (blk - 3) * 8 + kk: 48 + (blk - 3) * 8 + kk + 1],
                                  wst[b][:, kc, :].rearrange("p (m kk) -> p kk m", kk=8)[:, kk, :],
                                  scT[:, kc:kc + 1], start=(kc == 0), stop=(kc == 7)))
        op("pe", fns, reads=[("wst", b), "scT"], writes=["ps0"])
    op("dve", tt(modT, ps[0][:, 0:48], tab[:, TB_BADA:TB_BADA + 48], ALU.add),
       reads=["ps0", "tab"], writes=["modT"])
    op("dve", tt(modP, ps[0][:, 48:64], tab[:, TB_BADAP:TB_BADAP + 16], ALU.add),
       reads=["ps0", "tab"], writes=["modP"])
    op("dve", stt(scale2p, modP[:, 8:16], 1.0, tab[:, TB_GFFNP:TB_GFFNP + 8], ALU.add, ALU.mult),
       reads=["modP", "tab"], writes=["scale2p"])
    op("dve", stt(scale1, modT[:, 8:16], 1.0, tab[:, TB_GMIX:TB_GMIX + 8], ALU.add, ALU.mult),
       reads=["modT", "tab"], writes=["scale1"])
    op("dve", stt(scale2, modT[:, 32:40], 1.0, tab[:, TB_GFFN:TB_GFFN + 8], ALU.add, ALU.mult),
       reads=["modT", "tab"], writes=["scale2"])
    bias1 = modT[:, 0:8]
    bias2 = modT[:, 24:32]
    bias2p = modP[:, 0:8]
    Gt = ar.f32(8 * 128).rearrange("p (k f) -> p k f", k=8)
    for (col0, dst, nm) in ((16, gt1_bc, "gt1_bc"), (40, gt2_bc, "gt2_bc")):
        for kc in range(8):
            op("dve", ts(Gt[:, kc, :], ones_f, modT[:, col0 + kc:col0 + kc + 1], ALU.mult),
               reads=["modT", "ones_f"], writes=[("Gt", kc)])
        for half in range(2):
            fns = [mm(ps[1 + half][:, k4 * 128:(k4 + 1) * 128], Gt[:, half * 4 + k4, :], ident_f)
                   for k4 in range(4)]
            op("pe", fns, reads=[("Gt", half * 4 + k4) for k4 in range(4)] + ["cst"],
               writes=[f"ps{1 + half}"])
            op("act", acp(dst[:, half * 512:(half + 1) * 512], ps[1 + half]),
               reads=[f"ps{1 + half}"], writes=[nm])
    t4 = ar.f32(4)
    op("act", act(t4, tab[:, TB_LAM:TB_LAM + 4], AF.Exp, scale=-1.0), reads=["tab"], writes=["t4"])
    op("act", act(t4, t4, AF.Ln, bias=1.0), reads=["t4"], writes=["t4"])
    op("dve", ts(ltab[:, 0:4], t4, -8.0, ALU.mult), reads=["t4"], writes=["ltab"])
    op("dve", ts(ltab[:, 4:8], t4, -16.0, ALU.mult), reads=["t4"], writes=["ltab"])
    op("dve", ts(ltab[:, 8:10], tab[:, TB_BA:TB_BA + 2], -1.0, ALU.mult), reads=["tab"], writes=["ltab"])
    cl = ltab[:, 0:4]
    c2l = ltab[:, 4:8]
    nba = ltab[:, 8:10]
    m_setup_tmp = ar.mark()

    ar.reset(m0)
    w_in_bf = ar.bf(8 * NPROJ).rearrange("p (k f) -> p k f", k=8)
    w_out_s = ar.bf(8 * D).rearrange("p (k f) -> p k f", k=8)
    bdr_bf = ar.bf(512).rearrange("p (c m) -> p c m", c=4)
    bdi_bf = ar.bf(512).rearrange("p (c m) -> p c m", c=4)
    wa2_bf = ar.bf(256)
    wr_bf = ar.bf(8 * 36).rearrange("p (k n) -> p k n", k=8)
    m_w = ar.mark()
    sc.barrier()
    stg = [ar.f32(NPROJ) for _ in range(2)]
    cast_engs = ["dve", "pool", "act"]
    ci = 0
    for kc in range(8):
        b = kc % 2
        op("sp", dma(stg[b], win_d[kc * 128:(kc + 1) * 128, :]), writes=[("stg", b)], dma=True)
        e = cast_engs[ci % 3]
        ci += 1
        op(e, (acp if e == "act" else cp)(w_in_bf[:, kc, :], stg[b]), reads=[("stg", b)], writes=["w_in_bf"])
    for kc in range(8):
        b = kc % 2
        op("sp", dma(stg[b][:, 0:D], wout_d[kc * 128:(kc + 1) * 128, :]), writes=[("stg", b)], dma=True)
        op("dve", tt(w_out_s[:, kc, :], stg[b][:, 0:D], gt1_bc, ALU.mult),
           reads=[("stg", b), "gt1_bc"], writes=["w_out_s"])
    for (src, dst, nm) in ((bdr_d, bdr_bf, "bdr"), (bdi_d, bdi_bf, "bdi")):
        op("sp", dma(stg[0][:, 0:512], src), writes=[("stg", 0)], dma=True)
        op("dve", cp(dst.rearrange("p c m -> p (c m)"), stg[0][:, 0:512]), reads=[("stg", 0)], writes=[nm])
    op("sp", dma(stg[1][0:16, 0:256], wa2_d), writes=[("stg", 1)], dma=True)
    op("dve", cp(wa2_bf[0:16, :], stg[1][0:16, 0:256]), reads=[("stg", 1)], writes=["wa2_bf"])
    op("sp", dma(stg[0][:, 0:288].rearrange("p (k n) -> p k n", k=8),
                 wr_d.rearrange("(k p) n -> p k n", p=128)), writes=[("stg", 0)], dma=True)
    op("dve", cp(wr_bf.rearrange("p k n -> p (k n)"), stg[0][:, 0:288]), reads=[("stg", 0)], writes=["wr_bf"])
    sc.barrier()
    ar.reset(m_w)

    if stop_after == "setup":
        op("sp", dma(dbg2_d[:, 0:48], modT), reads=["modT"], writes=["dbg2"], dma=True)
        op("sp", dma(dbg2_d[:, 1024:2048], gt1_bc), reads=["gt1_bc"], writes=["dbg2"], dma=True)
        op("sp", dma(dbg2_d[:, 64:80], ltab), reads=["ltab"], writes=["dbg2"], dma=True)
        sc.emit()
        return nc
    NJ = T // 128
    NCH = T // 64
    xs = [ar.f32(NJ * D).rearrange("p (j d) -> p j d", j=NJ) for _ in range(2)]
    xn = ar.bf(NJ * D).rearrange("p (j d) -> p j d", j=NJ)
    hT = ar.bf(8 * T).rearrange("p (k t) -> p k t", k=8)
    xc = ar.f32(4 * (T + 4)).rearrange("p (c t) -> p c t", c=4)
    yb = ar.f32(4 * T).rearrange("p (c t) -> p c t", c=4)
    qf = ar.f32(2 * T).rearrange("p (c t) -> p c t", c=2)
    kf = ar.f32(2 * T).rearrange("p (c t) -> p c t", c=2)
    gfb = ar.f32(4 * T).rearrange("p (c t) -> p c t", c=4)
    glT = ar.bf(T)
    vb = ar.bf(NJ * 512).rearrange("p (j e) -> p j e", j=NJ)
    catT = ar.bf(8 * T).rearrange("p (k t) -> p k t", k=8)
    L = []
    for _ in range(2):
        L.append(dict(cv=ar.f32(T), cvb=ar.bf(T), r=ar.f32(T), i=ar.f32(T), a=ar.f32(T),
                      s=ar.f32(T), h=ar.f32(T), t1=ar.f32(T), t2=ar.f32(T)))
    hprev = ar.f32(4)
    lf = ar.f32(2 * T).rearrange("p (c t) -> p c t", c=2)
    cs = ar.f32(2 * T).rearrange("p (c t) -> p c t", c=2)
    eb = ar.f32(2 * T).rearrange("p (c t) -> p c t", c=2)
    enb = ar.f32(2 * T).rearrange("p (c t) -> p c t", c=2)
    dec = ar.f32(2 * NCH).rearrange("p (c n) -> p c n", c=2)
    qd = ar.bf(2 * T).rearrange("p (c t) -> p c t", c=2)
    kd = ar.bf(2 * T).rearrange("p (c t) -> p c t", c=2)
    ke = ar.bf(2 * T).rearrange("p (c t) -> p c t", c=2)
    keT = ar.bf(NJ * 256).rearrange("p (j f) -> p j f", j=NJ)
    scT_sb = ar.bf(512)
    Sf = ar.f32(256).rearrange("p (c e) -> p c e", c=2)
    Sb = [ar.bf(256).rearrange("p (c e) -> p c e", c=2) for _ in range(2)]
    osq = ar.bf(T)
    sd = ar.f32(T)
    rs = ar.f32(T)
    sg = ar.f32(T)
    gs = ar.f32(T)
    to = ar.f32(T)
    stat = ar.f32(16)
    lg = ar.f32(NJ * 36).rearrange("p (j n) -> p j n", j=NJ)
    rt = ar.f32(NJ * 64).rearrange("p (j n) -> p j n", j=NJ)
    mf = ar.f32(NJ * 32).rearrange("p (j n) -> p j n", j=NJ)
    m8 = ar.f32(NJ * 8).rearrange("p (j n) -> p j n", j=NJ)
    oh = ar.f32(NJ * 64).rearrange("p (j n) -> p j n", j=NJ)
    Ot = ar.f32(NJ * 32).rearrange("p (j n) -> p j n", j=NJ)
    tmp32 = ar.f32(NJ * 64).rearrange("p (j n) -> p j n", j=NJ)
    print("arena used (words):", ar.off, "of", ARW)

    for c in range(4):
        op("pool", mset(xc[:, c, 0:4], 0.0), writes=[("xc", c)])
    op("pool", mset(hprev, 0.0), writes=["hprev"])
    op("pool", mset(Sf.rearrange("p c e -> p (c e)"), 0.0), writes=["Sf"])
    op("pool", mset(Sb[0].rearrange("p c e -> p (c e)"), 0.0), writes=[("Sb", 0)])
    op("pool", mset(Sb[1].rearrange("p c e -> p (c e)"), 0.0), writes=[("Sb", 1)])
    op("pool", mset(Ocum, 0.0), writes=["Ocum"])

    mmslot = [0]

    mmgroup = ["all"]
    MMB = {"all": [2, 3, 4], "front": [2, 3], "back": [4], "lru0": [2], "lru1": [3], "gla": [5]}

    def next_mm():
        banks = MMB[mmgroup[0]]
        bk = banks[mmslot[0] % len(banks)]
        mmslot[0] += 1
        return ps[bk], f"ps{bk}"

    psTb = [ps[0].bitcast(BF16), ps[1].bitcast(BF16)]
    psS = ps[5]
    psKV = ps[5]
    psO = [ps[6], ps[7]]
    qz = [[ar.bf(T) for _ in range(2)] for _ in range(2)]
    osq2 = ar.bf(2 * T)
    sd2 = ar.f32(2 * T)
    rs2 = ar.f32(2 * T)
    sg2 = ar.f32(2 * T)
    gs2 = ar.f32(2 * T)
    to2 = ar.f32(2 * T)
    print("arena used (words):", ar.off, "of", ARW)

    xnB = ar.bf(NJ * D).rearrange("p (j d) -> p j d", j=NJ)
    hTB = ar.bf(8 * T).rearrange("p (k t) -> p k t", k=8)
    catT2 = [catT, ar.bf(8 * T).rearrange("p (k t) -> p k t", k=8)]
    statB = ar.f32(16)
    print("arena used (words):", ar.off, "of", ARW)

    def norm_stats(xsrc_key, xbuf, xn_, st_, tag):
        sk = "stat" + tag
        for j in range(NJ):
            op("act", act(xn_[:, j, :], xbuf[:, j, :], AF.Square, accum_out=st_[:, j:j + 1]),
               reads=[xsrc_key], writes=[("xn" + tag, j), sk])
        op("act", act(st_[:, 2:4], st_[:, 0:2], AF.Sqrt, bias=EPS, scale=1.0 / D),
           reads=[sk], writes=[sk])
        op("dve", lambda e: e.reciprocal(out=st_[:, 4:6], in_=st_[:, 2:4]),
           reads=[sk], writes=[sk])
        for j in range(NJ):
            op("dve", ts(xn_[:, j, :], xbuf[:, j, :], st_[:, 4 + j:5 + j], ALU.mult),
               reads=[xsrc_key, sk], writes=[("xn" + tag, j)])

    def transposes_to_hT(scale_t, bias_t, xn_, hT_, tag, tb):
        for hb_ in range(2):
            hb = tb
            fns = []
            for k4 in range(4):
                kc = hb_ * 4 + k4
                for j in range(NJ):
                    fns.append(tr(psTb[hb][:, k4 * T + j * 128: k4 * T + (j + 1) * 128],
                                  xn_[:, j, kc * 128:(kc + 1) * 128], ident_bf))
            op("pe", fns, reads=[("xn" + tag, j) for j in range(NJ)] + ["ident_bf"], writes=[f"ps{hb}"])
            for k4 in range(4):
                kc = hb_ * 4 + k4
                src = psTb[hb][:, k4 * T:(k4 + 1) * T]
                op("act", act(hT_[:, kc, :], src, AF.Identity, bias=bias_t[:, kc:kc + 1],
                              scale=scale_t[:, kc:kc + 1]),
                   reads=[f"ps{hb}", "scale1", "scale2", "modT"], writes=[("hT" + tag, kc)])

    hT_keys = [("hTF", kc) for kc in range(8)]
    hTB_keys = [("hTB", kc) for kc in range(8)]
    ev_i = [0]

    def evac(dst, src, skey, dkey):
        e = "act" if (ev_i[0] // 2) % 2 == 0 else "dve"
        ev_i[0] += 1
        op(e, (acp if e == "act" else cp)(dst, src), reads=[skey], writes=[dkey])

    def load_x(t_):
        op("sp", dma(xs[t_ % 2], x_d[t_ * T:(t_ + 1) * T, :].rearrange("(j p) d -> p j d", p=128)),
           writes=[("xs", t_ % 2)], dma=True)

    def front(it):
        xb = it % 2
        X = xs[xb]
        xk = ("xs", xb)
        catT = catT2[it % 2]
        ctag = "catT%d" % (it % 2)
        norm_stats(xk, X, xn, stat, "F")
        mmgroup[0] = "front"
        transposes_to_hT(scale1, bias1, xn, hT, "F", 0)

        fm_list = []
        for c in range(4):
            fm_list.append((c * 128, xc[:, c, 4:4 + T], ("xc", c)))
        for c in range(4):
            fm_list.append((512 + c * 128, yb[:, c, :], ("yb", c)))
        for i in range(2):
            fm_list.append((1024 + i * 128, qf[:, i, :], ("qf", i)))
        for i in range(2):
            fm_list.append((1280 + i * 128, kf[:, i, :], ("kf", i)))
        for h in range(4):
            fm_list.append((2048 + h * 128, gfb[:, h, :], ("gf", h)))
        for pi_ in range(0, 16, 2):
            pso, key = next_mm()
            fns = []
            for u in range(2):
                col0 = fm_list[pi_ + u][0]
                fns += [mm(pso[:, u * T:(u + 1) * T], w_in_bf[:, kc, col0:col0 + 128], hT[:, kc, :],
                           start=(kc == 0), stop=(kc == 7)) for kc in range(8)]
            op("pe", fns, reads=hT_keys + ["w_in_bf"], writes=[key])
            for u in range(2):
                _, dst, dkey = fm_list[pi_ + u]
                evac(dst, pso[:, u * T:(u + 1) * T], key, dkey)
        pso, key = next_mm()
        fns = [mm(pso[0:16, 0:T], w_in_bf[:, kc, 2560:2576], hT[:, kc, :], start=(kc == 0), stop=(kc == 7))
               for kc in range(8)]
        op("pe", fns, reads=hT_keys + ["w_in_bf"], writes=[key])
        evac(glT[0:16, :], pso[0:16, 0:T], key, "glT")
        ev_i[0] += 1
        for j in range(NJ):
            pso, key = next_mm()
            fns = [mm(pso, hT[:, kc, j * 128:(j + 1) * 128], w_in_bf[:, kc, 1536:2048],
                      start=(kc == 0), stop=(kc == 7)) for kc in range(8)]
            op("pe", fns, reads=hT_keys + ["w_in_bf"], writes=[key])
            evac(vb[:, j, :], pso, key, ("vb", j))
            ev_i[0] += 1

        lru_lists = {0: [], 1: []}
        for c in range(4):
            mmgroup[0] = "lru%d" % (c % 2)
            sc.rec_begin()
            B = L[c % 2]
            lk = ("L", c % 2)
            cw = tab[:, TB_CW + c * 4:TB_CW + c * 4 + 4]
            op("dve", ts(B["cv"], xc[:, c, 4:4 + T], cw[:, 3:4], ALU.mult, tab[:, TB_CB + c:TB_CB + c + 1], ALU.add),
               reads=[("xc", c), "tab"], writes=[lk + ("cv",)])
            for k in range(3):
                op("dve", stt(B["cv"], xc[:, c, 1 + k:1 + k + T], cw[:, k:k + 1], B["cv"], ALU.mult, ALU.add),
                   reads=[("xc", c), "tab", lk + ("cv",)], writes=[lk + ("cv",)])
            op("pool", cp(B["cvb"], B["cv"]), reads=[lk + ("cv",)], writes=[lk + ("cvb",)])
            op("pool", cp(xc[:, c, 0:4], xc[:, c, T:T + 4]), reads=[("xc", c)], writes=[("xc", c)])
            pg, kg = next_mm()
            op("pe", [mm(pg[:, 0:T], bdr_bf[:, c, :], B["cvb"]), mm(pg[:, T:2 * T], bdi_bf[:, c, :], B["cvb"])],
               reads=[lk + ("cvb",), "bdr", "bdi"], writes=[kg])
            op("act", act(B["r"], pg[:, 0:T], AF.Sigmoid, bias=tab[:, TB_BR + c:TB_BR + c + 1]),
               reads=[kg, "tab"], writes=[lk + ("r",)])
            op("act", act(B["i"], pg[:, T:2 * T], AF.Sigmoid, bias=tab[:, TB_BI + c:TB_BI + c + 1]),
               reads=[kg, "tab"], writes=[lk + ("i",)])
            op("act", act(B["a"], B["r"], AF.Exp, scale=cl[:, c:c + 1]),
               reads=[lk + ("r",), "ltab"], writes=[lk + ("a",)])
            op("act", act(B["s"], B["r"], AF.Exp, scale=c2l[:, c:c + 1]),
               reads=[lk + ("r",), "ltab"], writes=[lk + ("s",)])
            op("act", act(B["s"], B["s"], AF.Sqrt, bias=1.0, scale=-1.0),
               reads=[lk + ("s",)], writes=[lk + ("s",)])
            op("pool", tt(B["i"], B["i"], B["cv"], ALU.mult), reads=[lk + ("i",), lk + ("cv",)], writes=[lk + ("i",)])
            op("pool", tt(B["i"], B["i"], B["s"], ALU.mult), reads=[lk + ("i",), lk + ("s",)], writes=[lk + ("i",)])
            op("dve", scan(B["h"], B["a"], B["i"], hprev[:, c:c + 1], ALU.mult, ALU.add),
               reads=[lk + ("a",), lk + ("i",), "hprev"], writes=[lk + ("h",)])
            op("pool", cp(hprev[:, c:c + 1], B["h"][:, T - 1:T]), reads=[lk + ("h",)], writes=["hprev"])
            Y = yb[:, c, :]
            op("pool", tt(B["t1"], Y, Y, ALU.mult), reads=[("yb", c)], writes=[lk + ("t1",)])
            op("pool", ts(B["t1"], B["t1"], 0.044715, ALU.mult, 1.0, ALU.add),
               reads=[lk + ("t1",)], writes=[lk + ("t1",)])
            op("pool", tt(B["t1"], B["t1"], Y, ALU.mult), reads=[lk + ("t1",), ("yb", c)], writes=[lk + ("t1",)])
            op("act", act(B["t2"], B["t1"], AF.Sigmoid, scale=1.5957691216057308),
               reads=[lk + ("t1",)], writes=[lk + ("t2",)])
            op("pool", tt(B["t2"], B["t2"], Y, ALU.mult), reads=[lk + ("t2",), ("yb", c)], writes=[lk + ("t2",)])
            op("dve", tt(catT[:, c, :], B["h"], B["t2"], ALU.mult),
               reads=[lk + ("h",), lk + ("t2",)], writes=[(ctag, c)])
            lru_lists[c % 2] += sc.rec_end()
        mmgroup[0] = "gla"
        sc.rec_begin()

        pz, kz = next_mm()
        op("pe", [mm(pz[:, i * T:(i + 1) * T], wa2_bf[0:16, i * 128:(i + 1) * 128], glT[0:16, :]) for i in range(2)],
           reads=["glT", "wa2_bf"], writes=[kz])
        for i in range(2):
            op("act", act(lf[:, i, :], pz[:, i * T:(i + 1) * T], AF.Exp, bias=nba[:, i:i + 1], scale=-1.0),
               reads=[kz, "ltab"], writes=[("lf", i)])
            op("act", act(lf[:, i, :], lf[:, i, :], AF.Ln, bias=1.0), reads=[("lf", i)], writes=[("lf", i)])
            op("dve", scan(cs[:, i, :], rmask[:, 0:T], lf[:, i, :], 0.0, ALU.mult, ALU.add),
               reads=[("lf", i), "cst"], writes=[("cs", i)])
            op("act", act(eb[:, i, :], cs[:, i, :], AF.Exp, scale=-1.0 / 16), reads=[("cs", i)], writes=[("eb", i)])
            op("act", act(enb[:, i, :], cs[:, i, :], AF.Exp, scale=1.0 / 16), reads=[("cs", i)], writes=[("enb", i)])
            op("act", act(dec[:, i, 0:NJ], cs[:, i, :].rearrange("p (n t) -> p n t", t=128)[:, :, 127],
                          AF.Exp, scale=-1.0 / 16), reads=[("cs", i)], writes=[("dec", i)])
            op("dve", stt(qd[:, i, :], qf[:, i, :], 0.125, eb[:, i, :], ALU.mult, ALU.mult),
               reads=[("qf", i), ("eb", i)], writes=[("qd", i)])
            for hh in range(2):
                op("pool", ts(qz[i][hh], qd[:, i, :], hmask[:, hh:hh + 1], ALU.mult),
                   reads=[("qd", i), "cst"], writes=[("qz", i, hh)])
            op("dve", tt(kd[:, i, :], kf[:, i, :], enb[:, i, :], ALU.mult),
               reads=[("kf", i), ("enb", i)], writes=[("kd", i)])
            op("pool", tt(ke[:, i, :].rearrange("p (n t) -> p n t", t=128),
                          kd[:, i, :].rearrange("p (n t) -> p n t", t=128),
                          dec[:, i, 0:NJ].unsqueeze(2).to_broadcast([128, NJ, 128]), ALU.mult),
               reads=[("kd", i), ("dec", i)], writes=[("ke", i)])
        fns = []
        for j in range(NJ):
            for i in range(2):
                fns.append(tr(psTb[0][:, (j * 2 + i) * 128:(j * 2 + i + 1) * 128], ke[:, i, j * 128:(j + 1) * 128], ident_bf))
        op("pe", fns, reads=[("ke", 0), ("ke", 1), "ident_bf"], writes=["ps0"])
        op("act", acp(keT.rearrange("p j f -> p (j f)"), psTb[0][:, 0:NJ * 256]), reads=["ps0"], writes=["keT"])
        for j in range(NJ):
            par = (it * NJ + j) % 2
            tsl = slice(j * 128, (j + 1) * 128)
            fns = []
            for h in range(4):
                i, hh = h // 2, h % 2
                fns.append(mm(psS[:, h * 128:(h + 1) * 128], kd[:, i, tsl], qz[i][hh][:, tsl]))
            op("pe", fns, reads=[("kd", 0), ("kd", 1)] + [("qz", i, hh) for i in range(2) for hh in range(2)],
               writes=["ps5"])
            op("dve", tt(scT_sb.rearrange("p (a c) -> p a c", c=128),
                         psS.rearrange("p (a c) -> p a c", c=128),
                         cmask.unsqueeze(1).to_broadcast([128, 4, 128]), ALU.mult),
               reads=["ps5", "cst"], writes=["scT"])
            for i in range(2):
                fns = []
                for hh in range(2):
                    h = 2 * i + hh
                    o_ap = psO[i][:, hh * T + j * 128: hh * T + (j + 1) * 128]
                    fns.append(mm(o_ap, vb[:, j, h * 128:(h + 1) * 128], scT_sb[:, h * 128:(h + 1) * 128],
                                  start=True, stop=False))
                    fns.append(mm(o_ap, Sb[par][:, i, :], qz[i][hh][:, tsl], start=False, stop=True))
                op("pe", fns, reads=[("vb", j), "scT", ("Sb", par), ("qz", i, 0), ("qz", i, 1)], writes=[f"ps{6 + i}"])
            fns = []
            for h in range(4):
                i = h // 2
                fns.append(mm(psKV[:, h * 128:(h + 1) * 128], keT[:, j, i * 128:(i + 1) * 128],
                              vb[:, j, h * 128:(h + 1) * 128]))
            op("pe", fns, reads=["keT", ("vb", j)], writes=["ps5"])
            for h in range(4):
                i, hh = h // 2, h % 2
                r0, r1 = hh * 64, (hh + 1) * 64
                op("dve", stt(Sf[r0:r1, i, :], Sf[r0:r1, i, :], dec[r0:r1, i, j:j + 1],
                              psKV[r0:r1, h * 128:(h + 1) * 128], ALU.mult, ALU.add),
                   reads=["Sf", ("dec", i), "ps5"], writes=["Sf"])
            op("act", acp(Sb[1 - par].rearrange("p c e -> p (c e)"), Sf.rearrange("p c e -> p (c e)")),
               reads=["Sf"], writes=[("Sb", 1 - par)])
        for i in range(2):
            O = psO[i]
            ok = f"ps{6 + i}"
            op("act", act(osq2, O, AF.Square), reads=[ok], writes=["osq"])
            pss, kss = next_mm()
            op("pe", mm(pss, ones_bf, osq2), reads=["osq", "ones_bf"], writes=[kss])
            op("act", act(sd2, pss, AF.Sqrt, bias=EPS, scale=1.0 / 128), reads=[kss], writes=["sd"])
            op("dve", lambda e: e.reciprocal(out=rs2, in_=sd2), reads=["sd"], writes=["rs"])
            G = gfb[:, 2 * i:2 * i + 2, :].rearrange("p c t -> p (c t)")
            gk = [("gf", 2 * i), ("gf", 2 * i + 1)]
            op("act", act(sg2, G, AF.Sigmoid), reads=gk, writes=["sg"])
            op("dve", stt(gs2, G, tab[:, TB_GN:TB_GN + 1], sg2, ALU.mult, ALU.mult),
               reads=gk + ["sg", "tab"], writes=["gs"])
            op("dve", tt(to2, O, rs2, ALU.mult), reads=[ok, "rs"], writes=["to"])
            op("dve", tt(catT[:, 4 + 2 * i:6 + 2 * i, :].rearrange("p c t -> p (c t)"), to2, gs2, ALU.mult),
               reads=["to", "gs"], writes=[(ctag, 4 + 2 * i), (ctag, 5 + 2 * i)])

        gla_list = sc.rec_end()
        sc.play(Sched.merge([lru_lists[0], lru_lists[1], gla_list]))

    def back(it):
        xb = it % 2
        X = xs[xb]
        xk = ("xs", xb)
        catT = catT2[it % 2]
        ctag = "catT%d" % (it % 2)
        mmgroup[0] = "back"
        cat_keys = [(ctag, k) for k in range(8)]
        for j in range(NJ):
            for half in range(2):
                pso, key = next_mm()
                fns = [mm(pso, catT[:, kc, j * 128:(j + 1) * 128], w_out_s[:, kc, half * 512:(half + 1) * 512],
                          start=(kc == 0), stop=(kc == 7)) for kc in range(8)]
                op("pe", fns, reads=cat_keys + ["w_out_s"], writes=[key])
                op("dve", tt(X[:, j, half * 512:(half + 1) * 512], X[:, j, half * 512:(half + 1) * 512], pso, ALU.add),
                   reads=[key, xk], writes=[xk])
        op("sp", dma(xmid_d[it * T:(it + 1) * T, :].rearrange("(j p) d -> p j d", p=128), X),
           reads=[xk], writes=["xmid_scr"], dma=True)
        if dbg and stop_after == "mixer":
            op("sp", dma(dbg_d[it * T:(it + 1) * T, :].rearrange("(j p) d -> p j d", p=128), X),
               reads=[xk], writes=["dbg"], dma=True)
            if it + 2 < NT:
                load_x(it + 2)
            return

        norm_stats(xk, X, xnB, statB, "B")
        op("sp", dma(xn2_d[it * T:(it + 1) * T, :].rearrange("(j p) d -> p j d", p=128), xnB),
           reads=[("xnB", j) for j in range(NJ)], writes=["xn2_scr"], dma=True)
        if it + 2 < NT:
            load_x(it + 2)
        transposes_to_hT(scale2, bias2, xnB, hTB, "B", 1)
        pso, key = next_mm()
        fns = []
        for j in range(NJ):
            for kc in range(8):
                fns.append(mm(pso[:, j * 64:j * 64 + 36], hTB[:, kc, j * 128:(j + 1) * 128], wr_bf[:, kc, :],
                              start=(kc == 0), stop=(kc == 7)))
        op("pe", fns, reads=hTB_keys + ["wr_bf"], writes=[key])
        for j in range(NJ):
            op("dve", tt(lg[:, j, :], pso[:, j * 64:j * 64 + 36], brb, ALU.add), reads=[key, "brb"], writes=[("rt", j)])
        for j in range(NJ):
            st = it * NJ + j
            rk = ("rt", j)
            op("dve", red(rt[:, j, 0:1], lg[:, j, 0:4], ALU.max), reads=[rk], writes=[rk])
            op("dve", ts(rt[:, j, 4:8], lg[:, j, 0:4], rt[:, j, 0:1], ALU.is_equal), reads=[rk], writes=[rk])
            op("dve", ts(rt[:, j, 8:12], lg[:, j, 0:4], rt[:, j, 0:1], ALU.subtract), reads=[rk], writes=[rk])
            op("act", act(rt[:, j, 8:12], rt[:, j, 8:12], AF.Exp, accum_out=rt[:, j, 1:2]), reads=[rk], writes=[rk])
            op("dve", lambda e, j=j: e.reciprocal(out=rt[:, j, 2:3], in_=rt[:, j, 1:2]), reads=[rk], writes=[rk])
            op("dve", ts(rt[:, j, 12:16], rt[:, j, 4:8], 1e30, ALU.mult, -1e30, ALU.add), reads=[rk], writes=[rk])
            op("dve", tt(mf[:, j, :].rearrange("p (g e) -> p g e", g=4),
                         lg[:, j, 4:36].rearrange("p (g e) -> p g e", g=4),
                         rt[:, j, 12:16].unsqueeze(2).to_broadcast([128, 4, 8]), ALU.add), reads=[rk], writes=[rk])
            op("dve", lambda e, j=j: e.max(out=m8[:, j, :], in_=mf[:, j, :]), reads=[rk], writes=[rk])
            op("dve", ts(oh[:, j, 0:32], mf[:, j, :], m8[:, j, 0:1], ALU.is_equal), reads=[rk], writes=[rk])
            op("dve", ts(oh[:, j, 32:64], mf[:, j, :], m8[:, j, 1:2], ALU.is_equal), reads=[rk], writes=[rk])
            op("dve", cp(ohs[:, st * 64:(st + 1) * 64], oh[:, j, :]), reads=[rk], writes=["ohs"])
            op("dve", tt(rt[:, j, 16:17], m8[:, j, 1:2], m8[:, j, 0:1], ALU.subtract), reads=[rk], writes=[rk])
            op("act", act(rt[:, j, 17:18], rt[:, j, 16:17], AF.Exp), reads=[rk], writes=[rk])
            op("dve", ts(rt[:, j, 18:19], rt[:, j, 17:18], 1.0, ALU.add), reads=[rk], writes=[rk])
            op("dve", lambda e, j=j: e.reciprocal(out=rt[:, j, 19:20], in_=rt[:, j, 18:19]), reads=[rk], writes=[rk])
            op("dve", tt(wts_all[:, st * 2:st * 2 + 1], rt[:, j, 19:20], rt[:, j, 2:3], ALU.mult),
               reads=[rk], writes=["wts_all"])
            op("dve", tt(wts_all[:, st * 2 + 1:st * 2 + 2], wts_all[:, st * 2:st * 2 + 1], rt[:, j, 17:18], ALU.mult),
               reads=[rk, "wts_all"], writes=["wts_all"])
            op("dve", tt(Ot[:, j, :], oh[:, j, 0:32], oh[:, j, 32:64], ALU.add), reads=[rk], writes=[("Ot", j)])
            pp, kp = next_mm()
            op("pe", [mm(pp[:, 0:32], Umat, Ot[:, j, :], start=True, stop=False),
                      mm(pp[:, 0:32], ones_f, Ocum, start=False, stop=True)],
               reads=[("Ot", j), "Ocum", "cst", "ones_f"], writes=[kp])
            op("dve", tt(Ocum, Ocum, Ot[:, j, :], ALU.add), reads=[("Ot", j), "Ocum"], writes=["Ocum"])
            for k in range(2):
                o_k = oh[:, j, k * 32:(k + 1) * 32]
                op("dve", tt(tmp32[:, j, 0:32], o_k, pp[:, 0:32], ALU.mult), reads=[rk, kp], writes=[("tmp32", j)])
                op("dve", red(pos_all[:, st * 2 + k:st * 2 + k + 1], tmp32[:, j, 0:32], ALU.add),
                   reads=[("tmp32", j)], writes=["pos_all"])
                op("dve", tt(tmp32[:, j, 32:64], o_k, iota32, ALU.mult), reads=[rk, "cst"], writes=[("tmp32", j)])
                op("dve", red(eid_all[:, st * 2 + k:st * 2 + k + 1], tmp32[:, j, 32:64], ALU.add),
                   reads=[("tmp32", j)], writes=["eid_all"])
                op("dve", stt(rt[:, j, 20 + k:21 + k], eid_all[:, st * 2 + k:st * 2 + k + 1], float(CAP),
                              pos_all[:, st * 2 + k:st * 2 + k + 1], ALU.mult, ALU.add),
                   reads=["eid_all", "pos_all", rk], writes=[rk])
                op("dve", cp(slot_u[:, st * 2 + k:st * 2 + k + 1], rt[:, j, 20 + k:21 + k]), reads=[rk], writes=["slot_u"])
                op("pool", cp(pay_i[:, (st * 2 + k) * 2:(st * 2 + k) * 2 + 1], tokid[:, st:st + 1]),
                   reads=["tokid"], writes=[("pay", st, k)])
                op("pool", cp(pay_f[:, (st * 2 + k) * 2 + 1:(st * 2 + k) * 2 + 2], wts_all[:, st * 2 + k:st * 2 + k + 1]),
                   reads=["wts_all", ("pay", st, k)], writes=[("pay", st, k)])
                sl = slot_u[:, st * 2 + k:st * 2 + k + 1]
                py = pay_i[:, (st * 2 + k) * 2:(st * 2 + k) * 2 + 2]
                op("pool", lambda e, sl=sl, py=py: e.indirect_dma_start(
                    out=sidx_d[:, :], out_offset=bass.IndirectOffsetOnAxis(ap=sl, axis=0),
                    in_=py, in_offset=None), reads=["slot_u", ("pay", st, k)], writes=["sidx_scr"], dma=True)

    load_x(0)
    if NT > 1:
        load_x(1)
    front(0)
    for it in range(NT):
        lists = []
        if it + 1 < NT:
            sc.rec_begin()
            front(it + 1)
            lists.append(sc.rec_end())
        sc.rec_begin()
        back(it)
        lists.append(sc.rec_end())
        sc.play(Sched.merge(lists))
    mmgroup[0] = "all"

    if stop_after in ("mixer", "route"):
        if dbg and stop_after == "route":
            op("sp", dma(dbg2_d[:, 0:NST * 2], pos_all), reads=["pos_all"], writes=["dbg2"], dma=True)
            op("sp", dma(dbg2_d[:, 1024:1024 + NST * 2], eid_all), reads=["eid_all"], writes=["dbg2"], dma=True)
            op("sp", dma(dbg2_d[:, 2048:2048 + NST * 2], wts_all), reads=["wts_all"], writes=["dbg2"], dma=True)
        sc.emit()
        return nc

    sc.barrier()
    ar.reset(m0)
    thr = cst[:, CS_TH:CS_TH + NTH]
    cnt = ar.f32(32)
    big_full = ar.f32(max(NST * 2 * 32, 32 * NTH))
    big = big_full[:, 0:NST * 2 * 32]
    nblk = ar.f32(32)
    padded = ar.f32(32)
    pends = ar.f32(32)
    ebase = ar.f32(32)
    bt = ar.f32(64)
    slc_f = ar.f32(NST * 2)
    op("pe", mm(ps[2][:, 0:32], ones_f, Ocum), reads=["Ocum", "ones_f"], writes=["ps2"])
    op("dve", cp(cnt, ps[2][:, 0:32]), reads=["ps2"], writes=["cnt"])
    op("dve", tt(big_full[:, 0:32 * NTH].rearrange("p (e m) -> p e m", m=NTH),
                 cnt.unsqueeze(2).to_broadcast([128, 32, NTH]),
                 thr.unsqueeze(1).to_broadcast([128, 32, NTH]), ALU.is_gt),
       reads=["cnt", "cst"], writes=["big"])
    op("dve", red(nblk, big_full[:, 0:32 * NTH].rearrange("p (e m) -> p e m", m=NTH), ALU.add),
       reads=["big"], writes=["nblk"])
    op("dve", ts(padded, nblk, float(BLK), ALU.mult), reads=["nblk"], writes=["padded"])
    op("dve", scan(pends, ones_f[:, 0:32], padded, 0.0, ALU.mult, ALU.add), reads=["padded", "ones_f"], writes=["pends"])
    op("dve", tt(ebase, pends, padded, ALU.subtract), reads=["pends", "padded"], writes=["ebase"])
    op("dve", ts(bt[:, 0:32], pends, blk512, ALU.is_le), reads=["pends", "cst"], writes=["bt"])
    op("dve", red(bt[:, 32:33], bt[:, 0:32], ALU.add), reads=["bt"], writes=["bt"])
    op("dve", ts(bt[:, 32:33], bt[:, 32:33], float(NE - 1), ALU.min), reads=["bt"], writes=["bt"])
    op("dve", ts(bt[:, 0:32], iota32, bt[:, 32:33], ALU.is_equal), reads=["bt", "cst"], writes=["bt"])
    op("dve", tt(bt[:, 0:32], bt[:, 0:32], ebase, ALU.mult), reads=["bt", "ebase"], writes=["bt"])
    op("dve", red(bt[:, 33:34], bt[:, 0:32], ALU.add), reads=["bt"], writes=["bt"])
    op("dve", ts(bt[:, 34:35], blk512, pends[:, 31:32], ALU.is_lt), reads=["pends", "cst"], writes=["bt"])
    op("dve", stt(bt[:, 35:36], bt[:, 32:33], float(CAP), blk512, ALU.mult, ALU.add), reads=["bt", "cst"], writes=["bt"])
    op("dve", tt(bt[:, 35:36], bt[:, 35:36], bt[:, 33:34], ALU.subtract), reads=["bt"], writes=["bt"])
    op("dve", ts(bt[:, 35:36], bt[:, 35:36], float(-NULLSTART), ALU.add), reads=["bt"], writes=["bt"])
    op("dve", tt(bt[:, 35:36], bt[:, 35:36], bt[:, 34:35], ALU.mult), reads=["bt"], writes=["bt"])
    op("dve", ts(bt[:, 35:36], bt[:, 35:36], float(NULLSTART), ALU.add), reads=["bt"], writes=["bt"])
    Gb = ar.f32(256).rearrange("p (a m) -> p a m", a=2)
    bcf = ar.f32(256).rearrange("p (a m) -> p a m", a=2)
    pcol = ar.f32(2)
    op("dve", ts(Gb[:, 0, :], ones_f, bt[:, 32:33], ALU.mult), reads=["bt", "ones_f"], writes=["Gb"])
    op("dve", ts(Gb[:, 1, :], ones_f, bt[:, 35:36], ALU.mult), reads=["bt", "ones_f"], writes=["Gb"])
    op("pe", [mm(ps[3][:, 0:128], Gb[:, 0, :], ident_f), mm(ps[3][:, 128:256], Gb[:, 1, :], ident_f)],
       reads=["Gb", "cst"], writes=["ps3"])
    op("dve", cp(bcf.rearrange("p a m -> p (a m)"), ps[3][:, 0:256]), reads=["ps3"], writes=["bcf"])
    op("dve", ts(pcol[:, 0:1], blk512, 1.0 / BLK, ALU.mult), reads=["cst"], writes=["pcol"])
    op("dve", ts(bcf[:, 0, :], bcf[:, 0, :], 128.0, ALU.mult, pcol[:, 0:1], ALU.add), reads=["bcf", "pcol"], writes=["bcf"])
    op("dve", cp(widx, bcf[:, 0, :]), reads=["bcf"], writes=["widx"])
    op("dve", ts(bcf[:, 1, :], bcf[:, 1, :], 0.25, ALU.mult, pcol[:, 0:1], ALU.add), reads=["bcf", "pcol"], writes=["bcf"])
    op("dve", cp(sidx4[:, 0:128], bcf[:, 1, :]), reads=["bcf"], writes=["sidx4"])
    op("dve", tt(big.rearrange("p (a e) -> p a e", e=32), ohs.rearrange("p (a e) -> p a e", e=32),
                 ebase.unsqueeze(1).to_broadcast([128, NST * 2, 32]), ALU.mult),
       reads=["ohs", "ebase", "big"], writes=["big"])
    op("dve", red(slc_f, big.rearrange("p (a e) -> p a e", e=32), ALU.add), reads=["big"], writes=["slc_f"])
    op("dve", tt(slc_f, slc_f, pos_all, ALU.add), reads=["slc_f", "pos_all"], writes=["slc_f"])
    op("dve", cp(slot_c, slc_f), reads=["slc_f"], writes=["slot_c"])
    if dbg and stop_after == "tables":
        op("sp", dma(dbg2_d[:, 0:128], widx.bitcast(F32)), reads=["widx"], writes=["dbg2"], dma=True)
        op("sp", dma(dbg2_d[:, 512:1024], sidx4.bitcast(F32)), reads=["sidx4"], writes=["dbg2"], dma=True)
        op("sp", dma(dbg2_d[:, 1024:1024 + NST * 2], slot_c.bitcast(F32)), reads=["slot_c"], writes=["dbg2"], dma=True)
        op("sp", dma(dbg2_d[:, 256:288], pends), reads=["pends"], writes=["dbg2"], dma=True)
        sc.emit()
        return nc

    m3 = ar.mark()
    NSTG = 3
    stg3 = [ar.f32(4096) for _ in range(NSTG)]
    wg_bf = [ar.bf(8 * 512).rearrange("p (k f) -> p k f", k=8) for _ in range(2)]
    wu_bf = [ar.bf(8 * 512).rearrange("p (k f) -> p k f", k=8) for _ in range(2)]
    wd_bf = [ar.bf(4 * 1024).rearrange("p (k f) -> p k f", k=4) for _ in range(2)]
    Xg = [ar.bf(4 * D).rearrange("p (j d) -> p j d", j=4) for _ in range(2)]
    h2T = ar.bf(8 * BLK).rearrange("p (k t) -> p k t", k=8)
    hidT = ar.bf(4 * BLK).rearrange("p (k t) -> p k t", k=4)
    sgb = [ar.f32(BLK) for _ in range(2)]
    ysb = [ar.f32(D) for _ in range(2)]
    print("arena used phase3 (words):", ar.off, "of", ARW)
    stg_i = [0]
    cast_i = [0]
    wgv = wg_d.rearrange("e (p kk) f -> (e p) (kk f)", kk=8)
    wuv = wu_d.rearrange("e (p kk) f -> (e p) (kk f)", kk=8)
    wdv = wd_d.rearrange("e (p kk) f -> (e p) (kk f)", kk=4)

    def issue_loads(b):
        q = b % 2
        op("pool", lambda e, q=q, b=b: e.indirect_dma_start(
            out=idx_sb[q][:, 0:8], out_offset=None, in_=sidx_d.rearrange("(r f) c -> r (f c)", f=4),
            in_offset=bass.IndirectOffsetOnAxis(ap=sidx4[:, b:b + 1], axis=0)),
           reads=["sidx4", "sidx_scr"], writes=[("idx", q)], dma=True)
        casts = []
        for (wv, dst, nm) in ((wgv, wg_bf[q], "wg"), (wuv, wu_bf[q], "wu"), (wdv, wd_bf[q], "wd")):
            sgi = stg_i[0] % NSTG
            stg_i[0] += 1
            op("pool", lambda e, wv=wv, sgi=sgi, b=b: e.indirect_dma_start(
                out=stg3[sgi], out_offset=None, in_=wv,
                in_offset=bass.IndirectOffsetOnAxis(ap=widx[:, b:b + 1], axis=0)),
               reads=["widx"], writes=[("stg3", sgi)], dma=True)
            casts.append((dst, nm, sgi))
        for j in range(4):
            op("pool", lambda e, j=j, q=q: e.indirect_dma_start(
                out=Xg[q][:, j, :], out_offset=None, in_=xn2_d[:, :],
                in_offset=bass.IndirectOffsetOnAxis(ap=idx_sb[q][:, 2 * j:2 * j + 1], axis=0)),
               reads=[("idx", q), "xn2_scr"], writes=[("Xg", q, j)], dma=True)
        sc.rec_begin()
        for (dst, nm, sgi) in casts:
            dflat = dst.rearrange("p k f -> p (k f)")
            for hf in range(2):
                sl = slice(hf * 2048, (hf + 1) * 2048)
                if nm != "wd":
                    ce = ("act", "pool", "dve", "act")[cast_i[0] % 4]
                    cast_i[0] += 1
                    op(ce, (acp if ce == "act" else cp)(dflat[:, sl], stg3[sgi][:, sl]),
                       reads=[("stg3", sgi)], writes=[(nm, q, hf)])
                else:
                    ce = ("dve", "pool")[hf]
                    op(ce, tt(dflat[:, sl].rearrange("p (k f) -> p k f", k=2),
                              stg3[sgi][:, sl].rearrange("p (k f) -> p k f", k=2),
                              gt2_bc.unsqueeze(1).to_broadcast([128, 2, D]), ALU.mult),
                       reads=[("stg3", sgi), "gt2_bc"], writes=[(nm, q, hf)])
        return sc.rec_end()

    h2_keys = [("h2T", kc) for kc in range(8)]
    hid_keys = [("hidT", hc) for hc in range(4)]

    def stageA(b):
        q = b % 2
        for r4 in range(4):
            hb = r4 % 2
            fns = []
            for u in range(2):
                kc = r4 * 2 + u
                for j in range(4):
                    fns.append(tr(psTb[hb][:, u * BLK + j * 128:u * BLK + (j + 1) * 128],
                                  Xg[q][:, j, :].rearrange("p (m kk) -> p kk m", kk=8)[:, kc, :], ident_bf))
            op("pe", fns, reads=[("Xg", q, j) for j in range(4)] + ["ident_bf"], writes=[f"ps{hb}"])
            for u in range(2):
                kc = r4 * 2 + u
                op("act", act(h2T[:, kc, :], psTb[hb][:, u * BLK:(u + 1) * BLK], AF.Identity,
                              bias=bias2p[:, kc:kc + 1], scale=scale2p[:, kc:kc + 1]),
                   reads=[f"ps{hb}", "scale2p", "modP"], writes=[("h2T", kc)])

    def stageG(b):
        q = b % 2
        for hc in range(4):
            pg, kg = next_mm()
            op("pe", [mm(pg, wg_bf[q][:, kc, :].rearrange("p (m c) -> p c m", c=4)[:, hc, :], h2T[:, kc, :], start=(kc == 0), stop=(kc == 7))
                      for kc in range(8)], reads=h2_keys + [("wg", q, 0), ("wg", q, 1)], writes=[kg])
            pu, ku = next_mm()
            op("pe", [mm(pu, wu_bf[q][:, kc, :].rearrange("p (m c) -> p c m", c=4)[:, hc, :], h2T[:, kc, :], start=(kc == 0), stop=(kc == 7))
                      for kc in range(8)], reads=h2_keys + [("wu", q, 0), ("wu", q, 1)], writes=[ku])
            sgk = ("sgb", hc % 2)
            op("act", act(sgb[hc % 2], pg, AF.Sigmoid), reads=[kg], writes=[sgk])
            op("dve", tt(sgb[hc % 2], sgb[hc % 2], pg, ALU.mult), reads=[kg, sgk], writes=[sgk])
            op("dve", tt(hidT[:, hc, :], sgb[hc % 2], pu, ALU.mult), reads=[ku, sgk], writes=[("hidT", hc)])

    def stageD(b):
        q = b % 2
        for j in range(4):
            yq = (b * 4 + j) % 2
            for half in range(2):
                pd, kd_ = next_mm()
                op("pe", [mm(pd, hidT[:, hc, j * 128:(j + 1) * 128], wd_bf[q][:, hc, half * 512:(half + 1) * 512],
                             start=(hc == 0), stop=(hc == 3)) for hc in range(4)],
                   reads=hid_keys + [("wd", q, 0), ("wd", q, 1)], writes=[kd_])
                wtok = idx_sb[q].bitcast(F32)[:, 2 * j + 1:2 * j + 2]
                op("act", act(ysb[yq][:, half * 512:(half + 1) * 512], pd, AF.Identity, scale=wtok),
                   reads=[kd_, ("idx", q)], writes=[("ysb", yq)])
            op("sp", dma(ybuf_d[b * BLK:(b + 1) * BLK, :].rearrange("(p j) d -> p j d", j=4)[:, j, :], ysb[yq]),
               reads=[("ysb", yq)], writes=[("ybuf", b, j)], dma=True)

    sc.play(issue_loads(0))
    stageA(0)
    for b in range(NB):
        lists = []
        if b + 1 < NB:
            lists.append(issue_loads(b + 1))
        stageG(b)
        if b + 1 < NB:
            sc.rec_begin()
            stageA(b + 1)
            lists.append(sc.rec_end())
        sc.rec_begin()
        stageD(b)
        lists.append(sc.rec_end())
        sc.play(Sched.merge(lists))

    sc.barrier()
    ar.reset(m3)
    def _final_compute(st):
        f = F[st % NF]
        fk = ("F", st % NF)
        op("dve", tt(f["xm"], f["xm"], f["y0"], ALU.add), reads=[fk + ("xm",), fk + ("y0",)], writes=[fk + ("xm",)])
        op("dve", tt(f["xm"], f["xm"], f["y1"], ALU.add), reads=[fk + ("xm",), fk + ("y1",)], writes=[fk + ("xm",)])
        op("act", act(f["jk"], f["xm"], AF.Square, accum_out=f["st"][:, 0:1]), reads=[fk + ("xm",)], writes=[fk + ("st",), fk + ("jk",)])
        op("act", act(f["st"][:, 1:2], f["st"][:, 0:1], AF.Sqrt, bias=EPS, scale=1.0 / D), reads=[fk + ("st",)], writes=[fk + ("st",)])
        op("dve", lambda e, f=f: e.reciprocal(out=f["st"][:, 2:3], in_=f["st"][:, 1:2]), reads=[fk + ("st",)], writes=[fk + ("st",)])
        op("dve", stt(f["y0"], f["xm"], f["st"][:, 2:3], gf_bc, ALU.mult, ALU.mult),
           reads=[fk + ("xm",), fk + ("st",), "gf_bc"], writes=[fk + ("y0",)])
        op("sp", dma(out_d[st * 128:(st + 1) * 128, :], f["y0"]), reads=[fk + ("y0",)], writes=[("out", st)], dma=True)

    NF = 4
    F = [dict(xm=ar.f32(D), y0=ar.f32(D), y1=ar.f32(D), jk=ar.bf(D), st=ar.f32(4)) for _ in range(NF)]
    for st in range(NST):
        f = F[st % NF]
        fk = ("F", st % NF)
        op("sp", dma(f["xm"], xmid_d[st * 128:(st + 1) * 128, :]), reads=["xmid_scr"], writes=[fk + ("xm",)], dma=True)
        for k in range(2):
            op("pool", lambda e, f=f, k=k, st=st: e.indirect_dma_start(
                out=f["y%d" % k], out_offset=None, in_=ybuf_d[:, :],
                in_offset=bass.IndirectOffsetOnAxis(ap=slot_c[:, st * 2 + k:st * 2 + k + 1], axis=0)),
               reads=["slot_c"], writes=[fk + ("y%d" % k,)], dma=True)
        if st >= NF - 1:
            s2 = st - (NF - 1)
            _final_compute(s2)
    for s2 in range(max(NST - (NF - 1), 0), NST):
        _final_compute(s2)

    sc.emit()
    return nc


def host_consts(S):
    NST = S // 128
    cst = np.zeros((128, CS_N), np.float32)
    cst[:, CS_ID:CS_ID + 128] = np.eye(128, dtype=np.float32)
    p = np.arange(128)
    cst[:, CS_U:CS_U + 128] = (p[:, None] < p[None, :]).astype(np.float32)
    cst[:, CS_CM:CS_CM + 128] = (p[:, None] <= p[None, :]).astype(np.float32)
    cst[:, CS_HM] = (p < 64).astype(np.float32)
    cst[:, CS_HM + 1] = (p >= 64).astype(np.float32)
    cst[:, CS_TH:CS_TH + NTH] = (np.arange(NTH) * BLK).astype(np.float32)[None, :]
    rm = np.ones((512,), np.float32)
    rm[::128] = 0.0
    cst[:, CS_RM:CS_RM + 512] = rm[None, :]
    cst[:, CS_IO:CS_IO + 32] = np.arange(32, dtype=np.float32)[None, :]
    cst[:, CS_B5] = (p * BLK).astype(np.float32)
    tokid = (np.arange(NST)[None, :] * 128 + p[:, None]).astype(np.int32)
    return cst, tokid


def fm(v, n):
    return np.ascontiguousarray(np.asarray(v, np.float32).reshape(n, 128).T)


def host_inputs(inp, b, S):
    L = 0
    tab = np.zeros((128, TB_N), np.float32)
    tab[:, TB_BADA:TB_BADA + 48] = fm(inp["b_ada"][L], 48)
    tab[:, TB_GMIX:TB_GMIX + 8] = fm(inp["g_mix"][L], 8)
    tab[:, TB_GFFN:TB_GFFN + 8] = fm(inp["g_ffn"][L], 8)
    cw = np.asarray(inp["conv_w"][L], np.float32)
    for c in range(4):
        tab[:, TB_CW + c * 4:TB_CW + c * 4 + 4] = cw[:, c * 128:(c + 1) * 128].T
    tab[:, TB_CB:TB_CB + 4] = fm(inp["conv_b"][L], 4)
    tab[:, TB_BR:TB_BR + 4] = fm(inp["lru_br"][L], 4)
    tab[:, TB_BI:TB_BI + 4] = fm(inp["lru_bi"][L], 4)
    tab[:, TB_LAM:TB_LAM + 4] = fm(inp["lru_lambda"][L], 4)
    tab[:, TB_BA:TB_BA + 2] = fm(inp["gla_ba"][L], 2)
    tab[:, TB_GN] = np.asarray(inp["gla_gnorm"][L], np.float32)
    ba = np.asarray(inp["b_ada"][L], np.float32)
    tab[:, TB_BADAP:TB_BADAP + 8] = ba[3 * D:4 * D].reshape(128, 8)
    tab[:, TB_BADAP + 8:TB_BADAP + 16] = ba[4 * D:5 * D].reshape(128, 8)
    tab[:, TB_GFFNP:TB_GFFNP + 8] = np.asarray(inp["g_ffn"][L], np.float32).reshape(128, 8)

    def bd(w):
        w = np.asarray(w, np.float32)
        o = np.zeros((128, 4, 128), np.float32)
        for c in range(4):
            for hh in range(2):
                o[hh * 64:(hh + 1) * 64, c, hh * 64:(hh + 1) * 64] = w[2 * c + hh]
        return o.reshape(128, 512)

    cst, tokid = host_consts(S)
    m = {
        "x": np.ascontiguousarray(np.asarray(inp["x"][b], np.float32)),
        "cT": fm(inp["c"][b], 8),
        "w_ada": np.ascontiguousarray(np.asarray(inp["w_ada"][L], np.float32)),
        "tab": tab,
        "cst": cst,
        "tokid": tokid,
        "gf_bc": np.ascontiguousarray(np.broadcast_to(np.asarray(inp["g_final"], np.float32)[None, :], (128, D))),
        "br_bc": np.ascontiguousarray(np.broadcast_to(
            np.concatenate([np.asarray(inp["b_coarse"][L], np.float32),
                            np.asarray(inp["b_fine"][L], np.float32)])[None, :], (128, 36))),
        "w_in": np.ascontiguousarray(np.asarray(inp["w_in"][L], np.float32)),
        "w_out": np.ascontiguousarray(np.asarray(inp["w_out"][L], np.float32)),
        "bd_r": bd(inp["lru_wr"][L]),
        "bd_i": bd(inp["lru_wi"][L]),
        "wa2": np.ascontiguousarray(np.asarray(inp["gla_wa2"][L], np.float32)),
        "w_r": np.ascontiguousarray(np.concatenate([np.asarray(inp["w_coarse"][L], np.float32),
                                                    np.asarray(inp["w_fine"][L], np.float32)], axis=1)),
        "w_gate": np.ascontiguousarray(np.asarray(inp["w_gate"][L], np.float32)),
        "w_up": np.ascontiguousarray(np.asarray(inp["w_up"][L], np.float32)),
        "w_down": np.ascontiguousarray(np.asarray(inp["w_down"][L], np.float32)),
    }
    return m


def kernel(**inputs):
    B, S = inputs["x"].shape[0], inputs["x"].shape[1]
    nc = build(S)
    in_maps = [host_inputs(inputs, b, S) for b in range(B)]
    res = run_bass_kernel_spmd(nc, in_maps, core_ids=list(range(B)))
    return np.stack([np.asarray(r["out"]) for r in res.results], axis=0).astype(np.float32)
```

```python
import numpy as np
import concourse.bass as bass
import concourse.mybir as mybir
from concourse.bass_utils import run_bass_kernel_spmd

F32 = mybir.dt.float32
BF16 = mybir.dt.bfloat16
I32 = mybir.dt.int32
U32 = mybir.dt.uint32
AF = mybir.ActivationFunctionType
ALU = mybir.AluOpType
AX = mybir.AxisListType

D = 1024
NPROJ = 2576
NE = 32
CAP = 8192 + 512
EPS = 1e-6
T = 256
BLK = 512


class Sched:
    EPOCH = 6000

    def __init__(self, nc, same_engine_sync=True):
        self.nc = nc
        self.ops = []
        self.lw = {}
        self.rd = {}
        self.same = same_engine_sync
        self.stack = []

    def rec_begin(self):
        self.stack.append([])

    def rec_end(self):
        return self.stack.pop()

    def play(self, lst):
        for a in lst:
            self.op(*a)

    @staticmethod
    def merge(lists):
        lists = [l for l in lists if l]
        out = []
        n = [len(l) for l in lists]
        pos = [0] * len(lists)
        total = sum(n)
        for _ in range(total):
            bi, bv = -1, 2.0
            for i in range(len(lists)):
                if pos[i] < n[i]:
                    v = pos[i] / n[i]
                    if v < bv:
                        bi, bv = i, v
            out.append(lists[bi][pos[bi]])
            pos[bi] += 1
        return out

    def op(self, eng, fns, reads=(), writes=(), dma=False):
        if self.stack:
            self.stack[-1].append((eng, fns, list(reads), list(writes), dma))
            return
        if callable(fns):
            fns = [fns]
        reads, writes = list(reads), list(writes)
        for k in reads:
            if isinstance(k, str) and k.startswith("ps") and k[2:].isdigit() and k not in writes:
                writes.append(k)
        idx = len(self.ops)
        deps = set()
        for k in reads:
            if k in self.lw:
                deps.add(self.lw[k])
        for k in writes:
            if k in self.lw:
                deps.add(self.lw[k])
            r = self.rd.get(k)
            if r:
                deps.update(r[0].values())
                deps.update(r[1])
        self.ops.append(dict(eng=eng, fns=fns, deps=deps, dma=dma, tag=(list(reads), list(writes))))
        for k in writes:
            self.lw[k] = idx
            self.rd[k] = ({}, [])
        for k in reads:
            r = self.rd.setdefault(k, ({}, []))
            if dma:
                r[1].append(idx)
            else:
                r[0][eng] = idx
        return idx

    def barrier(self):
        last = {}
        dmas = []
        for i, o in enumerate(self.ops):
            if o["dma"]:
                dmas.append(i)
            else:
                last[o["eng"]] = i
        deps = set(last.values()) | set(dmas)
        for eng in ("pe", "act", "dve", "pool", "sp"):
            idx = len(self.ops)
            self.ops.append(dict(eng=eng, fns=[lambda e: e.nop()], deps=set(deps), dma=False))
            last[eng] = idx
        self._pending_dma_done = True
        self.lw = {}
        self.rd = {}
        self._bar = dict(last)
        for eng, i in last.items():
            self.lw[("__bar__", eng)] = i

    def emit(self):
        import os
        lim = int(os.environ.get("OPLIMIT", "0"))
        print("total ops", len(self.ops))
        if lim:
            self.ops = self.ops[:lim]
            o = self.ops[-1]
            print("last op", o["eng"], o.get("tag"))
        nc = self.nc
        engs = ("pe", "act", "dve", "pool", "sp")
        count = {e: 0 for e in engs}
        prog = {e: [] for e in engs}
        dma_pool = {}
        dma_rr = {e: 0 for e in engs}
        dma_val = {}
        NPOOL = {"sp": 10, "pool": 8, "act": 4, "dve": 2, "pe": 2}
        tokens = []
        pre_wait = []
        for o in self.ops:
            e = o["eng"]
            if o["dma"]:
                pl = dma_pool.setdefault(e, [])
                if len(pl) < NPOOL[e]:
                    s = nc.alloc_semaphore(name=f"dq_{e}_{len(pl)}")
                    pl.append(s)
                    dma_val[id(s)] = 0
                s = pl[dma_rr[e] % len(pl)] if len(pl) == NPOOL[e] else pl[-1]
                dma_rr[e] += 1
                prev = dma_val[id(s)]
                dma_val[id(s)] = prev + 16
                tokens.append((s, prev + 16))
                pre_wait.append((s, prev) if prev > 0 else None)
            else:
                c = count[e]
                ep = c // self.EPOCH
                while len(prog[e]) <= ep:
                    prog[e].append(nc.alloc_semaphore(name=f"pg_{e}_{len(prog[e])}"))
                tokens.append((prog[e][ep], c % self.EPOCH + 1))
                pre_wait.append(None)
                count[e] = c + 1
        waited = {e: {} for e in engs}
        streams = {e: [] for e in engs}
        for i, o in enumerate(self.ops):
            e = o["eng"]
            ws = []
            cand = []
            if pre_wait[i] is not None:
                cand.append(pre_wait[i])
            for d in sorted(o["deps"]):
                od = self.ops[d]
                if (not od["dma"]) and od["eng"] == e and (e == "pe" or not self.same):
                    continue
                cand.append(tokens[d])
            for (s, v) in cand:
                w = waited[e]
                if w.get(id(s), 0) >= v:
                    continue
                w[id(s)] = v
                ws.append((s, v))
            streams[e].append((ws, o["fns"], tokens[i], o["dma"]))
        final_waits = []
        for e, pl in dma_pool.items():
            for s in pl:
                if dma_val[id(s)] > 0:
                    final_waits.append((s, dma_val[id(s)]))
        for e in engs:
            if count[e] > 0:
                c = count[e] - 1
                final_waits.append((prog[e][c // self.EPOCH], c % self.EPOCH + 1))

        def run(engine, name):
            for ws, fns, tok, is_dma in streams[name]:
                for (s, v) in ws:
                    engine.wait_ge(s, v)
                ins = None
                for f in fns:
                    ins = f(engine)
                ins.then_inc(tok[0], 16 if is_dma else 1)

        with nc.Block() as block:
            @block.tensor
            def _(eng):
                run(eng, "pe")

            @block.scalar
            def _(eng):
                run(eng, "act")

            @block.vector
            def _(eng):
                run(eng, "dve")

            @block.gpsimd
            def _(eng):
                run(eng, "pool")

            @block.sync
            def _(eng):
                run(eng, "sp")
                for (s, v) in final_waits:
                    eng.wait_ge(s, v)


def act(out, in_, func, bias=None, scale=None, accum_out=None):
    def f(e):
        kw = {}
        if bias is not None:
            kw["bias"] = bias
        if scale is not None:
            kw["scale"] = scale
        if accum_out is not None:
            kw["accum_out"] = accum_out
        return e.activation(out=out, in_=in_, func=func, **kw)
    return f


def ts(out, in0, s1, op0, s2=None, op1=None, accum_out=None):
    def f(e):
        kw = {}
        if op1 is not None:
            kw["op1"] = op1
        if accum_out is not None:
            kw["accum_out"] = accum_out
        return e.tensor_scalar(out=out, in0=in0, scalar1=s1, scalar2=s2, op0=op0, **kw)
    return f


def tt(out, in0, in1, op):
    return lambda e: e.tensor_tensor(out=out, in0=in0, in1=in1, op=op)


def stt(out, in0, scalar, in1, op0, op1):
    return lambda e: e.scalar_tensor_tensor(out=out, in0=in0, scalar=scalar, in1=in1, op0=op0, op1=op1)


def mm(out, lhsT, rhs, start=True, stop=True):
    return lambda e: e.matmul(out, lhsT, rhs, start=start, stop=stop)


def tr(out, in_, ident):
    return lambda e: e.transpose(out, in_, ident)


def dma(out, in_, **kw):
    return lambda e: e.dma_start(out=out, in_=in_, **kw)


def cp(out, in_):
    return lambda e: e.tensor_copy(out=out, in_=in_)


def acp(out, in_):
    return lambda e: e.activation(out=out, in_=in_, func=AF.Copy)


def scan(out, d0, d1, init, op0, op1):
    return lambda e: e.tensor_tensor_scan(out=out, data0=d0, data1=d1, initial=init, op0=op0, op1=op1)


def red(out, in_, op):
    return lambda e: e.tensor_reduce(out=out, in_=in_, axis=AX.X, op=op)


def mset(ap, v):
    return lambda e: e.memset(ap, v)


TB_BADA = 0
TB_GMIX = 48
TB_GFFN = 56
TB_CW = 64
TB_CB = 80
TB_BR = 84
TB_BI = 88
TB_LAM = 92
TB_BA = 96
TB_GN = 98
TB_BADAP = 99
TB_GFFNP = 115
TB_N = 123

CS_ID = 0
CS_U = 128
CS_CM = 256
CS_RM = 384
CS_IO = 896
CS_B5 = 928
CS_HM = 929
CS_TH = 931
NTH = 17
CS_N = 948


class Arena:
    def __init__(self, ap, nwords):
        self.ap = ap
        self.n = nwords
        self.off = 0

    def f32(self, n):
        n = (n + 1) // 2 * 2
        assert self.off + n <= self.n, ("arena overflow", self.off, n, self.n)
        v = self.ap[:, self.off:self.off + n]
        self.off += n
        return v

    def bf(self, nel):
        w = (nel + 1) // 2
        w = (w + 1) // 2 * 2
        v = self.f32(w).bitcast(BF16)
        return v[:, 0:nel]

    def mark(self):
        return self.off

    def reset(self, m):
        self.off = m


def build(S, stop_after=None, dbg=False):
    NT = S // T
    NST = S // 128
    NB = (2 * S) // BLK + NE
    NROWS = NE * CAP + BLK
    NULLSTART = NE * CAP
    nc = bass.Bass("TRN2", target_bir_lowering=False)

    def din(name, shape, dt=F32):
        return nc.dram_tensor(name, shape, dt, kind="ExternalInput").ap()

    x_d = din("x", [S, D])
    cT_d = din("cT", [128, 8])
    wada_d = din("w_ada", [D, 6 * D])
    tab_d = din("tab", [128, TB_N])
    cst_d = din("cst", [128, CS_N])
    tok_d = din("tokid", [128, NST], I32)
    gf_d = din("gf_bc", [128, D])
    brb_d = din("br_bc", [128, 36])
    win_d = din("w_in", [D, NPROJ])
    wout_d = din("w_out", [D, D])
    bdr_d = din("bd_r", [128, 512])
    bdi_d = din("bd_i", [128, 512])
    wa2_d = din("wa2", [16, 256])
    wr_d = din("w_r", [D, 36])
    wg_d = din("w_gate", [NE, D, 512])
    wu_d = din("w_up", [NE, D, 512])
    wd_d = din("w_down", [NE, 512, D])
    out_d = nc.dram_tensor("out", [S, D], F32, kind="ExternalOutput").ap()

    xmid_d = nc.dram_tensor("xmid_scr", [S, D], F32, kind="Internal").ap()
    xn2_d = nc.dram_tensor("xn2_scr", [S, D], BF16, kind="Internal").ap()
    sidx_d = nc.dram_tensor("sidx_scr", [NROWS, 2], I32, kind="Internal").ap()
    ybuf_d = nc.dram_tensor("ybuf_scr", [NB * BLK, D], F32, kind="Internal").ap()
    dbg_d = None
    if dbg:
        dbg_d = nc.dram_tensor("dbg", [S, D], F32, kind="ExternalOutput").ap()
        dbg2_d = nc.dram_tensor("dbg2", [128, 4096], F32, kind="ExternalOutput").ap()

    sc = Sched(nc)
    op = sc.op

    def sb(name, shape, dt=F32):
        return nc.alloc_sbuf_tensor(name + "_sb", shape, dt)[:]

    tab = sb("tab", [128, TB_N])
    cst = sb("cst", [128, CS_N])
    tokid = sb("tokid", [128, NST], I32)
    gf_bc = sb("gf_bc", [128, D])
    gt1_bc = sb("gt1_bc", [128, D])
    gt2_bc = sb("gt2_bc", [128, D])
    brb = sb("brb", [128, 36])
    modT = sb("modT", [128, 48])
    scale1 = sb("scale1", [128, 8])
    scale2 = sb("scale2", [128, 8])
    scale2p = sb("scale2p", [128, 8])
    modP = sb("modP", [128, 16])
    widx = sb("widx", [128, 128], I32)
    sidx4 = sb("sidx4", [128, 4 * 128], I32)
    ltab = sb("ltab", [128, 16])
    ident_bf = sb("ident_bf", [128, 128], BF16)
    ones_bf = sb("ones_bf", [128, 128], BF16)
    ones_f = sb("ones_f", [128, 128])
    ohs = sb("ohs", [128, NST * 2 * 32], BF16)
    pos_all = sb("pos_all", [128, NST * 2])
    eid_all = sb("eid_all", [128, NST * 2])
    wts_all = sb("wts_all", [128, NST * 2])
    Ocum = sb("Ocum", [128, 32])
    pay_i = sb("pay", [128, NST * 4], I32)
    pay_f = pay_i.bitcast(F32)
    slot_u = sb("slot_u", [128, NST * 2], I32)
    slot_c = sb("slot_c", [128, NST * 2], I32)
    tbl_i = sb("tbl_i", [1, 256], I32)
    idx_sb = [sb(f"idx_sb{q}", [128, 8], I32) for q in range(2)]
    ident_f = cst[:, CS_ID:CS_ID + 128]
    Umat = cst[:, CS_U:CS_U + 128]
    cmask = cst[:, CS_CM:CS_CM + 128]
    hmask = cst[:, CS_HM:CS_HM + 2]
    rmask = cst[:, CS_RM:CS_RM + 512]
    iota32 = cst[:, CS_IO:CS_IO + 32]
    blk512 = cst[:, CS_B5:CS_B5 + 1]

    rem = nc.sbuf_bytes_remaining
    rem = rem() if callable(rem) else rem
    ARW = (int(rem) // 4) - 64
    ARW = ARW // 2 * 2
    arena_ap = sb("arena", [128, ARW])
    ar = Arena(arena_ap, ARW)

    ps = [nc.alloc_psum_tensor(f"ps{i}", [128, 512], F32)[:] for i in range(8)]

    m0 = ar.mark()
    op("sp", dma(tab, tab_d), writes=["tab"], dma=True)
    op("sp", dma(cst, cst_d), writes=["cst"], dma=True)
    cT = ar.f32(8)
    op("sp", dma(cT, cT_d), writes=["cT"], dma=True)
    op("sp", dma(tokid, tok_d), writes=["tokid"], dma=True)
    op("sp", dma(gf_bc, gf_d), writes=["gf_bc"], dma=True)
    op("sp", dma(brb, brb_d), writes=["brb"], dma=True)
    sgc = ar.f32(8)
    scT = ar.f32(8)
    op("act", act(sgc, cT, AF.Sigmoid), reads=["cT"], writes=["sgc"])
    op("dve", tt(scT, cT, sgc, ALU.mult), reads=["cT", "sgc"], writes=["scT"])
    op("dve", mset(ones_f, 1.0), writes=["ones_f"])
    op("dve", mset(ones_bf, 1.0), writes=["ones_bf"])
    op("dve", cp(ident_bf, ident_f), reads=["cst"], writes=["ident_bf"])

    zt = ar.f32(2048)
    op("pool", mset(zt, 0.0), writes=["zt"])
    zt_i = zt.bitcast(I32)
    rows_per = 128 * 1024
    r0 = 0
    while r0 < NROWS:
        n = min(rows_per, NROWS - r0)
        np_ = n // 1024
        if np_ > 0:
            op("sp", dma(sidx_d[r0:r0 + np_ * 1024, :].rearrange("(p r) c -> p (r c)", p=np_),
                         zt_i[0:np_, :]), reads=["zt"], writes=["sidx_scr"], dma=True)
            r0 += np_ * 1024
        else:
            op("sp", dma(sidx_d[r0:r0 + n, :].rearrange("(p r) c -> p (r c)", p=1),
                         zt_i[0:1, 0:2 * n]), reads=["zt"], writes=["sidx_scr"], dma=True)
            r0 += n

    wst = [ar.f32(8 * 1024).rearrange("p (k f) -> p k f", k=8) for _ in range(2)]
    mrow = ar.f32(6 * 1024)
    grp = 0
    for blk in range(6):
        b = blk % 2
        op("sp", dma(wst[b], wada_d[:, blk * 1024:(blk + 1) * 1024].rearrange("(k p) f -> p k f", p=128)),
           writes=[("wst", b)], dma=True)
        for half in range(2):
            bk = 1 + grp % 4
            grp += 1
            fns = [mm(ps[bk][0:1, :], scT[:, kc:kc + 1], wst[b][:, kc, half * 512:(half + 1) * 512],
                      start=(kc == 0), stop=(kc == 7)) for kc in range(8)]
            op("pe", fns, reads=[("wst", b), "scT"], writes=[f"ps{bk}"])
            c0 = blk * 1024 + half * 512
            op("act", acp(mrow[0:1, c0:c0 + 512], ps[bk][0:1, :]), reads=[f"ps{bk}"], writes=["mrow"])
    fns = []
    for j in range(48):
        fns.append(mm(ps[0][:, j:j + 1], mrow[0:1, j * 128:(j + 1) * 128], ones_f[0:1, 0:1]))
    for bi, blk in enumerate((3, 4)):
        for kk in range(8):
            fns.append(mm(ps[0][:, 48 + bi * 8 + kk:48 + bi * 8 + kk + 1],
                          mrow[0:1, blk * 1024:(blk + 1) * 1024].rearrange("p (m kk) -> p kk m", kk=8)[:, kk, :],
                          ones_f[0:1, 0:1]))
    op("pe", fns, reads=["mrow", "ones_f"], writes=["ps0"])
    op("dve", tt(modT, ps[0][:, 0:48], tab[:, TB_BADA:TB_BADA + 48], ALU.add),
       reads=["ps0", "tab"], writes=["modT"])
    op("dve", tt(modP, ps[0][:, 48:64], tab[:, TB_BADAP:TB_BADAP + 16], ALU.add),
       reads=["ps0", "tab"], writes=["modP"])
    op("dve", stt(scale2p, modP[:, 8:16], 1.0, tab[:, TB_GFFNP:TB_GFFNP + 8], ALU.add, ALU.mult),
       reads=["modP", "tab"], writes=["scale2p"])
    op("dve", stt(scale1, modT[:, 8:16], 1.0, tab[:, TB_GMIX:TB_GMIX + 8], ALU.add, ALU.mult),
       reads=["modT", "tab"], writes=["scale1"])
    op("dve", stt(scale2, modT[:, 32:40], 1.0, tab[:, TB_GFFN:TB_GFFN + 8], ALU.add, ALU.mult),
       reads=["modT", "tab"], writes=["scale2"])
    bias1 = modT[:, 0:8]
    bias2 = modT[:, 24:32]
    bias2p = modP[:, 0:8]
    Gt = ar.f32(8 * 128).rearrange("p (k f) -> p k f", k=8)
    for (col0, dst, nm) in ((16, gt1_bc, "gt1_bc"), (40, gt2_bc, "gt2_bc")):
        for kc in range(8):
            op("dve", ts(Gt[:, kc, :], ones_f, modT[:, col0 + kc:col0 + kc + 1], ALU.mult),
               reads=["modT", "ones_f"], writes=[("Gt", kc)])
        for half in range(2):
            fns = [mm(ps[1 + half][:, k4 * 128:(k4 + 1) * 128], Gt[:, half * 4 + k4, :], ident_f)
                   for k4 in range(4)]
            op("pe", fns, reads=[("Gt", half * 4 + k4) for k4 in range(4)] + ["cst"],
               writes=[f"ps{1 + half}"])
            op("act", acp(dst[:, half * 512:(half + 1) * 512], ps[1 + half]),
               reads=[f"ps{1 + half}"], writes=[nm])
    t4 = ar.f32(4)
    op("act", act(t4, tab[:, TB_LAM:TB_LAM + 4], AF.Exp, scale=-1.0), reads=["tab"], writes=["t4"])
    op("act", act(t4, t4, AF.Ln, bias=1.0), reads=["t4"], writes=["t4"])
    op("dve", ts(ltab[:, 0:4], t4, -8.0, ALU.mult), reads=["t4"], writes=["ltab"])
    op("dve", ts(ltab[:, 4:8], t4, -16.0, ALU.mult), reads=["t4"], writes=["ltab"])
    op("dve", ts(ltab[:, 8:10], tab[:, TB_BA:TB_BA + 2], -1.0, ALU.mult), reads=["tab"], writes=["ltab"])
    cl = ltab[:, 0:4]
    c2l = ltab[:, 4:8]
    nba = ltab[:, 8:10]
    m_setup_tmp = ar.mark()

    ar.reset(m0)
    w_in_bf = ar.bf(8 * NPROJ).rearrange("p (k f) -> p k f", k=8)
    w_out_s = ar.bf(8 * D).rearrange("p (k f) -> p k f", k=8)
    bdr_bf = ar.bf(512).rearrange("p (c m) -> p c m", c=4)
    bdi_bf = ar.bf(512).rearrange("p (c m) -> p c m", c=4)
    wa2_bf = ar.bf(256)
    wr_bf = ar.bf(8 * 36).rearrange("p (k n) -> p k n", k=8)
    m_w = ar.mark()
    sc.barrier()
    stg = [ar.f32(NPROJ) for _ in range(2)]
    cast_engs = ["dve", "pool", "act"]
    ci = 0
    for kc in range(8):
        b = kc % 2
        op("sp", dma(stg[b], win_d[kc * 128:(kc + 1) * 128, :]), writes=[("stg", b)], dma=True)
        e = cast_engs[ci % 3]
        ci += 1
        op(e, (acp if e == "act" else cp)(w_in_bf[:, kc, :], stg[b]), reads=[("stg", b)], writes=["w_in_bf"])
    for kc in range(8):
        b = kc % 2
        op("sp", dma(stg[b][:, 0:D], wout_d[kc * 128:(kc + 1) * 128, :]), writes=[("stg", b)], dma=True)
        op("dve", tt(w_out_s[:, kc, :], stg[b][:, 0:D], gt1_bc, ALU.mult),
           reads=[("stg", b), "gt1_bc"], writes=["w_out_s"])
    for (src, dst, nm) in ((bdr_d, bdr_bf, "bdr"), (bdi_d, bdi_bf, "bdi")):
        op("sp", dma(stg[0][:, 0:512], src), writes=[("stg", 0)], dma=True)
        op("dve", cp(dst.rearrange("p c m -> p (c m)"), stg[0][:, 0:512]), reads=[("stg", 0)], writes=[nm])
    op("sp", dma(stg[1][0:16, 0:256], wa2_d), writes=[("stg", 1)], dma=True)
    op("dve", cp(wa2_bf[0:16, :], stg[1][0:16, 0:256]), reads=[("stg", 1)], writes=["wa2_bf"])
    op("sp", dma(stg[0][:, 0:288].rearrange("p (k n) -> p k n", k=8),
                 wr_d.rearrange("(k p) n -> p k n", p=128)), writes=[("stg", 0)], dma=True)
    op("dve", cp(wr_bf.rearrange("p k n -> p (k n)"), stg[0][:, 0:288]), reads=[("stg", 0)], writes=["wr_bf"])
    sc.barrier()
    ar.reset(m_w)

    if stop_after == "setup":
        op("sp", dma(dbg2_d[:, 0:48], modT), reads=["modT"], writes=["dbg2"], dma=True)
        op("sp", dma(dbg2_d[:, 1024:2048], gt1_bc), reads=["gt1_bc"], writes=["dbg2"], dma=True)
        op("sp", dma(dbg2_d[:, 64:80], ltab), reads=["ltab"], writes=["dbg2"], dma=True)
        sc.emit()
        return nc
    NJ = T // 128
    NCH = T // 64
    xs = [ar.f32(NJ * D).rearrange("p (j d) -> p j d", j=NJ) for _ in range(2)]
    xn = ar.bf(NJ * D).rearrange("p (j d) -> p j d", j=NJ)
    hT = ar.bf(8 * T).rearrange("p (k t) -> p k t", k=8)
    xc = ar.f32(4 * (T + 4)).rearrange("p (c t) -> p c t", c=4)
    yb = ar.f32(4 * T).rearrange("p (c t) -> p c t", c=4)
    qf = ar.f32(2 * T).rearrange("p (c t) -> p c t", c=2)
    kf = ar.f32(2 * T).rearrange("p (c t) -> p c t", c=2)
    gfb = ar.f32(4 * T).rearrange("p (c t) -> p c t", c=4)
    glT = ar.bf(T)
    vb = ar.bf(NJ * 512).rearrange("p (j e) -> p j e", j=NJ)
    catT = ar.bf(8 * T).rearrange("p (k t) -> p k t", k=8)
    L = []
    for _ in range(2):
        L.append(dict(cv=ar.f32(T), cvb=ar.bf(T), r=ar.f32(T), i=ar.f32(T), a=ar.f32(T),
                      s=ar.f32(T), h=ar.f32(T), t1=ar.f32(T), t2=ar.f32(T)))
    hprev = ar.f32(4)
    lf = ar.f32(2 * T).rearrange("p (c t) -> p c t", c=2)
    cs = ar.f32(2 * T).rearrange("p (c t) -> p c t", c=2)
    eb = ar.f32(2 * T).rearrange("p (c t) -> p c t", c=2)
    enb = ar.f32(2 * T).rearrange("p (c t) -> p c t", c=2)
    dec = ar.f32(2 * NCH).rearrange("p (c n) -> p c n", c=2)
    qd = ar.bf(2 * T).rearrange("p (c t) -> p c t", c=2)
    kd = ar.bf(2 * T).rearrange("p (c t) -> p c t", c=2)
    ke = ar.bf(2 * T).rearrange("p (c t) -> p c t", c=2)
    keT = ar.bf(NJ * 256).rearrange("p (j f) -> p j f", j=NJ)
    scT_sb = ar.bf(512)
    Sf = ar.f32(256).rearrange("p (c e) -> p c e", c=2)
    Sb = [ar.bf(256).rearrange("p (c e) -> p c e", c=2) for _ in range(2)]
    osq = ar.bf(T)
    sd = ar.f32(T)
    rs = ar.f32(T)
    sg = ar.f32(T)
    gs = ar.f32(T)
    to = ar.f32(T)
    stat = ar.f32(16)
    lg = ar.f32(NJ * 36).rearrange("p (j n) -> p j n", j=NJ)
    rt = ar.f32(NJ * 64).rearrange("p (j n) -> p j n", j=NJ)
    mf = ar.f32(NJ * 32).rearrange("p (j n) -> p j n", j=NJ)
    m8 = ar.f32(NJ * 8).rearrange("p (j n) -> p j n", j=NJ)
    oh = ar.f32(NJ * 64).rearrange("p (j n) -> p j n", j=NJ)
    Ot = ar.f32(NJ * 32).rearrange("p (j n) -> p j n", j=NJ)
    tmp32 = ar.f32(NJ * 64).rearrange("p (j n) -> p j n", j=NJ)
    print("arena used (words):", ar.off, "of", ARW)

    for c in range(4):
        op("pool", mset(xc[:, c, 0:4], 0.0), writes=[("xc", c)])
    op("pool", mset(hprev, 0.0), writes=["hprev"])
    op("pool", mset(Sf.rearrange("p c e -> p (c e)"), 0.0), writes=["Sf"])
    op("pool", mset(Sb[0].rearrange("p c e -> p (c e)"), 0.0), writes=[("Sb", 0)])
    op("pool", mset(Sb[1].rearrange("p c e -> p (c e)"), 0.0), writes=[("Sb", 1)])
    op("pool", mset(Ocum, 0.0), writes=["Ocum"])

    mmslot = [0]

    mmgroup = ["all"]
    MMB = {"all": [2, 3, 4], "front": [2, 3], "back": [4], "lru0": [2], "lru1": [3], "gla": [5]}

    def next_mm():
        banks = MMB[mmgroup[0]]
        bk = banks[mmslot[0] % len(banks)]
        mmslot[0] += 1
        return ps[bk], f"ps{bk}"

    psTb = [ps[0].bitcast(BF16), ps[1].bitcast(BF16)]
    psS = ps[5]
    psKV = ps[5]
    psO = [ps[6], ps[7]]
    qz = [[ar.bf(T) for _ in range(2)] for _ in range(2)]
    osq2 = ar.bf(2 * T)
    sd2 = ar.f32(2 * T)
    rs2 = ar.f32(2 * T)
    sg2 = ar.f32(2 * T)
    gs2 = ar.f32(2 * T)
    to2 = ar.f32(2 * T)
    print("arena used (words):", ar.off, "of", ARW)

    xnB = ar.bf(NJ * D).rearrange("p (j d) -> p j d", j=NJ)
    hTB = ar.bf(8 * T).rearrange("p (k t) -> p k t", k=8)
    catT2 = [catT, ar.bf(8 * T).rearrange("p (k t) -> p k t", k=8)]
    statB = ar.f32(16)
    print("arena used (words):", ar.off, "of", ARW)

    def norm_stats(xsrc_key, xbuf, xn_, st_, tag):
        sk = "stat" + tag
        for j in range(NJ):
            op("act", act(xn_[:, j, :], xbuf[:, j, :], AF.Square, accum_out=st_[:, j:j + 1]),
               reads=[xsrc_key], writes=[("xn" + tag, j), sk])
        op("act", act(st_[:, 2:4], st_[:, 0:2], AF.Sqrt, bias=EPS, scale=1.0 / D),
           reads=[sk], writes=[sk])
        op("dve", lambda e: e.reciprocal(out=st_[:, 4:6], in_=st_[:, 2:4]),
           reads=[sk], writes=[sk])
        for j in range(NJ):
            op("dve", ts(xn_[:, j, :], xbuf[:, j, :], st_[:, 4 + j:5 + j], ALU.mult),
               reads=[xsrc_key, sk], writes=[("xn" + tag, j)])

    def transposes_to_hT(scale_t, bias_t, xn_, hT_, tag, tb):
        for hb_ in range(2):
            hb = tb
            fns = []
            for k4 in range(4):
                kc = hb_ * 4 + k4
                for j in range(NJ):
                    fns.append(tr(psTb[hb][:, k4 * T + j * 128: k4 * T + (j + 1) * 128],
                                  xn_[:, j, kc * 128:(kc + 1) * 128], ident_bf))
            op("pe", fns, reads=[("xn" + tag, j) for j in range(NJ)] + ["ident_bf"], writes=[f"ps{hb}"])
            for k4 in range(4):
                kc = hb_ * 4 + k4
                src = psTb[hb][:, k4 * T:(k4 + 1) * T]
                op("act", act(hT_[:, kc, :], src, AF.Identity, bias=bias_t[:, kc:kc + 1],
                              scale=scale_t[:, kc:kc + 1]),
                   reads=[f"ps{hb}", "scale1", "scale2", "modT"], writes=[("hT" + tag, kc)])

    hT_keys = [("hTF", kc) for kc in range(8)]
    hTB_keys = [("hTB", kc) for kc in range(8)]
    ev_i = [0]

    def evac(dst, src, skey, dkey):
        e = "act" if (ev_i[0] // 2) % 2 == 0 else "dve"
        ev_i[0] += 1
        op(e, (acp if e == "act" else cp)(dst, src), reads=[skey], writes=[dkey])

    def load_x(t_):
        op("sp", dma(xs[t_ % 2], x_d[t_ * T:(t_ + 1) * T, :].rearrange("(j p) d -> p j d", p=128)),
           writes=[("xs", t_ % 2)], dma=True)

    def front(it):
        xb = it % 2
        X = xs[xb]
        xk = ("xs", xb)
        catT = catT2[it % 2]
        ctag = "catT%d" % (it % 2)
        norm_stats(xk, X, xn, stat, "F")
        mmgroup[0] = "front"
        transposes_to_hT(scale1, bias1, xn, hT, "F", 0)

        fm_list = []
        for c in range(4):
            fm_list.append((c * 128, xc[:, c, 4:4 + T], ("xc", c)))
        for c in range(4):
            fm_list.append((512 + c * 128, yb[:, c, :], ("yb", c)))
        for i in range(2):
            fm_list.append((1024 + i * 128, qf[:, i, :], ("qf", i)))
        for i in range(2):
            fm_list.append((1280 + i * 128, kf[:, i, :], ("kf", i)))
        for h in range(4):
            fm_list.append((2048 + h * 128, gfb[:, h, :], ("gf", h)))
        for pi_ in range(0, 16, 2):
            pso, key = next_mm()
            fns = []
            for u in range(2):
                col0 = fm_list[pi_ + u][0]
                fns += [mm(pso[:, u * T:(u + 1) * T], w_in_bf[:, kc, col0:col0 + 128], hT[:, kc, :],
                           start=(kc == 0), stop=(kc == 7)) for kc in range(8)]
            op("pe", fns, reads=hT_keys + ["w_in_bf"], writes=[key])
            for u in range(2):
                _, dst, dkey = fm_list[pi_ + u]
                evac(dst, pso[:, u * T:(u + 1) * T], key, dkey)
        pso, key = next_mm()
        fns = [mm(pso[0:16, 0:T], w_in_bf[:, kc, 2560:2576], hT[:, kc, :], start=(kc == 0), stop=(kc == 7))
               for kc in range(8)]
        op("pe", fns, reads=hT_keys + ["w_in_bf"], writes=[key])
        evac(glT[0:16, :], pso[0:16, 0:T], key, "glT")
        ev_i[0] += 1
        for j in range(NJ):
            pso, key = next_mm()
            fns = [mm(pso, hT[:, kc, j * 128:(j + 1) * 128], w_in_bf[:, kc, 1536:2048],
                      start=(kc == 0), stop=(kc == 7)) for kc in range(8)]
            op("pe", fns, reads=hT_keys + ["w_in_bf"], writes=[key])
            evac(vb[:, j, :], pso, key, ("vb", j))
            ev_i[0] += 1

        lru_lists = {0: [], 1: []}
        for c in range(4):
            mmgroup[0] = "lru%d" % (c % 2)
            sc.rec_begin()
            B = L[c % 2]
            lk = ("L", c % 2)
            cw = tab[:, TB_CW + c * 4:TB_CW + c * 4 + 4]
            op("dve", ts(B["cv"], xc[:, c, 4:4 + T], cw[:, 3:4], ALU.mult, tab[:, TB_CB + c:TB_CB + c + 1], ALU.add),
               reads=[("xc", c), "tab"], writes=[lk + ("cv",)])
            for k in range(3):
                op("dve", stt(B["cv"], xc[:, c, 1 + k:1 + k + T], cw[:, k:k + 1], B["cv"], ALU.mult, ALU.add),
                   reads=[("xc", c), "tab", lk + ("cv",)], writes=[lk + ("cv",)])
            op("pool", cp(B["cvb"], B["cv"]), reads=[lk + ("cv",)], writes=[lk + ("cvb",)])
            op("pool", cp(xc[:, c, 0:4], xc[:, c, T:T + 4]), reads=[("xc", c)], writes=[("xc", c)])
            pg, kg = next_mm()
            op("pe", [mm(pg[:, 0:T], bdr_bf[:, c, :], B["cvb"]), mm(pg[:, T:2 * T], bdi_bf[:, c, :], B["cvb"])],
               reads=[lk + ("cvb",), "bdr", "bdi"], writes=[kg])
            op("act", act(B["r"], pg[:, 0:T], AF.Sigmoid, bias=tab[:, TB_BR + c:TB_BR + c + 1]),
               reads=[kg, "tab"], writes=[lk + ("r",)])
            op("act", act(B["i"], pg[:, T:2 * T], AF.Sigmoid, bias=tab[:, TB_BI + c:TB_BI + c + 1]),
               reads=[kg, "tab"], writes=[lk + ("i",)])
            op("act", act(B["a"], B["r"], AF.Exp, scale=cl[:, c:c + 1]),
               reads=[lk + ("r",), "ltab"], writes=[lk + ("a",)])
            op("act", act(B["s"], B["r"], AF.Exp, scale=c2l[:, c:c + 1]),
               reads=[lk + ("r",), "ltab"], writes=[lk + ("s",)])
            op("act", act(B["s"], B["s"], AF.Sqrt, bias=1.0, scale=-1.0),
               reads=[lk + ("s",)], writes=[lk + ("s",)])
            op("pool", tt(B["i"], B["i"], B["cv"], ALU.mult), reads=[lk + ("i",), lk + ("cv",)], writes=[lk + ("i",)])
            op("pool", tt(B["i"], B["i"], B["s"], ALU.mult), reads=[lk + ("i",), lk + ("s",)], writes=[lk + ("i",)])
            op("dve", scan(B["h"], B["a"], B["i"], hprev[:, c:c + 1], ALU.mult, ALU.add),
               reads=[lk + ("a",), lk + ("i",), "hprev"], writes=[lk + ("h",)])
            op("pool", cp(hprev[:, c:c + 1], B["h"][:, T - 1:T]), reads=[lk + ("h",)], writes=["hprev"])
            Y = yb[:, c, :]
            op("pool", tt(B["t1"], Y, Y, ALU.mult), reads=[("yb", c)], writes=[lk + ("t1",)])
            op("pool", ts(B["t1"], B["t1"], 0.044715, ALU.mult, 1.0, ALU.add),
               reads=[lk + ("t1",)], writes=[lk + ("t1",)])
            op("pool", tt(B["t1"], B["t1"], Y, ALU.mult), reads=[lk + ("t1",), ("yb", c)], writes=[lk + ("t1",)])
            op("act", act(B["t2"], B["t1"], AF.Sigmoid, scale=1.5957691216057308),
               reads=[lk + ("t1",)], writes=[lk + ("t2",)])
            op("pool", tt(B["t2"], B["t2"], Y, ALU.mult), reads=[lk + ("t2",), ("yb", c)], writes=[lk + ("t2",)])
            op("dve", tt(catT[:, c, :], B["h"], B["t2"], ALU.mult),
               reads=[lk + ("h",), lk + ("t2",)], writes=[(ctag, c)])
            lru_lists[c % 2] += sc.rec_end()
        mmgroup[0] = "gla"
        sc.rec_begin()

        pz, kz = next_mm()
        op("pe", [mm(pz[:, i * T:(i + 1) * T], wa2_bf[0:16, i * 128:(i + 1) * 128], glT[0:16, :]) for i in range(2)],
           reads=["glT", "wa2_bf"], writes=[kz])
        for i in range(2):
            op("act", act(lf[:, i, :], pz[:, i * T:(i + 1) * T], AF.Exp, bias=nba[:, i:i + 1], scale=-1.0),
               reads=[kz, "ltab"], writes=[("lf", i)])
            op("act", act(lf[:, i, :], lf[:, i, :], AF.Ln, bias=1.0), reads=[("lf", i)], writes=[("lf", i)])
            op("dve", scan(cs[:, i, :], rmask[:, 0:T], lf[:, i, :], 0.0, ALU.mult, ALU.add),
               reads=[("lf", i), "cst"], writes=[("cs", i)])
            op("act", act(eb[:, i, :], cs[:, i, :], AF.Exp, scale=-1.0 / 16), reads=[("cs", i)], writes=[("eb", i)])
            op("act", act(enb[:, i, :], cs[:, i, :], AF.Exp, scale=1.0 / 16), reads=[("cs", i)], writes=[("enb", i)])
            op("act", act(dec[:, i, 0:NJ], cs[:, i, :].rearrange("p (n t) -> p n t", t=128)[:, :, 127],
                          AF.Exp, scale=-1.0 / 16), reads=[("cs", i)], writes=[("dec", i)])
            op("dve", stt(qd[:, i, :], qf[:, i, :], 0.125, eb[:, i, :], ALU.mult, ALU.mult),
               reads=[("qf", i), ("eb", i)], writes=[("qd", i)])
            for hh in range(2):
                op("pool", ts(qz[i][hh], qd[:, i, :], hmask[:, hh:hh + 1], ALU.mult),
                   reads=[("qd", i), "cst"], writes=[("qz", i, hh)])
            op("dve", tt(kd[:, i, :], kf[:, i, :], enb[:, i, :], ALU.mult),
               reads=[("kf", i), ("enb", i)], writes=[("kd", i)])
            op("pool", tt(ke[:, i, :].rearrange("p (n t) -> p n t", t=128),
                          kd[:, i, :].rearrange("p (n t) -> p n t", t=128),
                          dec[:, i, 0:NJ].unsqueeze(2).to_broadcast([128, NJ, 128]), ALU.mult),
               reads=[("kd", i), ("dec", i)], writes=[("ke", i)])
        fns = []
        for j in range(NJ):
            for i in range(2):
                fns.append(tr(psTb[0][:, (j * 2 + i) * 128:(j * 2 + i + 1) * 128], ke[:, i, j * 128:(j + 1) * 128], ident_bf))
        op("pe", fns, reads=[("ke", 0), ("ke", 1), "ident_bf"], writes=["ps0"])
        op("act", acp(keT.rearrange("p j f -> p (j f)"), psTb[0][:, 0:NJ * 256]), reads=["ps0"], writes=["keT"])
        for j in range(NJ):
            par = (it * NJ + j) % 2
            tsl = slice(j * 128, (j + 1) * 128)
            fns = []
            for h in range(4):
                i, hh = h // 2, h % 2
                fns.append(mm(psS[:, h * 128:(h + 1) * 128], kd[:, i, tsl], qz[i][hh][:, tsl]))
            op("pe", fns, reads=[("kd", 0), ("kd", 1)] + [("qz", i, hh) for i in range(2) for hh in range(2)],
               writes=["ps5"])
            op("dve", tt(scT_sb.rearrange("p (a c) -> p a c", c=128),
                         psS.rearrange("p (a c) -> p a c", c=128),
                         cmask.unsqueeze(1).to_broadcast([128, 4, 128]), ALU.mult),
               reads=["ps5", "cst"], writes=["scT"])
            for i in range(2):
                fns = []
                for hh in range(2):
                    h = 2 * i + hh
                    o_ap = psO[i][:, hh * T + j * 128: hh * T + (j + 1) * 128]
                    fns.append(mm(o_ap, vb[:, j, h * 128:(h + 1) * 128], scT_sb[:, h * 128:(h + 1) * 128],
                                  start=True, stop=False))
                    fns.append(mm(o_ap, Sb[par][:, i, :], qz[i][hh][:, tsl], start=False, stop=True))
                op("pe", fns, reads=[("vb", j), "scT", ("Sb", par), ("qz", i, 0), ("qz", i, 1)], writes=[f"ps{6 + i}"])
            fns = []
            for h in range(4):
                i = h // 2
                fns.append(mm(psKV[:, h * 128:(h + 1) * 128], keT[:, j, i * 128:(i + 1) * 128],
                              vb[:, j, h * 128:(h + 1) * 128]))
            op("pe", fns, reads=["keT", ("vb", j)], writes=["ps5"])
            for h in range(4):
                i, hh = h // 2, h % 2
                r0, r1 = hh * 64, (hh + 1) * 64
                op("dve", stt(Sf[r0:r1, i, :], Sf[r0:r1, i, :], dec[r0:r1, i, j:j + 1],
                              psKV[r0:r1, h * 128:(h + 1) * 128], ALU.mult, ALU.add),
                   reads=["Sf", ("dec", i), "ps5"], writes=["Sf"])
            op("act", acp(Sb[1 - par].rearrange("p c e -> p (c e)"), Sf.rearrange("p c e -> p (c e)")),
               reads=["Sf"], writes=[("Sb", 1 - par)])
        for i in range(2):
            O = psO[i]
            ok = f"ps{6 + i}"
            op("act", act(osq2, O, AF.Square), reads=[ok], writes=["osq"])
            pss, kss = next_mm()
            op("pe", mm(pss, ones_bf, osq2), reads=["osq", "ones_bf"], writes=[kss])
            op("act", act(sd2, pss, AF.Sqrt, bias=EPS, scale=1.0 / 128), reads=[kss], writes=["sd"])
            op("dve", lambda e: e.reciprocal(out=rs2, in_=sd2), reads=["sd"], writes=["rs"])
            G = gfb[:, 2 * i:2 * i + 2, :].rearrange("p c t -> p (c t)")
            gk = [("gf", 2 * i), ("gf", 2 * i + 1)]
            op("act", act(sg2, G, AF.Sigmoid), reads=gk, writes=["sg"])
            op("dve", stt(gs2, G, tab[:, TB_GN:TB_GN + 1], sg2, ALU.mult, ALU.mult),
               reads=gk + ["sg", "tab"], writes=["gs"])
            op("dve", tt(to2, O, rs2, ALU.mult), reads=[ok, "rs"], writes=["to"])
            op("dve", tt(catT[:, 4 + 2 * i:6 + 2 * i, :].rearrange("p c t -> p (c t)"), to2, gs2, ALU.mult),
               reads=["to", "gs"], writes=[(ctag, 4 + 2 * i), (ctag, 5 + 2 * i)])

        gla_list = sc.rec_end()
        sc.play(Sched.merge([lru_lists[0], lru_lists[1], gla_list]))

    def back(it):
        xb = it % 2
        X = xs[xb]
        xk = ("xs", xb)
        catT = catT2[it % 2]
        ctag = "catT%d" % (it % 2)
        mmgroup[0] = "back"
        cat_keys = [(ctag, k) for k in range(8)]
        for j in range(NJ):
            for half in range(2):
                pso, key = next_mm()
                fns = [mm(pso, catT[:, kc, j * 128:(j + 1) * 128], w_out_s[:, kc, half * 512:(half + 1) * 512],
                          start=(kc == 0), stop=(kc == 7)) for kc in range(8)]
                op("pe", fns, reads=cat_keys + ["w_out_s"], writes=[key])
                op("dve", tt(X[:, j, half * 512:(half + 1) * 512], X[:, j, half * 512:(half + 1) * 512], pso, ALU.add),
                   reads=[key, xk], writes=[xk])
        op("sp", dma(xmid_d[it * T:(it + 1) * T, :].rearrange("(j p) d -> p j d", p=128), X),
           reads=[xk], writes=["xmid_scr"], dma=True)
        if dbg and stop_after == "mixer":
            op("sp", dma(dbg_d[it * T:(it + 1) * T, :].rearrange("(j p) d -> p j d", p=128), X),
               reads=[xk], writes=["dbg"], dma=True)
            if it + 2 < NT:
                load_x(it + 2)
            return

        norm_stats(xk, X, xnB, statB, "B")
        op("sp", dma(xn2_d[it * T:(it + 1) * T, :].rearrange("(j p) d -> p j d", p=128), xnB),
           reads=[("xnB", j) for j in range(NJ)], writes=["xn2_scr"], dma=True)
        if it + 2 < NT:
            load_x(it + 2)
        transposes_to_hT(scale2, bias2, xnB, hTB, "B", 1)
        pso, key = next_mm()
        fns = []
        for j in range(NJ):
            for kc in range(8):
                fns.append(mm(pso[:, j * 64:j * 64 + 36], hTB[:, kc, j * 128:(j + 1) * 128], wr_bf[:, kc, :],
                              start=(kc == 0), stop=(kc == 7)))
        op("pe", fns, reads=hTB_keys + ["wr_bf"], writes=[key])
        for j in range(NJ):
            op("dve", tt(lg[:, j, :], pso[:, j * 64:j * 64 + 36], brb, ALU.add), reads=[key, "brb"], writes=[("rt", j)])
        for j in range(NJ):
            st = it * NJ + j
            rk = ("rt", j)
            op("dve", red(rt[:, j, 0:1], lg[:, j, 0:4], ALU.max), reads=[rk], writes=[rk])
            op("dve", ts(rt[:, j, 4:8], lg[:, j, 0:4], rt[:, j, 0:1], ALU.is_equal), reads=[rk], writes=[rk])
            op("dve", ts(rt[:, j, 8:12], lg[:, j, 0:4], rt[:, j, 0:1], ALU.subtract), reads=[rk], writes=[rk])
            op("act", act(rt[:, j, 8:12], rt[:, j, 8:12], AF.Exp, accum_out=rt[:, j, 1:2]), reads=[rk], writes=[rk])
            op("dve", lambda e, j=j: e.reciprocal(out=rt[:, j, 2:3], in_=rt[:, j, 1:2]), reads=[rk], writes=[rk])
            op("dve", ts(rt[:, j, 12:16], rt[:, j, 4:8], 1e30, ALU.mult, -1e30, ALU.add), reads=[rk], writes=[rk])
            op("dve", tt(mf[:, j, :].rearrange("p (g e) -> p g e", g=4),
                         lg[:, j, 4:36].rearrange("p (g e) -> p g e", g=4),
                         rt[:, j, 12:16].unsqueeze(2).to_broadcast([128, 4, 8]), ALU.add), reads=[rk], writes=[rk])
            op("dve", lambda e, j=j: e.max(out=m8[:, j, :], in_=mf[:, j, :]), reads=[rk], writes=[rk])
            op("dve", ts(oh[:, j, 0:32], mf[:, j, :], m8[:, j, 0:1], ALU.is_equal), reads=[rk], writes=[rk])
            op("dve", ts(oh[:, j, 32:64], mf[:, j, :], m8[:, j, 1:2], ALU.is_equal), reads=[rk], writes=[rk])
            op("dve", cp(ohs[:, st * 64:(st + 1) * 64], oh[:, j, :]), reads=[rk], writes=["ohs"])
            op("dve", tt(rt[:, j, 16:17], m8[:, j, 1:2], m8[:, j, 0:1], ALU.subtract), reads=[rk], writes=[rk])
            op("act", act(rt[:, j, 17:18], rt[:, j, 16:17], AF.Exp), reads=[rk], writes=[rk])
            op("dve", ts(rt[:, j, 18:19], rt[:, j, 17:18], 1.0, ALU.add), reads=[rk], writes=[rk])
            op("dve", lambda e, j=j: e.reciprocal(out=rt[:, j, 19:20], in_=rt[:, j, 18:19]), reads=[rk], writes=[rk])
            op("dve", tt(wts_all[:, st * 2:st * 2 + 1], rt[:, j, 19:20], rt[:, j, 2:3], ALU.mult),
               reads=[rk], writes=["wts_all"])
            op("dve", tt(wts_all[:, st * 2 + 1:st * 2 + 2], wts_all[:, st * 2:st * 2 + 1], rt[:, j, 17:18], ALU.mult),
               reads=[rk, "wts_all"], writes=["wts_all"])
            op("dve", tt(Ot[:, j, :], oh[:, j, 0:32], oh[:, j, 32:64], ALU.add), reads=[rk], writes=[("Ot", j)])
            pp, kp = next_mm()
            op("pe", [mm(pp[:, 0:32], Umat, Ot[:, j, :], start=True, stop=False),
                      mm(pp[:, 0:32], ones_f, Ocum, start=False, stop=True)],
               reads=[("Ot", j), "Ocum", "cst", "ones_f"], writes=[kp])
            op("dve", tt(Ocum, Ocum, Ot[:, j, :], ALU.add), reads=[("Ot", j), "Ocum"], writes=["Ocum"])
            for k in range(2):
                o_k = oh[:, j, k * 32:(k + 1) * 32]
                op("dve", tt(tmp32[:, j, 0:32], o_k, pp[:, 0:32], ALU.mult), reads=[rk, kp], writes=[("tmp32", j)])
                op("dve", red(pos_all[:, st * 2 + k:st * 2 + k + 1], tmp32[:, j, 0:32], ALU.add),
                   reads=[("tmp32", j)], writes=["pos_all"])
                op("dve", tt(tmp32[:, j, 32:64], o_k, iota32, ALU.mult), reads=[rk, "cst"], writes=[("tmp32", j)])
                op("dve", red(eid_all[:, st * 2 + k:st * 2 + k + 1], tmp32[:, j, 32:64], ALU.add),
                   reads=[("tmp32", j)], writes=["eid_all"])
                op("dve", stt(rt[:, j, 20 + k:21 + k], eid_all[:, st * 2 + k:st * 2 + k + 1], float(CAP),
                              pos_all[:, st * 2 + k:st * 2 + k + 1], ALU.mult, ALU.add),
                   reads=["eid_all", "pos_all", rk], writes=[rk])
                op("dve", cp(slot_u[:, st * 2 + k:st * 2 + k + 1], rt[:, j, 20 + k:21 + k]), reads=[rk], writes=["slot_u"])
                op("pool", cp(pay_i[:, (st * 2 + k) * 2:(st * 2 + k) * 2 + 1], tokid[:, st:st + 1]),
                   reads=["tokid"], writes=[("pay", st, k)])
                op("pool", cp(pay_f[:, (st * 2 + k) * 2 + 1:(st * 2 + k) * 2 + 2], wts_all[:, st * 2 + k:st * 2 + k + 1]),
                   reads=["wts_all", ("pay", st, k)], writes=[("pay", st, k)])
                sl = slot_u[:, st * 2 + k:st * 2 + k + 1]
                py = pay_i[:, (st * 2 + k) * 2:(st * 2 + k) * 2 + 2]
                op("pool", lambda e, sl=sl, py=py: e.indirect_dma_start(
                    out=sidx_d[:, :], out_offset=bass.IndirectOffsetOnAxis(ap=sl, axis=0),
                    in_=py, in_offset=None), reads=["slot_u", ("pay", st, k)], writes=["sidx_scr"], dma=True)

    load_x(0)
    if NT > 1:
        load_x(1)
    front(0)
    for it in range(NT):
        lists = []
        if it + 1 < NT:
            sc.rec_begin()
            front(it + 1)
            lists.append(sc.rec_end())
        sc.rec_begin()
        back(it)
        lists.append(sc.rec_end())
        sc.play(Sched.merge(lists))
    mmgroup[0] = "all"

    if stop_after in ("mixer", "route"):
        if dbg and stop_after == "route":
            op("sp", dma(dbg2_d[:, 0:NST * 2], pos_all), reads=["pos_all"], writes=["dbg2"], dma=True)
            op("sp", dma(dbg2_d[:, 1024:1024 + NST * 2], eid_all), reads=["eid_all"], writes=["dbg2"], dma=True)
            op("sp", dma(dbg2_d[:, 2048:2048 + NST * 2], wts_all), reads=["wts_all"], writes=["dbg2"], dma=True)
        sc.emit()
        return nc

    sc.barrier()
    ar.reset(m0)
    thr = cst[:, CS_TH:CS_TH + NTH]
    cnt = ar.f32(32)
    big_full = ar.f32(max(NST * 2 * 32, 32 * NTH))
    big = big_full[:, 0:NST * 2 * 32]
    nblk = ar.f32(32)
    padded = ar.f32(32)
    pends = ar.f32(32)
    ebase = ar.f32(32)
    bt = ar.f32(64)
    slc_f = ar.f32(NST * 2)
    op("pe", mm(ps[2][:, 0:32], ones_f, Ocum), reads=["Ocum", "ones_f"], writes=["ps2"])
    op("dve", cp(cnt, ps[2][:, 0:32]), reads=["ps2"], writes=["cnt"])
    op("dve", tt(big_full[:, 0:32 * NTH].rearrange("p (e m) -> p e m", m=NTH),
                 cnt.unsqueeze(2).to_broadcast([128, 32, NTH]),
                 thr.unsqueeze(1).to_broadcast([128, 32, NTH]), ALU.is_gt),
       reads=["cnt", "cst"], writes=["big"])
    op("dve", red(nblk, big_full[:, 0:32 * NTH].rearrange("p (e m) -> p e m", m=NTH), ALU.add),
       reads=["big"], writes=["nblk"])
    op("dve", ts(padded, nblk, float(BLK), ALU.mult), reads=["nblk"], writes=["padded"])
    op("dve", scan(pends, ones_f[:, 0:32], padded, 0.0, ALU.mult, ALU.add), reads=["padded", "ones_f"], writes=["pends"])
    op("dve", tt(ebase, pends, padded, ALU.subtract), reads=["pends", "padded"], writes=["ebase"])
    op("dve", ts(bt[:, 0:32], pends, blk512, ALU.is_le), reads=["pends", "cst"], writes=["bt"])
    op("dve", red(bt[:, 32:33], bt[:, 0:32], ALU.add), reads=["bt"], writes=["bt"])
    op("dve", ts(bt[:, 32:33], bt[:, 32:33], float(NE - 1), ALU.min), reads=["bt"], writes=["bt"])
    op("dve", ts(bt[:, 0:32], iota32, bt[:, 32:33], ALU.is_equal), reads=["bt", "cst"], writes=["bt"])
    op("dve", tt(bt[:, 0:32], bt[:, 0:32], ebase, ALU.mult), reads=["bt", "ebase"], writes=["bt"])
    op("dve", red(bt[:, 33:34], bt[:, 0:32], ALU.add), reads=["bt"], writes=["bt"])
    op("dve", ts(bt[:, 34:35], blk512, pends[:, 31:32], ALU.is_lt), reads=["pends", "cst"], writes=["bt"])
    op("dve", stt(bt[:, 35:36], bt[:, 32:33], float(CAP), blk512, ALU.mult, ALU.add), reads=["bt", "cst"], writes=["bt"])
    op("dve", tt(bt[:, 35:36], bt[:, 35:36], bt[:, 33:34], ALU.subtract), reads=["bt"], writes=["bt"])
    op("dve", ts(bt[:, 35:36], bt[:, 35:36], float(-NULLSTART), ALU.add), reads=["bt"], writes=["bt"])
    op("dve", tt(bt[:, 35:36], bt[:, 35:36], bt[:, 34:35], ALU.mult), reads=["bt"], writes=["bt"])
    op("dve", ts(bt[:, 35:36], bt[:, 35:36], float(NULLSTART), ALU.add), reads=["bt"], writes=["bt"])
    Gb = ar.f32(256).rearrange("p (a m) -> p a m", a=2)
    bcf = ar.f32(256).rearrange("p (a m) -> p a m", a=2)
    pcol = ar.f32(2)
    op("dve", ts(Gb[:, 0, :], ones_f, bt[:, 32:33], ALU.mult), reads=["bt", "ones_f"], writes=["Gb"])
    op("dve", ts(Gb[:, 1, :], ones_f, bt[:, 35:36], ALU.mult), reads=["bt", "ones_f"], writes=["Gb"])
    op("pe", [mm(ps[3][:, 0:128], Gb[:, 0, :], ident_f), mm(ps[3][:, 128:256], Gb[:, 1, :], ident_f)],
       reads=["Gb", "cst"], writes=["ps3"])
    op("dve", cp(bcf.rearrange("p a m -> p (a m)"), ps[3][:, 0:256]), reads=["ps3"], writes=["bcf"])
    op("dve", ts(pcol[:, 0:1], blk512, 1.0 / BLK, ALU.mult), reads=["cst"], writes=["pcol"])
    op("dve", ts(bcf[:, 0, :], bcf[:, 0, :], 128.0, ALU.mult, pcol[:, 0:1], ALU.add), reads=["bcf", "pcol"], writes=["bcf"])
    op("dve", cp(widx, bcf[:, 0, :]), reads=["bcf"], writes=["widx"])
    op("dve", ts(bcf[:, 1, :], bcf[:, 1, :], 0.25, ALU.mult, pcol[:, 0:1], ALU.add), reads=["bcf", "pcol"], writes=["bcf"])
    op("dve", cp(sidx4[:, 0:128], bcf[:, 1, :]), reads=["bcf"], writes=["sidx4"])
    op("dve", tt(big.rearrange("p (a e) -> p a e", e=32), ohs.rearrange("p (a e) -> p a e", e=32),
                 ebase.unsqueeze(1).to_broadcast([128, NST * 2, 32]), ALU.mult),
       reads=["ohs", "ebase", "big"], writes=["big"])
    op("dve", red(slc_f, big.rearrange("p (a e) -> p a e", e=32), ALU.add), reads=["big"], writes=["slc_f"])
    op("dve", tt(slc_f, slc_f, pos_all, ALU.add), reads=["slc_f", "pos_all"], writes=["slc_f"])
    op("dve", cp(slot_c, slc_f), reads=["slc_f"], writes=["slot_c"])
    if dbg and stop_after == "tables":
        op("sp", dma(dbg2_d[:, 0:128], widx.bitcast(F32)), reads=["widx"], writes=["dbg2"], dma=True)
        op("sp", dma(dbg2_d[:, 512:1024], sidx4.bitcast(F32)), reads=["sidx4"], writes=["dbg2"], dma=True)
        op("sp", dma(dbg2_d[:, 1024:1024 + NST * 2], slot_c.bitcast(F32)), reads=["slot_c"], writes=["dbg2"], dma=True)
        op("sp", dma(dbg2_d[:, 256:288], pends), reads=["pends"], writes=["dbg2"], dma=True)
        sc.emit()
        return nc

    m3 = ar.mark()
    NSTG = 3
    stg3 = [ar.f32(4096) for _ in range(NSTG)]
    wg_bf = [ar.bf(8 * 512).rearrange("p (k f) -> p k f", k=8) for _ in range(2)]
    wu_bf = [ar.bf(8 * 512).rearrange("p (k f) -> p k f", k=8) for _ in range(2)]
    wd_bf = [ar.bf(4 * 1024).rearrange("p (k f) -> p k f", k=4) for _ in range(2)]
    Xg = [ar.bf(4 * D).rearrange("p (j d) -> p j d", j=4) for _ in range(2)]
    h2T = ar.bf(8 * BLK).rearrange("p (k t) -> p k t", k=8)
    hidT = ar.bf(4 * BLK).rearrange("p (k t) -> p k t", k=4)
    sgb = [ar.f32(BLK) for _ in range(2)]
    ysb = [ar.f32(D) for _ in range(2)]
    print("arena used phase3 (words):", ar.off, "of", ARW)
    stg_i = [0]
    cast_i = [0]
    wgv = wg_d.rearrange("e (p kk) f -> (e p) (kk f)", kk=8)
    wuv = wu_d.rearrange("e (p kk) f -> (e p) (kk f)", kk=8)
    wdv = wd_d.rearrange("e (p kk) f -> (e p) (kk f)", kk=4)

    def issue_loads(b):
        q = b % 2
        op("pool", lambda e, q=q, b=b: e.indirect_dma_start(
            out=idx_sb[q][:, 0:8], out_offset=None, in_=sidx_d.rearrange("(r f) c -> r (f c)", f=4),
            in_offset=bass.IndirectOffsetOnAxis(ap=sidx4[:, b:b + 1], axis=0)),
           reads=["sidx4", "sidx_scr"], writes=[("idx", q)], dma=True)
        casts = []
        for (wv, dst, nm) in ((wgv, wg_bf[q], "wg"), (wuv, wu_bf[q], "wu"), (wdv, wd_bf[q], "wd")):
            sgi = stg_i[0] % NSTG
            stg_i[0] += 1
            op("pool", lambda e, wv=wv, sgi=sgi, b=b: e.indirect_dma_start(
                out=stg3[sgi], out_offset=None, in_=wv,
                in_offset=bass.IndirectOffsetOnAxis(ap=widx[:, b:b + 1], axis=0)),
               reads=["widx"], writes=[("stg3", sgi)], dma=True)
            casts.append((dst, nm, sgi))
        for j in range(4):
            op("pool", lambda e, j=j, q=q: e.indirect_dma_start(
                out=Xg[q][:, j, :], out_offset=None, in_=xn2_d[:, :],
                in_offset=bass.IndirectOffsetOnAxis(ap=idx_sb[q][:, 2 * j:2 * j + 1], axis=0)),
               reads=[("idx", q), "xn2_scr"], writes=[("Xg", q, j)], dma=True)
        sc.rec_begin()
        for (dst, nm, sgi) in casts:
            dflat = dst.rearrange("p k f -> p (k f)")
            for hf in range(2):
                sl = slice(hf * 2048, (hf + 1) * 2048)
                if nm != "wd":
                    ce = ("act", "pool", "dve", "act")[cast_i[0] % 4]
                    cast_i[0] += 1
                    op(ce, (acp if ce == "act" else cp)(dflat[:, sl], stg3[sgi][:, sl]),
                       reads=[("stg3", sgi)], writes=[(nm, q, hf)])
                else:
                    ce = ("dve", "pool")[hf]
                    op(ce, tt(dflat[:, sl].rearrange("p (k f) -> p k f", k=2),
                              stg3[sgi][:, sl].rearrange("p (k f) -> p k f", k=2),
                              gt2_bc.unsqueeze(1).to_broadcast([128, 2, D]), ALU.mult),
                       reads=[("stg3", sgi), "gt2_bc"], writes=[(nm, q, hf)])
        return sc.rec_end()

    h2_keys = [("h2T", kc) for kc in range(8)]
    hid_keys = [("hidT", hc) for hc in range(4)]

    def stageA(b):
        q = b % 2
        for r4 in range(4):
            hb = r4 % 2
            fns = []
            for u in range(2):
                kc = r4 * 2 + u
                for j in range(4):
                    fns.append(tr(psTb[hb][:, u * BLK + j * 128:u * BLK + (j + 1) * 128],
                                  Xg[q][:, j, :].rearrange("p (m kk) -> p kk m", kk=8)[:, kc, :], ident_bf))
            op("pe", fns, reads=[("Xg", q, j) for j in range(4)] + ["ident_bf"], writes=[f"ps{hb}"])
            for u in range(2):
                kc = r4 * 2 + u
                op("act", act(h2T[:, kc, :], psTb[hb][:, u * BLK:(u + 1) * BLK], AF.Identity,
                              bias=bias2p[:, kc:kc + 1], scale=scale2p[:, kc:kc + 1]),
                   reads=[f"ps{hb}", "scale2p", "modP"], writes=[("h2T", kc)])

    def stageG(b):
        q = b % 2
        for hc in range(4):
            pg, kg = next_mm()
            op("pe", [mm(pg, wg_bf[q][:, kc, :].rearrange("p (m c) -> p c m", c=4)[:, hc, :], h2T[:, kc, :], start=(kc == 0), stop=(kc == 7))
                      for kc in range(8)], reads=h2_keys + [("wg", q, 0), ("wg", q, 1)], writes=[kg])
            pu, ku = next_mm()
            op("pe", [mm(pu, wu_bf[q][:, kc, :].rearrange("p (m c) -> p c m", c=4)[:, hc, :], h2T[:, kc, :], start=(kc == 0), stop=(kc == 7))
                      for kc in range(8)], reads=h2_keys + [("wu", q, 0), ("wu", q, 1)], writes=[ku])
            sgk = ("sgb", hc % 2)
            op("act", act(sgb[hc % 2], pg, AF.Sigmoid), reads=[kg], writes=[sgk])
            op("dve", tt(sgb[hc % 2], sgb[hc % 2], pg, ALU.mult), reads=[kg, sgk], writes=[sgk])
            op("dve", tt(hidT[:, hc, :], sgb[hc % 2], pu, ALU.mult), reads=[ku, sgk], writes=[("hidT", hc)])

    def stageD(b):
        q = b % 2
        for j in range(4):
            yq = (b * 4 + j) % 2
            for half in range(2):
                pd, kd_ = next_mm()
                op("pe", [mm(pd, hidT[:, hc, j * 128:(j + 1) * 128], wd_bf[q][:, hc, half * 512:(half + 1) * 512],
                             start=(hc == 0), stop=(hc == 3)) for hc in range(4)],
                   reads=hid_keys + [("wd", q, 0), ("wd", q, 1)], writes=[kd_])
                wtok = idx_sb[q].bitcast(F32)[:, 2 * j + 1:2 * j + 2]
                op("act", act(ysb[yq][:, half * 512:(half + 1) * 512], pd, AF.Identity, scale=wtok),
                   reads=[kd_, ("idx", q)], writes=[("ysb", yq)])
            op("sp", dma(ybuf_d[b * BLK:(b + 1) * BLK, :].rearrange("(p j) d -> p j d", j=4)[:, j, :], ysb[yq]),
               reads=[("ysb", yq)], writes=[("ybuf", b, j)], dma=True)

    sc.play(issue_loads(0))
    stageA(0)
    for b in range(NB):
        lists = []
        if b + 1 < NB:
            lists.append(issue_loads(b + 1))
        stageG(b)
        if b + 1 < NB:
            sc.rec_begin()
            stageA(b + 1)
            lists.append(sc.rec_end())
        sc.rec_begin()
        stageD(b)
        lists.append(sc.rec_end())
        sc.play(Sched.merge(lists))

    sc.barrier()
    ar.reset(m3)
    def _final_compute(st):
        f = F[st % NF]
        fk = ("F", st % NF)
        op("dve", tt(f["xm"], f["xm"], f["y0"], ALU.add), reads=[fk + ("xm",), fk + ("y0",)], writes=[fk + ("xm",)])
        op("dve", tt(f["xm"], f["xm"], f["y1"], ALU.add), reads=[fk + ("xm",), fk + ("y1",)], writes=[fk + ("xm",)])
        op("act", act(f["jk"], f["xm"], AF.Square, accum_out=f["st"][:, 0:1]), reads=[fk + ("xm",)], writes=[fk + ("st",), fk + ("jk",)])
        op("act", act(f["st"][:, 1:2], f["st"][:, 0:1], AF.Sqrt, bias=EPS, scale=1.0 / D), reads=[fk + ("st",)], writes=[fk + ("st",)])
        op("dve", lambda e, f=f: e.reciprocal(out=f["st"][:, 2:3], in_=f["st"][:, 1:2]), reads=[fk + ("st",)], writes=[fk + ("st",)])
        op("dve", stt(f["y0"], f["xm"], f["st"][:, 2:3], gf_bc, ALU.mult, ALU.mult),
           reads=[fk + ("xm",), fk + ("st",), "gf_bc"], writes=[fk + ("y0",)])
        op("sp", dma(out_d[st * 128:(st + 1) * 128, :], f["y0"]), reads=[fk + ("y0",)], writes=[("out", st)], dma=True)

    NF = 4
    F = [dict(xm=ar.f32(D), y0=ar.f32(D), y1=ar.f32(D), jk=ar.bf(D), st=ar.f32(4)) for _ in range(NF)]
    for st in range(NST):
        f = F[st % NF]
        fk = ("F", st % NF)
        op("sp", dma(f["xm"], xmid_d[st * 128:(st + 1) * 128, :]), reads=["xmid_scr"], writes=[fk + ("xm",)], dma=True)
        for k in range(2):
            op("pool", lambda e, f=f, k=k, st=st: e.indirect_dma_start(
                out=f["y%d" % k], out_offset=None, in_=ybuf_d[:, :],
                in_offset=bass.IndirectOffsetOnAxis(ap=slot_c[:, st * 2 + k:st * 2 + k + 1], axis=0)),
               reads=["slot_c"], writes=[fk + ("y%d" % k,)], dma=True)
        if st >= NF - 1:
            s2 = st - (NF - 1)
            _final_compute(s2)
    for s2 in range(max(NST - (NF - 1), 0), NST):
        _final_compute(s2)

    sc.emit()
    return nc


def host_consts(S):
    NST = S // 128
    cst = np.zeros((128, CS_N), np.float32)
    cst[:, CS_ID:CS_ID + 128] = np.eye(128, dtype=np.float32)
    p = np.arange(128)
    cst[:, CS_U:CS_U + 128] = (p[:, None] < p[None, :]).astype(np.float32)
    cst[:, CS_CM:CS_CM + 128] = (p[:, None] <= p[None, :]).astype(np.float32)
    cst[:, CS_HM] = (p < 64).astype(np.float32)
    cst[:, CS_HM + 1] = (p >= 64).astype(np.float32)
    cst[:, CS_TH:CS_TH + NTH] = (np.arange(NTH) * BLK).astype(np.float32)[None, :]
    rm = np.ones((512,), np.float32)
    rm[::128] = 0.0
    cst[:, CS_RM:CS_RM + 512] = rm[None, :]
    cst[:, CS_IO:CS_IO + 32] = np.arange(32, dtype=np.float32)[None, :]
    cst[:, CS_B5] = (p * BLK).astype(np.float32)
    tokid = (np.arange(NST)[None, :] * 128 + p[:, None]).astype(np.int32)
    return cst, tokid


def fm(v, n):
    return np.ascontiguousarray(np.asarray(v, np.float32).reshape(n, 128).T)


def host_inputs(inp, b, S):
    L = 0
    tab = np.zeros((128, TB_N), np.float32)
    tab[:, TB_BADA:TB_BADA + 48] = fm(inp["b_ada"][L], 48)
    tab[:, TB_GMIX:TB_GMIX + 8] = fm(inp["g_mix"][L], 8)
    tab[:, TB_GFFN:TB_GFFN + 8] = fm(inp["g_ffn"][L], 8)
    cw = np.asarray(inp["conv_w"][L], np.float32)
    for c in range(4):
        tab[:, TB_CW + c * 4:TB_CW + c * 4 + 4] = cw[:, c * 128:(c + 1) * 128].T
    tab[:, TB_CB:TB_CB + 4] = fm(inp["conv_b"][L], 4)
    tab[:, TB_BR:TB_BR + 4] = fm(inp["lru_br"][L], 4)
    tab[:, TB_BI:TB_BI + 4] = fm(inp["lru_bi"][L], 4)
    tab[:, TB_LAM:TB_LAM + 4] = fm(inp["lru_lambda"][L], 4)
    tab[:, TB_BA:TB_BA + 2] = fm(inp["gla_ba"][L], 2)
    tab[:, TB_GN] = np.asarray(inp["gla_gnorm"][L], np.float32)
    ba = np.asarray(inp["b_ada"][L], np.float32)
    tab[:, TB_BADAP:TB_BADAP + 8] = ba[3 * D:4 * D].reshape(128, 8)
    tab[:, TB_BADAP + 8:TB_BADAP + 16] = ba[4 * D:5 * D].reshape(128, 8)
    tab[:, TB_GFFNP:TB_GFFNP + 8] = np.asarray(inp["g_ffn"][L], np.float32).reshape(128, 8)

    def bd(w):
        w = np.asarray(w, np.float32)
        o = np.zeros((128, 4, 128), np.float32)
        for c in range(4):
            for hh in range(2):
                o[hh * 64:(hh + 1) * 64, c, hh * 64:(hh + 1) * 64] = w[2 * c + hh]
        return o.reshape(128, 512)

    cst, tokid = host_consts(S)
    m = {
        "x": np.ascontiguousarray(np.asarray(inp["x"][b], np.float32)),
        "cT": fm(inp["c"][b], 8),
        "w_ada": np.ascontiguousarray(np.asarray(inp["w_ada"][L], np.float32)),
        "tab": tab,
        "cst": cst,
        "tokid": tokid,
        "gf_bc": np.ascontiguousarray(np.broadcast_to(np.asarray(inp["g_final"], np.float32)[None, :], (128, D))),
        "br_bc": np.ascontiguousarray(np.broadcast_to(
            np.concatenate([np.asarray(inp["b_coarse"][L], np.float32),
                            np.asarray(inp["b_fine"][L], np.float32)])[None, :], (128, 36))),
        "w_in": np.ascontiguousarray(np.asarray(inp["w_in"][L], np.float32)),
        "w_out": np.ascontiguousarray(np.asarray(inp["w_out"][L], np.float32)),
        "bd_r": bd(inp["lru_wr"][L]),
        "bd_i": bd(inp["lru_wi"][L]),
        "wa2": np.ascontiguousarray(np.asarray(inp["gla_wa2"][L], np.float32)),
        "w_r": np.ascontiguousarray(np.concatenate([np.asarray(inp["w_coarse"][L], np.float32),
                                                    np.asarray(inp["w_fine"][L], np.float32)], axis=1)),
        "w_gate": np.ascontiguousarray(np.asarray(inp["w_gate"][L], np.float32)),
        "w_up": np.ascontiguousarray(np.asarray(inp["w_up"][L], np.float32)),
        "w_down": np.ascontiguousarray(np.asarray(inp["w_down"][L], np.float32)),
    }
    return m


def kernel(**inputs):
    B, S = inputs["x"].shape[0], inputs["x"].shape[1]
    nc = build(S)
    in_maps = [host_inputs(inputs, b, S) for b in range(B)]
    res = run_bass_kernel_spmd(nc, in_maps, core_ids=list(range(B)))
    return np.stack([np.asarray(r["out"]) for r in res.results], axis=0).astype(np.float32)
```
